# Optimizing a Trainium2 kernel written in Bass

```python
import numpy as np
import jax
import jax.numpy as jnp
from jax import lax

D_MODEL = 1024
BATCH = 4
SEQ = 4096
DEPTH = 2

GRID_W = 64
CTX_LEN = 256
N_EVEN = (DEPTH + 1) // 2
N_ODD = DEPTH // 2
CHUNK = 64
CONV_K = 4
CONV_PAD_L = 2
CONV_PAD_R = 1
EPS = 1e-6
M_HEADS = 4
M_DK = D_MODEL // (2 * M_HEADS)
M_DV = D_MODEL // M_HEADS
M_QK = 2 * M_HEADS * M_DK
M_V = M_HEADS * M_DV
LRU_W = D_MODEL
LRU_BLOCKS = 8
LRU_BW = LRU_W // LRU_BLOCKS
LRU_C = 8.0
G_HEADS = 4
G_DK = D_MODEL // (2 * G_HEADS)
G_DV = D_MODEL // G_HEADS
G_QK = 2 * G_HEADS * G_DK
G_V = G_HEADS * G_DV
G_RANK = 16
G_TAU = 16.0
S_HEADS = 16
S_P = D_MODEL // S_HEADS
S_INNER = S_HEADS * S_P
S_N = 128
S_GROUPS = 2
S_HPG = S_HEADS // S_GROUPS
S_XBC = S_INNER + 2 * S_GROUPS * S_N
FF_DENSE = 2816
N_EXPERTS = 8
TOP_K = 2
FF_EXPERT = 3584
MOE_BLOCK = 256
AB_SIZES = [M_QK, M_V, M_V, 4 * M_HEADS, LRU_W, LRU_W]
AB_IN = sum(AB_SIZES)
CD_SIZES = [G_QK, G_V, G_V, 2 * G_RANK, S_INNER, S_XBC, 2 * S_HEADS]
CD_IN = sum(CD_SIZES)

kernel_name = 'hybrid_mlstm_rglru_gla_ssd_moe_trunk'


def split_cols(a, sizes):
    return jnp.split(a, np.cumsum(sizes)[:-1].tolist(), axis=-1)


def rmsnorm(u, g):
    uf = u.astype(jnp.float32)
    uf = uf * lax.rsqrt(jnp.mean(uf * uf, axis=-1, keepdims=True) + EPS)
    return (uf * g.astype(jnp.float32)).astype(u.dtype)


def headnorm(u, g, n_heads):
    shp = u.shape
    uh = u.astype(jnp.float32).reshape(*shp[:-1], n_heads, shp[-1] // n_heads)
    uh = uh * lax.rsqrt(jnp.mean(uh * uh, axis=-1, keepdims=True) + EPS)
    return uh.reshape(shp) * g.astype(jnp.float32)


def adaln(u, g, shift, scale):
    return rmsnorm(u, g) * (1 + scale) + shift


def dwconv(u, w, b):
    y = lax.conv_general_dilated(u, w[:, None, :].astype(u.dtype), window_strides=(1,),
                                 padding=[(CONV_PAD_L, CONV_PAD_R)],
                                 dimension_numbers=('NWC', 'WIO', 'NWC'),
                                 feature_group_count=u.shape[-1])
    return y + b.astype(u.dtype)


def stream_conv(u, w, b, n_ctx, rows):
    uc, ul = u[:, :n_ctx], u[:, n_ctx:]
    bsz, s, ch = ul.shape
    yl = dwconv(ul.reshape(bsz * rows, GRID_W, ch), w, b).reshape(bsz, s, ch)
    return jnp.concatenate([dwconv(uc, w, b), yl], axis=1)


def flip_streams(a, n_ctx):
    return jnp.concatenate([jnp.flip(a[:, :n_ctx], axis=1), jnp.flip(a[:, n_ctx:], axis=1)], axis=1)


def to_chunks(a):
    bsz, t = a.shape[:2]
    return jnp.moveaxis(a.reshape(bsz, t // CHUNK, CHUNK, *a.shape[2:]), 1, 0)


def from_chunks(a):
    a = jnp.moveaxis(a, 0, 1)
    return a.reshape(a.shape[0], a.shape[1] * a.shape[2], *a.shape[3:])


def chunk_mask(n_trailing):
    m = jnp.tril(jnp.ones((CHUNK, CHUNK), dtype=bool))
    return m.reshape((1, CHUNK, CHUNK) + (1,) * n_trailing)


def mlstm_scan(q, k, v, ig, lf):
    bsz, _, nh, dk = q.shape
    dv = v.shape[-1]
    mask = chunk_mask(1)

    def step(carry, inp):
        cmat, nvec, m = carry
        qc, kc, vc, ic, fc = inp
        b = jnp.cumsum(fc, axis=1)
        g = b + m[:, None]
        dlog = jnp.where(mask, b[:, :, None] - b[:, None] + ic[:, None], -jnp.inf)
        m_t = jnp.maximum(g, dlog.max(axis=2))
        w_inter = jnp.exp(g - m_t)
        sc = jnp.einsum('bthd,bshd->btsh', qc, kc) * jnp.exp(dlog - m_t[:, :, None])
        num = w_inter[..., None] * jnp.einsum('bhvd,bthd->bthv', cmat, qc) + jnp.einsum('btsh,bshv->bthv', sc, vc)
        den = w_inter * jnp.einsum('bhd,bthd->bth', nvec, qc) + sc.sum(axis=2)
        h = num / jnp.maximum(jnp.abs(den), jnp.exp(-m_t))[..., None]
        bl = b[:, -1]
        a_s = bl[:, None] - b + ic
        m_new = jnp.maximum(bl + m, a_s.max(axis=1))
        decay = jnp.exp(bl + m - m_new)
        ws = jnp.exp(a_s - m_new[:, None])
        cmat = decay[..., None, None] * cmat + jnp.einsum('bsh,bshv,bshd->bhvd', ws, vc, kc)
        nvec = decay[..., None] * nvec + jnp.einsum('bsh,bshd->bhd', ws, kc)
        return (cmat, nvec, m_new), h

    init = (jnp.zeros((bsz, nh, dv, dk), jnp.float32), jnp.zeros((bsz, nh, dk), jnp.float32),
            jnp.zeros((bsz, nh), jnp.float32))
    _, hs = lax.scan(step, init, (to_chunks(q), to_chunks(k), to_chunks(v), to_chunks(ig), to_chunks(lf)))
    return from_chunks(hs)


def _lin_combine(left, right):
    a1, b1 = left
    a2, b2 = right
    return a1 * a2, a2 * b1 + b2


def rglru(u, wa, ba, wx, bx, lam):
    bsz, t, wdt = u.shape
    ub = u.reshape(bsz, t, LRU_BLOCKS, LRU_BW)
    r = jax.nn.sigmoid(jnp.einsum('btnc,ncd->btnd', ub, wa.astype(jnp.float32)).reshape(bsz, t, wdt) + ba)
    i = jax.nn.sigmoid(jnp.einsum('btnc,ncd->btnd', ub, wx.astype(jnp.float32)).reshape(bsz, t, wdt) + bx)
    log_a = -LRU_C * r * jax.nn.softplus(-lam.astype(jnp.float32))
    a = jnp.exp(log_a)
    b = jnp.sqrt(-jnp.expm1(2.0 * log_a)) * (i * u)
    _, hs = lax.associative_scan(_lin_combine, (a, b), axis=1)
    return hs


def gla_scan(q, k, v, lg):
    bsz, _, nh, dk = q.shape
    dv = v.shape[-1]
    mask = chunk_mask(2)

    def step(state, inp):
        qc, kc, vc, gc = inp
        b = jnp.cumsum(gc, axis=1)
        inter = jnp.einsum('bthd,bhdv->bthv', qc * jnp.exp(b), state)
        decay = jnp.exp(jnp.where(mask, b[:, :, None] - b[:, None], -jnp.inf))
        att = jnp.einsum('bthd,bshd,btshd->btsh', qc, kc, decay)
        out = inter + jnp.einsum('btsh,bshv->bthv', att, vc)
        bl = b[:, -1]
        state = jnp.exp(bl)[..., None] * state + jnp.einsum('bshd,bshv->bhdv', kc * jnp.exp(bl[:, None] - b), vc)
        return state, out

    init = jnp.zeros((bsz, nh, dk, dv), jnp.float32)
    _, outs = lax.scan(step, init, (to_chunks(q), to_chunks(k), to_chunks(v), to_chunks(lg)))
    return from_chunks(outs)


def ssd_scan(x, dt, la, bm, cm):
    bsz = x.shape[0]
    mask = chunk_mask(2)

    def step(state, inp):
        xc, dtc, lac, bc, cc = inp
        b = jnp.cumsum(lac, axis=1)
        cb = jnp.einsum('btgn,bsgn->btsg', cc, bc)
        decay = jnp.exp(jnp.where(mask, b[:, :, None] - b[:, None], -jnp.inf))
        intra = jnp.einsum('btsg,btsge,bsgep->btgep', cb, decay, xc * dtc[..., None])
        inter = jnp.einsum('btgn,bgepn->btgep', cc, state) * jnp.exp(b)[..., None]
        bl = b[:, -1]
        w = jnp.exp(bl[:, None] - b) * dtc
        state = jnp.exp(bl)[..., None, None] * state + jnp.einsum('bsge,bsgep,bsgn->bgepn', w, xc, bc)
        return state, intra + inter

    init = jnp.zeros((bsz, S_GROUPS, S_HPG, S_P, S_N), jnp.float32)
    _, ys = lax.scan(step, init, (to_chunks(x), to_chunks(dt), to_chunks(la), to_chunks(bm), to_chunks(cm)))
    return from_chunks(ys)


def mix_ab(h, n_ctx, rows, in_w, m_conv_w, m_conv_b, m_gate_b, m_norm_w, l_conv_w, l_conv_b,
           l_wa, l_ba, l_wx, l_bx, l_lam, out_w):
    bsz, t, _ = h.shape
    fl = lambda a: flip_streams(a, n_ctx)
    qk, v, o, gt, lx, lg = split_cols(h @ in_w, AB_SIZES)
    qk = jax.nn.silu(stream_conv(qk, m_conv_w, m_conv_b, n_ctx, rows))
    q, k = jnp.split(qk, 2, axis=-1)
    q = q.reshape(bsz, t, M_HEADS, M_DK).astype(jnp.float32)
    k = k.reshape(bsz, t, M_HEADS, M_DK).astype(jnp.float32) * (M_DK ** -0.5)
    v = v.reshape(bsz, t, M_HEADS, M_DV).astype(jnp.float32)
    gt = gt.astype(jnp.float32).reshape(bsz, t, 4, M_HEADS) + m_gate_b.astype(jnp.float32)
    ig_f, ig_b = gt[:, :, 0], gt[:, :, 1]
    lf_f, lf_b = jax.nn.log_sigmoid(gt[:, :, 2]), jax.nn.log_sigmoid(gt[:, :, 3])
    hm = mlstm_scan(q, k, v, ig_f, lf_f) + fl(mlstm_scan(fl(q), fl(k), fl(v), fl(ig_b), fl(lf_b)))
    hm = headnorm(hm.reshape(bsz, t, M_V), m_norm_w, M_HEADS) * jax.nn.sigmoid(o.astype(jnp.float32))
    xl = stream_conv(lx, l_conv_w, l_conv_b, n_ctx, rows).astype(jnp.float32)
    hl = (rglru(xl, l_wa[0], l_ba[0], l_wx[0], l_bx[0], l_lam[0])
          + fl(rglru(fl(xl), l_wa[1], l_ba[1], l_wx[1], l_bx[1], l_lam[1])))
    hl = hl * jax.nn.gelu(lg.astype(jnp.float32))
    return jnp.concatenate([hm, hl], axis=-1).astype(h.dtype) @ out_w


def mix_cd(h, n_ctx, rows, in_w, g_alpha_w, g_alpha_b, g_norm_w, s_conv_w, s_conv_b,
           s_dt_bias, s_A_log, s_D, s_norm_w, out_w):
    bsz, t, _ = h.shape
    fl = lambda a: flip_streams(a, n_ctx)
    gqk, gv, gr, ga, z, xbc, dt = split_cols(h @ in_w, CD_SIZES)
    gq, gk = jnp.split(gqk, 2, axis=-1)
    gq = gq.reshape(bsz, t, G_HEADS, G_DK).astype(jnp.float32) * (G_DK ** -0.5)
    gk = gk.reshape(bsz, t, G_HEADS, G_DK).astype(jnp.float32)
    gv = gv.reshape(bsz, t, G_HEADS, G_DV).astype(jnp.float32)
    ga = ga.astype(jnp.float32).reshape(bsz, t, 2, G_RANK)

    def loggate(d):
        pre = ga[:, :, d] @ g_alpha_w[d].astype(jnp.float32) + g_alpha_b[d].astype(jnp.float32)
        return (jax.nn.log_sigmoid(pre) / G_TAU).reshape(bsz, t, G_HEADS, G_DK)

    og = gla_scan(gq, gk, gv, loggate(0)) + fl(gla_scan(fl(gq), fl(gk), fl(gv), fl(loggate(1))))
    og = headnorm(og.reshape(bsz, t, G_V), g_norm_w, G_HEADS) * jax.nn.silu(gr.astype(jnp.float32))
    xbc = jax.nn.silu(stream_conv(xbc, s_conv_w, s_conv_b, n_ctx, rows)).astype(jnp.float32)
    sx, sb, sc = split_cols(xbc, [S_INNER, S_GROUPS * S_N, S_GROUPS * S_N])
    sx = sx.reshape(bsz, t, S_GROUPS, S_HPG, S_P)
    sb = sb.reshape(bsz, t, S_GROUPS, S_N)
    sc = sc.reshape(bsz, t, S_GROUPS, S_N)
    dt = jax.nn.softplus(dt.astype(jnp.float32).reshape(bsz, t, 2, S_HEADS) + s_dt_bias.astype(jnp.float32))
    la = dt * (-jnp.exp(s_A_log.astype(jnp.float32)))
    grp = lambda a, d: a[:, :, d].reshape(bsz, t, S_GROUPS, S_HPG)
    ys = (ssd_scan(sx, grp(dt, 0), grp(la, 0), sb, sc)
          + fl(ssd_scan(fl(sx), fl(grp(dt, 1)), fl(grp(la, 1)), fl(sb), fl(sc))))
    ys = ys + s_D.astype(jnp.float32).reshape(S_GROUPS, S_HPG)[..., None] * sx
    ys = rmsnorm(ys.reshape(bsz, t, S_INNER) * jax.nn.silu(z.astype(jnp.float32)), s_norm_w)
    return jnp.concatenate([og, ys], axis=-1).astype(h.dtype) @ out_w


def swiglu(h, w1, w2):
    g, u = jnp.split(h @ w1, 2, axis=-1)
    return (jax.nn.silu(g) * u) @ w2


def moe_swiglu(h, router_w, router_b, w1, w2):
    bsz, t, dm = h.shape
    tok = h.reshape(-1, dm)
    n = tok.shape[0]
    logits = (tok @ router_w).astype(jnp.float32) + router_b.astype(jnp.float32)
    top_logit, top_idx = lax.top_k(logits, TOP_K)
    gates = jax.nn.softmax(top_logit, axis=-1).astype(h.dtype)
    flat_e = top_idx.reshape(-1)
    n_assign = n * TOP_K
    flat_tok = jnp.arange(n_assign, dtype=jnp.int32) // TOP_K
    order = jnp.argsort(flat_e)
    sorted_e = flat_e[order]
    counts = jnp.bincount(flat_e, length=N_EXPERTS)
    padded = (counts + MOE_BLOCK - 1) // MOE_BLOCK * MOE_BLOCK
    starts = jnp.cumsum(counts) - counts
    ends_p = jnp.cumsum(padded)
    pstarts = ends_p - padded
    slot_sorted = (pstarts[sorted_e] + jnp.arange(n_assign, dtype=jnp.int32) - starts[sorted_e]).astype(jnp.int32)
    slot = jnp.zeros((n_assign,), jnp.int32).at[order].set(slot_sorted)
    n_blocks = -(-n_assign // MOE_BLOCK) + N_EXPERTS
    slot_tok = jnp.full((n_blocks * MOE_BLOCK,), n, jnp.int32).at[slot].set(flat_tok)
    block_e = jnp.minimum(jnp.searchsorted(ends_p, jnp.arange(n_blocks, dtype=jnp.int32) * MOE_BLOCK, side='right'),
                          N_EXPERTS - 1)
    tok_pad = jnp.concatenate([tok, jnp.zeros((1, dm), tok.dtype)], axis=0)
    xs = tok_pad[slot_tok].reshape(n_blocks, MOE_BLOCK, dm)

    def expert_block(args):
        xb, e = args
        return swiglu(xb, w1[e], w2[e])

    ys = lax.map(expert_block, (xs, block_e)).reshape(-1, dm)
    y = (ys[slot].reshape(n, TOP_K, dm) * gates[..., None]).sum(axis=1)
    return y.reshape(bsz, t, dm)


def setup_inputs(seed: int = 0) -> dict:
    key = jax.random.key(seed)
    ks = iter(jax.random.split(key, 48))

    def nrm(shape, scale):
        return jax.random.normal(next(ks), shape, jnp.float32) * scale

    def unif(shape, lo, hi):
        return jax.random.uniform(next(ks), shape, jnp.float32, lo, hi)

    D = D_MODEL
    x = nrm((BATCH, SEQ, D), 1.0)
    c = nrm((BATCH, D), 1.0)
    ctx = nrm((BATCH, CTX_LEN, D), 1.0)
    c_ctx = nrm((D,), 1.0)
    mod_w = nrm((DEPTH, D, 6 * D), 0.5 * D ** -0.5)
    mod_b = nrm((DEPTH, 6 * D), 0.01)
    norm_g = 1.0 + nrm((DEPTH, 4, D), 0.02)
    ab_in_w = nrm((N_EVEN, D, AB_IN), D ** -0.5)
    m_conv_w = nrm((N_EVEN, CONV_K, M_QK), 0.5)
    m_conv_b = nrm((N_EVEN, M_QK), 0.01)
    m_gate_b = jnp.concatenate([nrm((N_EVEN, 2, M_HEADS), 0.1), unif((N_EVEN, 2, M_HEADS), 3.0, 6.0)], axis=1)
    m_norm_w = 1.0 + nrm((N_EVEN, M_V), 0.02)
    l_conv_w = nrm((N_EVEN, CONV_K, LRU_W), 0.5)
    l_conv_b = nrm((N_EVEN, LRU_W), 0.01)
    l_wa = nrm((N_EVEN, 2, LRU_BLOCKS, LRU_BW, LRU_BW), LRU_BW ** -0.5)
    l_ba = nrm((N_EVEN, 2, LRU_W), 0.01)
    l_wx = nrm((N_EVEN, 2, LRU_BLOCKS, LRU_BW, LRU_BW), LRU_BW ** -0.5)
    l_bx = nrm((N_EVEN, 2, LRU_W), 0.01)
    a_c = unif((N_EVEN, 2, LRU_W), 0.9, 0.999) ** (1.0 / LRU_C)
    l_lam = jnp.log(a_c) - jnp.log1p(-a_c)
    ab_out_w = nrm((N_EVEN, M_V + LRU_W, D), (M_V + LRU_W) ** -0.5)
    ffn_w1 = nrm((N_EVEN, D, 2 * FF_DENSE), D ** -0.5)
    ffn_w2 = nrm((N_EVEN, FF_DENSE, D), FF_DENSE ** -0.5)
    cd_in_w = nrm((N_ODD, D, CD_IN), D ** -0.5)
    g_alpha_w = nrm((N_ODD, 2, G_RANK, G_HEADS * G_DK), G_RANK ** -0.5)
    g_alpha_b = nrm((N_ODD, 2, G_HEADS * G_DK), 0.1)
    g_norm_w = 1.0 + nrm((N_ODD, G_V), 0.02)
    s_conv_w = nrm((N_ODD, CONV_K, S_XBC), 0.5)
    s_conv_b = nrm((N_ODD, S_XBC), 0.01)
    dt0 = jnp.exp(unif((N_ODD, 2, S_HEADS), float(np.log(1e-3)), float(np.log(1e-1))))
    s_dt_bias = dt0 + jnp.log(-jnp.expm1(-dt0))
    s_A_log = jnp.log(unif((N_ODD, 2, S_HEADS), 1.0, 16.0))
    s_D = 1.0 + nrm((N_ODD, S_HEADS), 0.1)
    s_norm_w = 1.0 + nrm((N_ODD, S_INNER), 0.02)
    cd_out_w = nrm((N_ODD, G_V + S_INNER, D), (G_V + S_INNER) ** -0.5)
    router_w = nrm((N_ODD, D, N_EXPERTS), D ** -0.5)
    router_b = nrm((N_ODD, N_EXPERTS), 0.01)
    moe_w1 = nrm((N_ODD, N_EXPERTS, D, 2 * FF_EXPERT), D ** -0.5)
    moe_w2 = nrm((N_ODD, N_EXPERTS, FF_EXPERT, D), FF_EXPERT ** -0.5)
    return {'x': x, 'c': c, 'ctx': ctx, 'c_ctx': c_ctx, 'mod_w': mod_w, 'mod_b': mod_b, 'norm_g': norm_g,
            'ab_in_w': ab_in_w, 'm_conv_w': m_conv_w, 'm_conv_b': m_conv_b, 'm_gate_b': m_gate_b,
            'm_norm_w': m_norm_w, 'l_conv_w': l_conv_w, 'l_conv_b': l_conv_b, 'l_wa': l_wa, 'l_ba': l_ba,
            'l_wx': l_wx, 'l_bx': l_bx, 'l_lam': l_lam, 'ab_out_w': ab_out_w, 'ffn_w1': ffn_w1, 'ffn_w2': ffn_w2,
            'cd_in_w': cd_in_w, 'g_alpha_w': g_alpha_w, 'g_alpha_b': g_alpha_b, 'g_norm_w': g_norm_w,
            's_conv_w': s_conv_w, 's_conv_b': s_conv_b, 's_dt_bias': s_dt_bias, 's_A_log': s_A_log, 's_D': s_D,
            's_norm_w': s_norm_w, 'cd_out_w': cd_out_w, 'router_w': router_w, 'router_b': router_b,
            'moe_w1': moe_w1, 'moe_w2': moe_w2}


def reference(x, c, ctx, c_ctx, mod_w, mod_b, norm_g,
              ab_in_w, m_conv_w, m_conv_b, m_gate_b, m_norm_w, l_conv_w, l_conv_b, l_wa, l_ba,
              l_wx, l_bx, l_lam, ab_out_w, ffn_w1, ffn_w2,
              cd_in_w, g_alpha_w, g_alpha_b, g_norm_w, s_conv_w, s_conv_b, s_dt_bias, s_A_log, s_D,
              s_norm_w, cd_out_w, router_w, router_b, moe_w1, moe_w2):
    bsz, seq, _ = x.shape
    n_ctx = ctx.shape[1]
    rows = seq // GRID_W
    xl, xc = x, ctx
    for layer in range(DEPTH):
        j = layer // 2
        last = layer == DEPTH - 1
        mlm = (jax.nn.silu(c) @ mod_w[layer] + mod_b[layer]).reshape(bsz, 6, 1, D_MODEL)
        ml = [mlm[:, i] for i in range(6)]
        mc = (jax.nn.silu(c_ctx) @ mod_w[layer] + mod_b[layer]).reshape(6, D_MODEL)
        g = norm_g[layer]
        h = jnp.concatenate([adaln(xc, g[0], mc[0], mc[1]), adaln(xl, g[0], ml[0], ml[1])], axis=1)
        if layer % 2 == 0:
            y = mix_ab(h, n_ctx, rows, ab_in_w[j], m_conv_w[j], m_conv_b[j], m_gate_b[j], m_norm_w[j],
                       l_conv_w[j], l_conv_b[j], l_wa[j], l_ba[j], l_wx[j], l_bx[j], l_lam[j], ab_out_w[j])
        else:
            y = mix_cd(h, n_ctx, rows, cd_in_w[j], g_alpha_w[j], g_alpha_b[j], g_norm_w[j], s_conv_w[j],
                       s_conv_b[j], s_dt_bias[j], s_A_log[j], s_D[j], s_norm_w[j], cd_out_w[j])
        y = rmsnorm(y, g[1])
        xl = xl + ml[2] * y[:, n_ctx:]
        hl = adaln(xl, g[2], ml[3], ml[4])
        if last:
            h2 = hl
        else:
            xc = xc + mc[2] * y[:, :n_ctx]
            h2 = jnp.concatenate([adaln(xc, g[2], mc[3], mc[4]), hl], axis=1)
        if layer % 2 == 0:
            f = swiglu(h2, ffn_w1[j], ffn_w2[j])
        else:
            f = moe_swiglu(h2, router_w[j], router_b[j], moe_w1[j], moe_w2[j])
        f = rmsnorm(f, g[3])
        xl = xl + ml[5] * f[:, f.shape[1] - seq:]
        if not last:
            xc = xc + mc[5] * f[:, :n_ctx]
    return xl
```

```python
import numpy as np
import concourse.bass as bass
import concourse.mybir as mybir
from concourse.bass_utils import run_bass_kernel_spmd
from concourse.alu_op_type import AluOpType as ALU
from contextlib import ExitStack

AF = mybir.ActivationFunctionType
F32 = mybir.dt.float32
BF16 = mybir.dt.bfloat16
AX = mybir.AxisListType

N_DMA_SLOTS = 40


class Prog:
    ENGS = ("pe", "act", "dve", "pool", "sp")

    def __init__(self, nc, es):
        self.nc = nc
        self.ops = []
        self.final = []
        self.esem = {e: es.enter_context(nc.semaphore("s_" + e)) for e in self.ENGS}
        self.dsem = [es.enter_context(nc.semaphore("d%d" % k)) for k in range(N_DMA_SLOTS)]
        self.cnt = {e: 0 for e in self.ENGS}
        self.dcnt = [0] * N_DMA_SLOTS
        self.nslot = 0

    SEG_KEYS = ("G0", "G1", "G2", "G3", "QK", "XB")

    @classmethod
    def _expand(cls, keys):
        out = []
        for k in keys:
            if k in cls.SEG_KEYS:
                out += [k + "/0", k + "/1"]
            else:
                out.append(k)
        return tuple(out)

    def op(self, eng, fn, r=(), w=(), dma=False):
        self.ops.append(dict(eng=eng, fn=fn, r=self._expand(r), w=self._expand(w), dma=dma))
        return len(self.ops) - 1

    def pe(self, fn, r=(), w=()):
        return self.op("pe", fn, r, w)

    def act(self, fn, r=(), w=()):
        return self.op("act", fn, r, w)

    def dve(self, fn, r=(), w=()):
        return self.op("dve", fn, r, w)

    def pool(self, fn, r=(), w=()):
        return self.op("pool", fn, r, w)

    def dma(self, q, out, in_, r=(), w=(), final=False):
        i = self.op(q, lambda e: e.dma_start(out=out, in_=in_), r, w, dma=True)
        return i

    class _Cap:
        def __init__(self, prog):
            self.prog = prog

        def __enter__(self):
            self.saved = self.prog.ops
            self.prog.ops = []
            self.ops = None
            return self

        def __exit__(self, *a):
            self.ops = self.prog.ops
            self.prog.ops = self.saved
            return False

    def capture(self):
        return Prog._Cap(self)

    def add_interleaved(self, caps):
        lists = [c.ops for c in caps]
        n = max(len(l) for l in lists)
        for j in range(n):
            for l in lists:
                if j < len(l):
                    self.ops.append(l[j])

    def emit(self):
        nc = self.nc
        ops = self.ops
        self.ops = []
        last_w = {}
        readers = {}
        slot_last = {}
        for i, o in enumerate(ops):
            deps = set()
            for k in o["r"]:
                if k in last_w:
                    deps.add(last_w[k])
                if k.startswith("pb"):
                    deps.update(j for j in readers.get(k, ()) if ops[j]["eng"] != o["eng"])
            for k in o["w"]:
                if k in last_w:
                    deps.add(last_w[k])
                deps.update(readers.get(k, ()))
            if o["dma"]:
                s = self.nslot % N_DMA_SLOTS
                self.nslot += 1
                o["slot"] = s
                if s in slot_last:
                    deps.add(slot_last[s])
                slot_last[s] = i
            deps.discard(i)
            if o["eng"] == "pe":
                deps = {d for d in deps if not (ops[d]["eng"] == "pe")}
            o["deps"] = deps
            for k in o["r"]:
                readers.setdefault(k, []).append(i)
            for k in o["w"]:
                last_w[k] = i
                readers[k] = []
        needed = set()
        for o in ops:
            needed.update(o["deps"])
        esem, dsem, cnt, dcnt = self.esem, self.dsem, self.cnt, self.dcnt
        for i, o in enumerate(ops):
            if o["dma"]:
                dcnt[o["slot"]] += 16
                o["tok"] = (("d", o["slot"]), dsem[o["slot"]], dcnt[o["slot"]])
            elif i in needed:
                cnt[o["eng"]] += 1
                o["tok"] = (("e", o["eng"]), esem[o["eng"]], cnt[o["eng"]])
            else:
                o["tok"] = None
        dfinal = list(dcnt)
        with nc.Block() as block:
            def run(engname, eng):
                waited = {}
                for i, o in enumerate(ops):
                    if o["eng"] != engname:
                        continue
                    need = {}
                    for d in o["deps"]:
                        key, sem, val = ops[d]["tok"]
                        if waited.get(key, 0) < val:
                            if need.get(key, (None, 0))[1] < val:
                                need[key] = (sem, val)
                    for key, (sem, val) in need.items():
                        eng.wait_ge(sem, val)
                        waited[key] = val
                    inst = o["fn"](eng)
                    if o["tok"] is not None:
                        inst.then_inc(o["tok"][1], 16 if o["dma"] else 1)
                if engname == "sp":
                    for k in range(N_DMA_SLOTS):
                        if dfinal[k] > 0:
                            eng.wait_ge(dsem[k], dfinal[k])

            @block.tensor
            def _(e):
                run("pe", e)

            @block.scalar
            def _(e):
                run("act", e)

            @block.vector
            def _(e):
                run("dve", e)

            @block.gpsimd
            def _(e):
                run("pool", e)

            @block.sync
            def _(e):
                run("sp", e)


D = 1024
EPS = 1e-6


_PHASE = [0]


def _mk(nc, es):
    _PHASE[0] += 1
    pfx = "p%d_" % _PHASE[0]
    sb = lambda name, shape, dt=F32: es.enter_context(nc.sbuf_tensor(pfx + name, shape, dt))
    ps = lambda name, shape, dt=F32: es.enter_context(nc.psum_tensor(pfx + name, shape, dt))
    return sb, ps


def emit_pe_warm(P, pbw, onesb, hT, n):
    for _ in range(n):
        P.pe(lambda e: e.matmul(pbw[:, 0:512], lhsT=onesb[:], rhs=hT[:, 0, 0:512], start=True, stop=True), r=["onesb", "hT"], w=["pb7"])


def emit_tok2fm(P, pb7, identb, ob, obkey, obT, obTkey, dst_ap):
    pT = pb7[:].bitcast(BF16)
    for cc in range(2):
        for chb in range(2):
            q = chb * 2 + cc
            P.pe(lambda e, cc=cc, chb=chb, q=q: e.transpose(pT[:, q * 128:(q + 1) * 128], ob[:, cc * 256 + chb * 128:cc * 256 + (chb + 1) * 128], identb[:]),
                 r=[obkey, "identb"], w=["pb7"])
    P.act(lambda e: e.activation(out=obT[:], in_=pT[:, 0:512], func=AF.Copy), r=["pb7"], w=[obTkey])
    P.dma("sp", dst_ap, obT[:].rearrange("p (b t) -> p b t", b=2), r=[obTkey])


def emit_modulation(P, nc, sb, pb7, modw, modb, cT, nch, mwbufs):
    cs = sb("cs", [128, 8, 2]); csr = sb("csr", [128, 8, 2]); mb = sb("mb", [128, nch]); modT = sb("modT", [128, nch, 2])
    mw = [b for b, _ in mwbufs]; mwk = [k for _, k in mwbufs]
    P.dma("sp", csr[:], cT, w=["csr"])
    P.dma("sp", mb[:], modb, w=["mb"])
    P.act(lambda e: e.activation(out=cs[:], in_=csr[:], func=AF.Silu), r=["csr"], w=["cs"])
    for ch in range(nch):
        s = ch % len(mw)
        P.dma("sp", mw[s], modw[ch], w=mwk[s])
        for k in range(8):
            P.pe(lambda e, s=s, k=k, ch=ch: e.matmul(pb7[:, 2 * ch:2 * ch + 2], lhsT=mw[s][:, k, :], rhs=cs[:, k, :],
                                                     start=(k == 0), stop=(k == 7)),
                 r=mwk[s] + ["cs"], w=["pb7"])
    P.dve(lambda e: e.tensor_tensor(out=modT[:], in0=pb7[:, 0:2 * nch].rearrange("p (c t) -> p c t", t=2),
                                    in1=mb[:].unsqueeze(2).to_broadcast([128, nch, 2]), op=ALU.add),
          r=["pb7", "mb"], w=["modT"])
    return modT


def phase_post(nc, P, pb, layer, dr, tiles, xsrc, mixsrc, dst, msel_d=None):
    moe = layer == 1
    NJ = 28 if moe else 22
    NE = 8 if moe else 1
    outw = dr("outw", [128, 16, 1024]); w1 = dr("w1", [NE * NJ, 128, 8, 256]); w2 = dr("w2", [NE * 8, 128, NJ, 128])
    modw = dr("modw", [32, 128, 8, 128]); modb = dr("modb", [128, 32]); cT = dr("cT", [128, 8, 2]); gT = dr("gT", [128, 3, 8])
    if moe:
        routw = dr("routw", [128, 8, 8]); routb = dr("routb", [128, 8]); identd = dr("ident", [128, 128]); seld = dr("sel", [8, 1024])
        snwd = dr("snw", [128, 8])
    with ExitStack() as es:
        sb, ps = _mk(nc, es)
        ow = sb("ow", [128, 16, 1024], BF16); mx = sb("mx", [128, 16, 512], BF16); xl = sb("xl", [128, 8, 512])
        y = sb("y", [128, 8, 512]); sq = sb("sq", [128, 8, 512], BF16); h2 = sb("h2", [128, 8, 512], BF16)
        hid = sb("hid", [128, NJ, 512], BF16)
        NW1, NW2 = (3, 3) if moe else (6, 6)
        w1b = [sb("w1b%d" % i, [128, 8, 256], BF16)[:] for i in range(NW1)]
        w2b = [sb("w2b%d" % i, [128, NJ, 128], BF16)[:] for i in range(NW2)]
        w1k = [["w1b%d" % i] for i in range(NW1)]; w2k = [["w2b%d" % i] for i in range(NW2)]
        tmp = [sb("tmp%d" % i, [128, 512]) for i in range(2)]
        sg = [sb("sg%d" % i, [128, 512]) for i in range(2)]
        rstd = sb("rstd", [128, 512]); gsb = sb("gsb", [128, 3, 8])
        onesb = sb("onesb", [128, 128], BF16); epsc = sb("epsc", [128, 1])
        A1 = sb("A1", [128, 8, 2]); B2 = sb("B2", [128, 8, 2]); A3 = sb("A3", [128, 8, 2])
        if moe:
            h2f = sb("h2f", [128, 8, 512]); Gb = sb("Gb", [128, 8, 512]); rw = sb("rw", [128, 8, 8]); rb = sb("rb", [128, 8])
            ident = sb("ident_sb", [128, 128]); sel = sb("sel_sb", [8, 1024]); Lg = sb("Lg", [128, 4, 8]); mx8 = sb("mx8", [128, 4, 8])
            msk = sb("msk", [128, 4, 8]); Eg = sb("Eg", [128, 4, 8]); den = sb("den", [128, 4]); gTs = sb("gTs", [8, 512])
            snw = sb("snw_sb", [128, 8])
            for i in range(2):
                v = h2f[:, 2 * i:2 * i + 2, :].rearrange("p a b -> p (a b)").bitcast(BF16).rearrange("p (k c) -> p k c", c=256)
                w1b.append(v); w1k.append(["h2f%d" % (2 * i), "h2f%d" % (2 * i + 1)])
            v = h2f[:, 4:8, :].rearrange("p a b -> p (a b)").bitcast(BF16)[:, 0:NJ * 128].rearrange("p (j c) -> p j c", c=128)
            w2b.append(v); w2k.append(["h2f%d" % m for m in range(4, 8)])
            NW1 = len(w1b); NW2 = len(w2b)
            P.dma("sp", snw[:], snwd, w=["snw"])
            P.dma("sp", rw[:], routw, w=["rw"]); P.dma("sp", rb[:], routb, w=["rb"])
            P.dma("sp", ident[:], identd, w=["ident"]); P.dma("sp", sel[:], seld, w=["sel"])
        P.dve(lambda e: e.memset(onesb[:], 1.0), w=["onesb"])
        P.dve(lambda e: e.memset(epsc[:], EPS), w=["epsc"])
        P.dma("sp", gsb[:], gT, w=["gsb"])
        if msel_d is not None:
            msel = sb("msel_sb", [128, 2])
            P.dma("sp", msel[:], msel_d, w=["msel"])
        P.dma("pool", ow[:], outw, w=["ow"])
        mwb = [(y[:, 2 * i:2 * i + 2, :].rearrange("p a (b c) -> p (a b) c", c=128), ["y%d" % (2 * i), "y%d" % (2 * i + 1)]) for i in range(4)]
        modT = emit_modulation(P, nc, sb, pb[7], modw, modb, cT, 32, mwb)
        gbc = lambda i: gsb[:, i, :].unsqueeze(2).to_broadcast([128, 8, 2])
        P.dve(lambda e: e.tensor_tensor(out=A1[:], in0=modT[:, 0:8, :], in1=gbc(0), op=ALU.mult), r=["modT", "gsb"], w=["A1"])
        P.dve(lambda e: e.scalar_tensor_tensor(out=B2[:], in0=modT[:, 16:24, :], scalar=1.0, in1=gbc(1), op0=ALU.add, op1=ALU.mult),
              r=["modT", "gsb"], w=["B2"])
        P.dve(lambda e: e.tensor_tensor(out=A3[:], in0=modT[:, 24:32, :], in1=gbc(2), op=ALU.mult), r=["modT", "gsb"], w=["A3"])
        w1cnt = [0]; w2cnt = [0]

        def stats(n, srckey):
            for m in range(8):
                P.pe(lambda e, m=m: e.matmul(pb[6][:, :n], lhsT=onesb[:], rhs=sq[:, m, :n], start=(m == 0), stop=(m == 7)),
                     r=["onesb", "sq%d" % m], w=["pb6"])
            P.act(lambda e: e.activation(out=rstd[:, :n], in_=pb[6][:, :n], func=AF.Sqrt, bias=epsc[:, 0:1], scale=1.0 / D),
                  r=["pb6", "epsc"], w=["rstd"])
            P.dve(lambda e: e.reciprocal(out=rstd[:, :n], in_=rstd[:, :n]), r=["rstd"], w=["rstd"])

        def resid_add(n, col, A, Akey):
            for m in range(8):
                t = tmp[m % 2]; tk = "tmp%d" % (m % 2)
                P.dve(lambda e, m=m, t=t: e.scalar_tensor_tensor(out=t[:, :n], in0=y[:, m, :n], scalar=A[:, m, col:col + 1], in1=rstd[:, :n],
                                                                   op0=ALU.mult, op1=ALU.mult), r=["y%d" % m, Akey, "rstd"], w=[tk])
                P.dve(lambda e, m=m, t=t: e.tensor_tensor(out=xl[:, m, :n], in0=xl[:, m, :n], in1=t[:, :n], op=ALU.add),
                      r=[tk, "xl%d" % m], w=["xl%d" % m])

        def do_tile(t0, n, col):
            xs_ = xsrc(t0, n); ms_ = mixsrc(t0, n)
            xlk = ["xl%d" % m for m in range(8)]; yk = ["y%d" % m for m in range(8)]; hk16 = ["hid%d" % j for j in range(16)]
            P.dma("sp", xl[:, :, :n], xs_[0], w=xlk)
            P.dma("sp", mx[:, :, :n], ms_[0], w=["mx"])
            if len(xs_) == 2:
                P.dma("sp", y[:, :, :n], xs_[1], w=yk)
                P.dma("sp", hid[:, 0:16, :n], ms_[1], w=hk16)
                P.dve(lambda e: e.tensor_scalar(out=xl[:, :, :n], in0=xl[:, :, :n], scalar1=msel[:, 0:1], scalar2=None, op0=ALU.mult), r=xlk + ["msel"], w=xlk)
                P.dve(lambda e: e.scalar_tensor_tensor(out=xl[:, :, :n], in0=y[:, :, :n], scalar=msel[:, 1:2], in1=xl[:, :, :n], op0=ALU.mult, op1=ALU.add),
                      r=xlk + yk + ["msel"], w=xlk)
                P.dve(lambda e: e.tensor_scalar(out=mx[:, :, :n], in0=mx[:, :, :n], scalar1=msel[:, 0:1], scalar2=None, op0=ALU.mult), r=["mx", "msel"], w=["mx"])
                P.dve(lambda e: e.scalar_tensor_tensor(out=mx[:, :, :n], in0=hid[:, 0:16, :n], scalar=msel[:, 1:2], in1=mx[:, :, :n], op0=ALU.mult, op1=ALU.add),
                      r=["mx", "msel"] + hk16, w=["mx"])
            if moe:
                for m in range(8):
                    P.act(lambda e, m=m: e.activation(out=sq[:, m, :n], in_=mx[:, 8 + m, :n], func=AF.Square), r=["mx"], w=["sq%d" % m])
                stats(n, "ssd")
                for m in range(8):
                    P.dve(lambda e, m=m: e.scalar_tensor_tensor(out=mx[:, 8 + m, :n], in0=mx[:, 8 + m, :n], scalar=snw[:, m:m + 1], in1=rstd[:, :n],
                                                                 op0=ALU.mult, op1=ALU.mult), r=["mx", "snw", "rstd"], w=["mx"])
            for m in range(8):
                pbm = pb[m % 2]; pk = "pb%d" % (m % 2)
                for k in range(16):
                    P.pe(lambda e, m=m, k=k, pbm=pbm: e.matmul(pbm[:, :n], lhsT=ow[:, k, m * 128:(m + 1) * 128], rhs=mx[:, k, :n],
                                                               start=(k == 0), stop=(k == 15)), r=["ow", "mx"], w=[pk])
                P.act(lambda e, m=m, pbm=pbm: e.activation(out=y[:, m, :n], in_=pbm[:, :n], func=AF.Copy), r=[pk], w=["y%d" % m])
                P.act(lambda e, m=m, pbm=pbm: e.activation(out=sq[:, m, :n], in_=pbm[:, :n], func=AF.Square), r=[pk], w=["sq%d" % m])
            stats(n, "y")
            resid_add(n, col, A1, "A1")
            for m in range(8):
                P.act(lambda e, m=m: e.activation(out=sq[:, m, :n], in_=xl[:, m, :n], func=AF.Square), r=["xl%d" % m], w=["sq%d" % m])
            stats(n, "xl")
            for m in range(8):
                t = tmp[m % 2]; tk = "tmp%d" % (m % 2)
                P.dve(lambda e, m=m, t=t: e.scalar_tensor_tensor(out=t[:, :n], in0=xl[:, m, :n], scalar=B2[:, m, col:col + 1], in1=rstd[:, :n],
                                                                   op0=ALU.mult, op1=ALU.mult), r=["xl%d" % m, "B2", "rstd"], w=[tk])
                if moe:
                    P.act(lambda e, m=m, t=t: e.activation(out=h2f[:, m, :n], in_=t[:, :n], func=AF.Identity, bias=modT[:, 8 + m, col:col + 1], scale=1.0),
                          r=[tk, "modT"], w=["h2f%d" % m])
                    P.dve(lambda e, m=m: e.tensor_copy(out=h2[:, m, :n], in_=h2f[:, m, :n]), r=["h2f%d" % m], w=["h2_%d" % m])
                else:
                    P.act(lambda e, m=m, t=t: e.activation(out=h2[:, m, :n], in_=t[:, :n], func=AF.Identity, bias=modT[:, 8 + m, col:col + 1], scale=1.0),
                          r=[tk, "modT"], w=["h2_%d" % m])
            h2keys = ["h2_%d" % m for m in range(8)]
            if moe:
                for s4 in range(4):
                    for k in range(8):
                        P.pe(lambda e, s4=s4, k=k: e.matmul(pb[7][:, s4 * 8:(s4 + 1) * 8], lhsT=h2f[:, k, s4 * 128:(s4 + 1) * 128], rhs=rw[:, k, :],
                                                            start=(k == 0), stop=(k == 7)), r=["h2f%d" % k, "rw"], w=["pb7"])
                P.dve(lambda e: e.tensor_tensor(out=Lg[:], in0=pb[7][:, 0:32].rearrange("p (s e) -> p s e", e=8),
                                                in1=rb[:].unsqueeze(1).to_broadcast([128, 4, 8]), op=ALU.add), r=["pb7", "rb"], w=["Lg"])
                for s4 in range(4):
                    P.dve(lambda e, s4=s4: e.max(out=mx8[:, s4, :], in_=Lg[:, s4, :]), r=["Lg"], w=["mx8"])
                P.dve(lambda e: e.tensor_tensor(out=msk[:], in0=Lg[:], in1=mx8[:, :, 1:2].to_broadcast([128, 4, 8]), op=ALU.is_ge),
                      r=["Lg", "mx8"], w=["msk"])
                P.dve(lambda e: e.tensor_tensor(out=Eg[:], in0=Lg[:], in1=mx8[:, :, 0:1].to_broadcast([128, 4, 8]), op=ALU.subtract),
                      r=["Lg", "mx8"], w=["Eg"])
                P.act(lambda e: e.activation(out=Eg[:], in_=Eg[:], func=AF.Exp), r=["Eg"], w=["Eg"])
                P.dve(lambda e: e.tensor_tensor(out=Eg[:], in0=Eg[:], in1=msk[:], op=ALU.mult), r=["Eg", "msk"], w=["Eg"])
                P.dve(lambda e: e.tensor_reduce(out=den[:], in_=Eg[:], axis=AX.X, op=ALU.add), r=["Eg"], w=["den"])
                P.dve(lambda e: e.reciprocal(out=den[:], in_=den[:]), r=["den"], w=["den"])
                P.dve(lambda e: e.tensor_tensor(out=Eg[:], in0=Eg[:], in1=den[:].unsqueeze(2).to_broadcast([128, 4, 8]), op=ALU.mult),
                      r=["Eg", "den"], w=["Eg"])
                for s4 in range(4):
                    P.pe(lambda e, s4=s4: e.transpose(pb[6][0:8, s4 * 128:(s4 + 1) * 128], Eg[:, s4, :], ident[:]), r=["Eg", "ident"], w=["pb6"])
                P.dve(lambda e: e.tensor_copy(out=gTs[:], in_=pb[6][0:8, :]), r=["pb6"], w=["gTs"])
                for ex in range(8):
                    pbm = pb[ex % 2]; pk = "pb%d" % (ex % 2)
                    P.pe(lambda e, ex=ex, pbm=pbm: e.matmul(pbm[:, :n], lhsT=sel[0:8, ex * 128:(ex + 1) * 128], rhs=gTs[0:8, :n], start=True, stop=True),
                         r=["sel", "gTs"], w=[pk])
                    P.act(lambda e, ex=ex, pbm=pbm: e.activation(out=Gb[:, ex, :n], in_=pbm[:, :n], func=AF.Copy), r=[pk], w=["Gb%d" % ex])
            for ex in range(NE):
                for j in range(NJ):
                    s = w1cnt[0] % NW1; w1cnt[0] += 1
                    P.dma("pool", w1b[s], w1[ex * NJ + j], w=w1k[s])
                    for half in range(2):
                        pbi = 2 + 2 * (j % 2) + half
                        for k in range(8):
                            P.pe(lambda e, s=s, k=k, half=half, pbi=pbi: e.matmul(pb[pbi][:, :n], lhsT=w1b[s][:, k, half * 128:(half + 1) * 128],
                                                                                   rhs=h2[:, k, :n], start=(k == 0), stop=(k == 7)),
                                 r=w1k[s] + ["h2_%d" % k], w=["pb%d" % pbi])
                    pg = 2 + 2 * (j % 2)
                    P.act(lambda e, j=j, pg=pg: e.activation(out=sg[j % 2][:, :n], in_=pb[pg][:, :n], func=AF.Silu), r=["pb%d" % pg], w=["sg%d" % (j % 2)])
                    P.dve(lambda e, j=j, pg=pg: e.tensor_tensor(out=hid[:, j, :n], in0=sg[j % 2][:, :n], in1=pb[pg + 1][:, :n], op=ALU.mult),
                          r=["sg%d" % (j % 2), "pb%d" % (pg + 1)], w=["hid%d" % j])
                for m in range(8):
                    s = w2cnt[0] % NW2; w2cnt[0] += 1
                    P.dma("pool", w2b[s], w2[ex * 8 + m], w=w2k[s])
                    pbm = pb[m % 2]; pk = "pb%d" % (m % 2)
                    for j in range(NJ):
                        P.pe(lambda e, s=s, j=j, pbm=pbm: e.matmul(pbm[:, :n], lhsT=w2b[s][:, j, :], rhs=hid[:, j, :n], start=(j == 0), stop=(j == NJ - 1)),
                             r=w2k[s] + ["hid%d" % j], w=[pk])
                    if not moe:
                        P.act(lambda e, m=m, pbm=pbm: e.activation(out=y[:, m, :n], in_=pbm[:, :n], func=AF.Copy), r=[pk], w=["y%d" % m])
                        P.act(lambda e, m=m, pbm=pbm: e.activation(out=sq[:, m, :n], in_=pbm[:, :n], func=AF.Square), r=[pk], w=["sq%d" % m])
                    elif ex == 0:
                        P.dve(lambda e, m=m, pbm=pbm, ex=ex: e.tensor_tensor(out=y[:, m, :n], in0=pbm[:, :n], in1=Gb[:, ex, :n], op=ALU.mult),
                              r=[pk, "Gb%d" % ex], w=["y%d" % m])
                    else:
                        t = tmp[m % 2]; tk = "tmp%d" % (m % 2)
                        P.dve(lambda e, m=m, pbm=pbm, ex=ex, t=t: e.tensor_tensor(out=t[:, :n], in0=pbm[:, :n], in1=Gb[:, ex, :n], op=ALU.mult),
                              r=[pk, "Gb%d" % ex], w=[tk])
                        P.dve(lambda e, m=m, t=t: e.tensor_tensor(out=y[:, m, :n], in0=y[:, m, :n], in1=t[:, :n], op=ALU.add),
                              r=[tk, "y%d" % m], w=["y%d" % m])
                        if ex == NE - 1:
                            P.act(lambda e, m=m: e.activation(out=sq[:, m, :n], in_=y[:, m, :n], func=AF.Square), r=["y%d" % m], w=["sq%d" % m])
            stats(n, "f")
            resid_add(n, col, A3, "A3")
            P.dma("sp", dst(t0, n), xl[:, :, :n], r=["xl%d" % m for m in range(8)], final=True)
        for (t0_, n_, col_) in tiles:
            do_tile(t0_, n_, col_)
        P.emit()


def fm(a):
    T, C = a.shape
    return np.ascontiguousarray(a.T.reshape(C // 128, 128, T).transpose(1, 0, 2))


def fm_inv(b):
    p, kc, T = b.shape
    return np.ascontiguousarray(b.transpose(2, 1, 0).reshape(T, kc * 128))


def kblk(w):
    K, N = w.shape
    return np.ascontiguousarray(w.reshape(K // 128, 128, N).transpose(1, 0, 2))


def w1blk(w, nj):
    return np.ascontiguousarray(w.reshape(8, 128, 2, nj, 128).transpose(3, 1, 0, 2, 4).reshape(nj, 128, 8, 256))


def w2blk(w, nj):
    return np.ascontiguousarray(w.reshape(nj, 128, 8, 128).transpose(2, 1, 0, 3))


def modblk(w, c0, c1):
    n = (c1 - c0) // 128
    return np.ascontiguousarray(w[:, c0:c1].reshape(8, 128, n, 128).transpose(2, 1, 0, 3))


def colvec(v):
    return np.ascontiguousarray(v.reshape(-1, 128).T)


def post_consts(layer, I):
    moe = layer == 1
    d = {}
    if moe:
        d["outw"] = kblk(I["cd_out_w"][0])
        d["w1"] = np.concatenate([w1blk(I["moe_w1"][0, e], 28) for e in range(8)], axis=0)
        d["w2"] = np.concatenate([w2blk(I["moe_w2"][0, e], 28) for e in range(8)], axis=0)
        d["routw"] = kblk(I["router_w"][0])
        d["routb"] = np.ascontiguousarray(np.broadcast_to(I["router_b"][0][None, :], (128, 8)))
        d["ident"] = np.eye(128, dtype=np.float32)
        sel = np.zeros((8, 1024), np.float32)
        for e in range(8):
            sel[e, e * 128:(e + 1) * 128] = 1.0
        d["sel"] = sel
        d["snw"] = colvec(I["s_norm_w"][0])
    else:
        d["outw"] = kblk(I["ab_out_w"][0])
        d["w1"] = w1blk(I["ffn_w1"][0], 22)
        d["w2"] = w2blk(I["ffn_w2"][0], 22)
    d["modw"] = modblk(I["mod_w"][layer], 2048, 6144)
    d["modb"] = colvec(I["mod_b"][layer][2048:6144])
    d["gT"] = np.ascontiguousarray(I["norm_g"][layer][1:4].reshape(3, 8, 128).transpose(2, 0, 1))
    return d


def c_cols(I, b):
    return np.ascontiguousarray(np.stack([I["c"][b], I["c_ctx"]], 0).reshape(2, 8, 128).transpose(2, 1, 0))


T_ALL = 4352
NCH = 34
ORDER_F = list(range(NCH))
ORDER_B = [1, 0] + list(range(NCH - 1, 1, -1))
TOK_TILES = [(0, 256, 1)] + [(256 + 512 * i, 512, 0) for i in range(8)]
LN_DK = float(np.log(128.0 ** -0.5))


def emit_adaln_in(P, nc, sb, pb, xT, modT, g0sb, hT, xin, xinkey, sq, sqkey, tmp, rstd, onesb, epsc, A0):
    P.dve(lambda e: e.scalar_tensor_tensor(out=A0[:], in0=modT[:, 8:16, :], scalar=1.0, in1=g0sb[:].unsqueeze(2).to_broadcast([128, 8, 2]),
                                           op0=ALU.add, op1=ALU.mult), r=["modT", "g0sb"], w=["A0"])

    def tile(t0, n, col):
        P.dma("sp", xin[:, :, :n], xT[:, :, t0:t0 + n], w=[xinkey])
        for k in range(8):
            P.act(lambda e, k=k: e.activation(out=sq[:, k, :n], in_=xin[:, k, :n], func=AF.Square), r=[xinkey], w=[sqkey + str(k)])
        for k in range(8):
            P.pe(lambda e, k=k: e.matmul(pb[6][:, :n], lhsT=onesb[:], rhs=sq[:, k, :n], start=(k == 0), stop=(k == 7)),
                 r=["onesb", sqkey + str(k)], w=["pb6"])
        P.act(lambda e: e.activation(out=rstd[:, :n], in_=pb[6][:, :n], func=AF.Sqrt, bias=epsc[:, 0:1], scale=1.0 / D), r=["pb6", "epsc"], w=["rstd"])
        P.dve(lambda e: e.reciprocal(out=rstd[:, :n], in_=rstd[:, :n]), r=["rstd"], w=["rstd"])
        for k in range(8):
            t = tmp[k % 2]; tk = "tmp%d" % (k % 2)
            P.dve(lambda e, k=k, t=t: e.scalar_tensor_tensor(out=t[:, :n], in0=xin[:, k, :n], scalar=A0[:, k, col:col + 1], in1=rstd[:, :n],
                                                               op0=ALU.mult, op1=ALU.mult), r=[xinkey, "A0", "rstd"], w=[tk])
            P.act(lambda e, k=k, t=t: e.activation(out=hT[:, k, t0:t0 + n], in_=t[:, :n], func=AF.Identity, bias=modT[:, k, col:col + 1], scale=1.0),
                  r=[tk, "modT"], w=["hT"])
    for (t0, n, col) in TOK_TILES:
        tile(t0, n, col)


def emit_conv(P, src, skey, dst, dkey, wcol, bcol, wkeys):
    P.dve(lambda e: e.tensor_scalar(out=dst[:, :], in0=src[:, :], scalar1=wcol[:, 2:3], scalar2=bcol, op0=ALU.mult, op1=ALU.add),
          r=[skey] + wkeys, w=[dkey])
    views = [(lambda a: a[:, 0:256].rearrange("p (r w) -> p r w", w=256), 256),
             (lambda a: a[:, 256:T_ALL].rearrange("p (r w) -> p r w", w=64), 64)]
    for (j, d) in [(0, -2), (1, -1), (3, 1)]:
        for vf, W in views:
            sv = vf(src); dv = vf(dst)
            if d < 0:
                o_ = dv[:, :, -d:W]; i_ = sv[:, :, 0:W + d]
            else:
                o_ = dv[:, :, 0:W - d]; i_ = sv[:, :, d:W]
            P.dve(lambda e, o_=o_, i_=i_, j=j: e.scalar_tensor_tensor(out=o_, in0=i_, scalar=wcol[:, j:j + 1], in1=o_, op0=ALU.mult, op1=ALU.add),
                  r=[skey, dkey] + wkeys, w=[dkey])


SEG_SPLIT = 2304
SEGS = [(0, SEG_SPLIT), (SEG_SPLIT, T_ALL)]


def sk(key, t0):
    return key + ("/0" if t0 < SEG_SPLIT else "/1")


def emit_conv_seg(P, src, skey, dst, dkey, wcol, bcol, wkeys, seg):
    a, b = SEGS[seg]
    sk_, dk_ = skey + "/%d" % seg, dkey + "/%d" % seg
    P.dve(lambda e: e.tensor_scalar(out=dst[:, a:b], in0=src[:, a:b], scalar1=wcol[:, 2:3], scalar2=bcol, op0=ALU.mult, op1=ALU.add),
          r=[sk_] + wkeys, w=[dk_])
    if seg == 0:
        views = [(lambda t: t[:, 0:256].rearrange("p (r w) -> p r w", w=256), 256),
                 (lambda t: t[:, 256:SEG_SPLIT].rearrange("p (r w) -> p r w", w=64), 64)]
    else:
        views = [(lambda t: t[:, SEG_SPLIT:T_ALL].rearrange("p (r w) -> p r w", w=64), 64)]
    for (j, d) in [(0, -2), (1, -1), (3, 1)]:
        for vf, W in views:
            sv = vf(src); dv = vf(dst)
            if d < 0:
                o_ = dv[:, :, -d:W]; i_ = sv[:, :, 0:W + d]
            else:
                o_ = dv[:, :, 0:W - d]; i_ = sv[:, :, d:W]
            P.dve(lambda e, o_=o_, i_=i_, j=j: e.scalar_tensor_tensor(out=o_, in0=i_, scalar=wcol[:, j:j + 1], in1=o_, op0=ALU.mult, op1=ALU.add),
                  r=[sk_, dk_] + wkeys, w=[dk_])


def phase_mix0(nc, P, pb, drs, xT, out_mix):
    dr = drs[0]
    modw = dr("modw", [16, 128, 8, 128])
    modb = dr("modb", [128, 16])
    cT = dr("cT", [128, 8, 2])
    g0 = dr("g0", [128, 8])
    triUd = dr("triU", [128, 128])
    triLd = dr("triL", [128, 128])
    identd = dr("identb", [128, 128], BF16)
    with ExitStack() as es:
        sb, ps = _mk(nc, es)
        obT = sb("obT", [128, 512], BF16)
        hT = sb("hT", [128, 8, T_ALL], BF16)
        G = [sb("G%d" % i, [128, T_ALL]) for i in range(4)]
        QK = sb("QK", [128, T_ALL]); XB = sb("XB", [128, T_ALL], BF16)
        qkb = QK[:].bitcast(BF16)
        qT = qkb[:, 0:T_ALL]; kT = qkb[:, T_ALL:2 * T_ALL]
        VT = G[1][:].bitcast(BF16)
        vtok = VT.rearrange("p (c v) -> p c v", v=256)
        ktok = G[0][:].bitcast(BF16)[:, 0:T_ALL].rearrange("p (c d) -> p c d", d=128)
        tmp = [sb("tmp%d" % i, [128, 512]) for i in range(2)]
        rstd = sb("rstd", [128, 512]); onesb = sb("onesb", [128, 128], BF16); onesf = sb("onesf", [128, 128]); epsc = sb("epsc", [128, 1])
        g0sb = sb("g0sb", [128, 8]); A0 = sb("A0", [128, 8, 2])
        triU = sb("triU_sb", [128, 128]); triL = sb("triL_sb", [128, 128]); identb = sb("identb_sb", [128, 128], BF16)
        wfb = [sb("wfb%d" % i, [128, 8, 128], BF16) for i in range(4)]
        wvb = sb("wvb", [128, 8, 256], BF16); wgb = sb("wgb", [128, 8, 8], BF16)
        gbs = sb("gbs", [128, 8]); cws = sb("cws", [128, 8, 4]); cbs = sb("cbs", [128, 8]); mns = sb("mns", [128, 512])
        lwab = sb("lwab", [128, 2, 4, 128], BF16); lwxb = sb("lwxb", [128, 2, 4, 128], BF16)
        lbas = sb("lbas", [128, 2, 4]); lbxs = sb("lbxs", [128, 2, 4]); lams = sb("lams", [128, 2, 4]); LC = sb("LC", [128, 2, 4])
        GA = sb("GA", [128, NCH, 8]); Lsp = sb("Lsp", [128, NCH, 4]); EX = sb("EX", [128, 4, NCH, 2]); WV = sb("WV", [128, 2, NCH, 2])
        Sacc = [sb("Sacc%d" % i, [128, 257]) for i in range(2)]; Sbf = [sb("Sbf%d" % i, [128, 257], BF16) for i in range(2)]
        PT = [sb("PT%d" % i, [128, 128], BF16) for i in range(2)]; vs = [sb("vs%d" % i, [128, 257], BF16) for i in range(4)]
        dn = [sb("dn%d" % i, [128, 4]) for i in range(2)]
        ssq = sb("ssq", [128, NCH]); osig = tmp; obf = [sb("obf%d" % i, [128, 512], BF16) for i in range(2)]
        hlb = obf
        for (dst, src, key) in [(g0sb, g0, "g0sb"), (triU, triUd, "triU"), (triL, triLd, "triL"), (identb, identd, "identb")]:
            P.dma("sp", dst[:], src, w=[key])
        P.dve(lambda e: e.memset(onesb[:], 1.0), w=["onesb"])
        P.dve(lambda e: e.memset(onesf[:], 1.0), w=["onesf"])
        P.dve(lambda e: e.memset(epsc[:], EPS), w=["epsc"])
        mwb = [(G[1][:, 1024 * i:1024 * (i + 1)].rearrange("p (a b) -> p a b", b=128), ["mwst%d" % i]) for i in range(4)]
        modT = emit_modulation(P, nc, sb, pb[7], modw, modb, cT, 16, mwb)
        xin = G[0][:, 0:4096].rearrange("p (k n) -> p k n", k=8)
        sqv = XB[:, 0:4096].rearrange("p (k n) -> p k n", k=8)
        emit_adaln_in(P, nc, sb, pb, xT, modT, g0sb, hT, xin, "G0", sqv, "XB", tmp, rstd, onesb, epsc, A0)
        def half(s, dr):
            wfm = dr("wfm", [12, 128, 8, 128])
            wv = dr("wv", [2, 128, 8, 256])
            wo = dr("wo", [128, 8, 512])
            wg = dr("wg", [128, 8, 8])
            gbias = dr("gbias", [128, 8])
            cw = dr("cw", [128, 8, 4])
            cb = dr("cb", [128, 8])
            mnorm = dr("mnorm", [128, 512])
            lwa = dr("lwa", [128, 2, 4, 128])
            lwx = dr("lwx", [128, 2, 4, 128])
            lba = dr("lba", [128, 2, 4])
            lbx = dr("lbx", [128, 2, 4])
            llam = dr("llam", [128, 2, 4])
            for (dst, src, key) in [(gbs, gbias, "gbs"), (cws, cw, "cws"), (cbs, cb, "cbs"), (mns, mnorm, "mns"), (lbas, lba, "lbas"), (lbxs, lbx, "lbxs"), (lams, llam, "lams")]:
                P.dma("sp", dst[:], src, w=[key])
            for (dst, src, key) in [(wgb, wg, "wgb"), (lwab, lwa, "lwab"), (lwxb, lwx, "lwxb")]:
                P.dma("pool", dst[:], src, w=[key])
            P.act(lambda e: e.activation(out=LC[:], in_=lams[:], func=AF.Exp, scale=-1.0), r=["lams"], w=["LC"])
            P.act(lambda e: e.activation(out=LC[:], in_=LC[:], func=AF.Ln, bias=1.0), r=["LC"], w=["LC"])
            P.dve(lambda e: e.tensor_scalar(out=LC[:], in0=LC[:], scalar1=-8.0, scalar2=None, op0=ALU.mult), r=["LC"], w=["LC"])
            wfcnt = [0]

            def proj_fm(blk, dst, dkey, evac=None, pbo=0):
                s = wfcnt[0] % 4; wfcnt[0] += 1
                P.dma("pool", wfb[s][:], wfm[blk], w=["wfb%d" % s])
                for ti, (t0, n, col) in enumerate(TOK_TILES):
                    pbi = pbo + ti % 2
                    for k in range(8):
                        P.pe(lambda e, k=k, s=s, t0=t0, n=n, pbi=pbi: e.matmul(pb[pbi][:, :n], lhsT=wfb[s][:, k, :], rhs=hT[:, k, t0:t0 + n],
                                                                              start=(k == 0), stop=(k == 7)), r=["wfb%d" % s, "hT"], w=["pb%d" % pbi])
                    if evac is None:
                        P.act(lambda e, t0=t0, n=n, pbi=pbi: e.activation(out=dst[:, t0:t0 + n], in_=pb[pbi][:, :n], func=AF.Copy), r=["pb%d" % pbi], w=[sk(dkey, t0)])
                    else:
                        evac(ti, t0, n, pbi)

            for c in range(NCH):
                for k in range(8):
                    P.pe(lambda e, c=c, k=k: e.matmul(pb[7][:, c * 8:(c + 1) * 8], lhsT=hT[:, k, c * 128:(c + 1) * 128], rhs=wgb[:, k, :],
                                                      start=(k == 0), stop=(k == 7)), r=["hT", "wgb"], w=["pb7"])
            P.dve(lambda e: e.tensor_tensor(out=GA[:], in0=pb[7][:, 0:NCH * 8].rearrange("p (c g) -> p c g", g=8),
                                            in1=gbs[:].unsqueeze(1).to_broadcast([128, NCH, 8]), op=ALU.add), r=["pb7", "gbs"], w=["GA"])
            P.act(lambda e: e.activation(out=Lsp[:], in_=GA[:, :, 4:8], func=AF.Exp, scale=-1.0), r=["GA"], w=["Lsp"])
            P.act(lambda e: e.activation(out=Lsp[:], in_=Lsp[:], func=AF.Ln, bias=1.0), r=["Lsp"], w=["Lsp"])
            NG = NCH * 2
            for gi, (lh, sl) in enumerate([(triU, slice(0, 2)), (triL, slice(2, 4)), (onesf, slice(0, 2)), (onesf, slice(2, 4))]):
                P.pe(lambda e, gi=gi, lh=lh, sl=sl: e.matmul(pb[6][:, gi * NG:(gi + 1) * NG], lhsT=lh[:], rhs=Lsp[:, :, sl], start=True, stop=True),
                     r=["triU", "triL", "onesf", "Lsp"], w=["pb6"])
            P.act(lambda e: e.activation(out=EX[:].rearrange("p a c h -> p (a c h)"), in_=pb[6][:, 0:4 * NG], func=AF.Exp, scale=-1.0), r=["pb6"], w=["EX"])
            for d in range(2):
                P.dve(lambda e, d=d: e.tensor_tensor(out=WV[:, d], in0=GA[:, :, 2 * d:2 * d + 2],
                                                     in1=pb[6][:, d * NG:(d + 1) * NG].rearrange("p (c h) -> p c h", h=2), op=ALU.add), r=["GA", "pb6", "EX"], w=["WV"])
            lnc = sb("lnc%d" % s, [128, 1])
            P.dve(lambda e: e.memset(lnc[:], LN_DK), w=["lnc"])
            P.act(lambda e: e.activation(out=WV[:], in_=WV[:], func=AF.Exp, bias=lnc[:, 0:1], scale=1.0), r=["WV", "lnc"], w=["WV"])
            masks = [triU, triL]; orders = [ORDER_F, ORDER_B]
            Hm = [G[2], G[3]]

            def Hv(c):
                g = Hm[c // 17]; cc = c % 17
                return g[:, cc * 256:(cc + 1) * 256]

            for hh in range(2):
                caps = []
                for (blk, dstb, ga, gb, pbo) in [(2 * hh, qT, 0, 1, 0), (2 * hh + 1, kT, 2, 3, 2)]:
                    with P.capture() as cap:
                        proj_fm(blk, G[ga], "G%d" % ga, pbo=pbo)
                        emit_conv(P, G[ga], "G%d" % ga, G[gb], "G%d" % gb, cws[:, blk, :], cbs[:, blk:blk + 1], ["cws", "cbs"])
                        P.act(lambda e, dstb=dstb, gb=gb: e.activation(out=dstb, in_=G[gb][:, :], func=AF.Silu), r=["G%d" % gb], w=["QK"])
                    caps.append(cap)
                P.add_interleaved(caps)
                P.dma("pool", wvb[:], wv[hh], w=["wvb"])
                for c2 in range(NCH // 2):
                    pbi = c2 % 2
                    for cc in range(2):
                        c = 2 * c2 + cc
                        for k in range(8):
                            P.pe(lambda e, c=c, cc=cc, k=k, pbi=pbi: e.matmul(pb[pbi][:, cc * 256:(cc + 1) * 256], lhsT=hT[:, k, c * 128:(c + 1) * 128], rhs=wvb[:, k, :],
                                                                              start=(k == 0), stop=(k == 7)), r=["hT", "wvb"], w=["pb%d" % pbi])
                    P.act(lambda e, c2=c2, pbi=pbi: e.activation(out=VT[:, c2 * 512:(c2 + 1) * 512], in_=pb[pbi][:, :], func=AF.Copy), r=["pb%d" % pbi], w=["G1"])
                pT = pb[7][:].bitcast(BF16)
                for c4 in range(0, NCH, 4):
                    nn = min(4, NCH - c4)
                    for cc in range(nn):
                        c = c4 + cc
                        P.pe(lambda e, c=c, cc=cc: e.transpose(pT[:, cc * 128:(cc + 1) * 128], kT[:, c * 128:(c + 1) * 128], identb[:]), r=["QK", "identb"], w=["pb7"])
                    P.act(lambda e, c4=c4, nn=nn: e.activation(out=ktok[:, c4:c4 + nn, :].rearrange("p c d -> p (c d)"), in_=pT[:, 0:nn * 128], func=AF.Copy),
                          r=["pb7"], w=["G0"])
                P.dve(lambda e: e.memset(G[2][:, :], 0.0), w=["G2"])
                P.dve(lambda e: e.memset(G[3][:, :], 0.0), w=["G3"])
                vcnt = 0
                for i in range(NCH):
                    caps = []
                    for d in range(2):
                        with P.capture() as cap:
                            c = orders[d][i]; cprev = orders[d][i - 1] if i > 0 else None
                            qc = qT[:, c * 128:(c + 1) * 128]; kc = kT[:, c * 128:(c + 1) * 128]
                            pS = pb[0 + d]; pO = pb[2 + d]; pD = pb[4 + d]
                            kS, kO, kD = "pb%d" % d, "pb%d" % (2 + d), "pb%d" % (4 + d)
                            v = vs[vcnt % 4]; vk = "vs%d" % (vcnt % 4); vcnt += 1
                            wcol = WV[:, d, c, hh:hh + 1]
                            hk = "G%d" % (2 + c // 17)
                            P.pe(lambda e, pS=pS, kc=kc, qc=qc: e.matmul(pS[:, 0:128], lhsT=kc, rhs=qc, start=True, stop=True), r=["QK"], w=[kS])
                            P.dve(lambda e, d=d, pS=pS: e.tensor_tensor(out=PT[d][:], in0=pS[:, 0:128], in1=masks[d][:], op=ALU.mult),
                                  r=[kS, "triU", "triL"], w=["PT%d" % d])
                            P.act(lambda e, v=v, c=c, wcol=wcol: e.activation(out=v[:, 0:256], in_=vtok[:, c, :], func=AF.Identity, scale=wcol),
                                  r=["G1", "WV"], w=[vk])
                            P.act(lambda e, v=v, wcol=wcol: e.activation(out=v[:, 256:257], in_=wcol, func=AF.Identity), r=["WV"], w=[vk])
                            P.pe(lambda e, d=d, pO=pO, v=v, i=i: e.matmul(pO[:, 0:257], lhsT=PT[d][:], rhs=v[:], start=True, stop=(i == 0)), r=["PT%d" % d, vk], w=[kO])
                            if i > 0:
                                P.pe(lambda e, d=d, pO=pO, qc=qc: e.matmul(pO[:, 0:257], lhsT=qc, rhs=Sbf[d][:], start=False, stop=True), r=["QK", "Sbf%d" % d], w=[kO])
                            ebc = EX[:, d, c, hh:hh + 1]
                            dd = dn[d]; dk_ = "dn%d" % d
                            P.dve(lambda e, dd=dd, pO=pO: e.tensor_copy(out=dd[:, 1:2], in_=pO[:, 256:257]), r=[kO], w=[dk_])
                            P.dve(lambda e, dd=dd: e.scalar_tensor_tensor(out=dd[:, 0:1], in0=dd[:, 1:2], scalar=-1.0, in1=dd[:, 1:2], op0=ALU.mult, op1=ALU.max), r=[dk_], w=[dk_])
                            P.dve(lambda e, dd=dd: e.reciprocal(out=dd[:, 2:3], in_=dd[:, 0:1]), r=[dk_], w=[dk_])
                            P.dve(lambda e, dd=dd, ebc=ebc: e.tensor_tensor(out=dd[:, 3:4], in0=dd[:, 2:3], in1=ebc, op=ALU.min), r=[dk_, "EX"], w=[dk_])
                            P.dve(lambda e, dd=dd, pO=pO, c=c: e.scalar_tensor_tensor(out=Hv(c), in0=pO[:, 0:256], scalar=dd[:, 3:4], in1=Hv(c), op0=ALU.mult, op1=ALU.add),
                                  r=[kO, dk_, hk], w=[hk])
                            if i < NCH - 1:
                                P.pe(lambda e, pD=pD, c=c, v=v: e.matmul(pD[:, 0:257], lhsT=ktok[:, c, :], rhs=v[:], start=True, stop=True), r=["G0", vk], w=[kD])
                                if i == 0:
                                    P.dve(lambda e, d=d, pD=pD: e.tensor_copy(out=Sacc[d][:], in_=pD[:, 0:257]), r=[kD], w=["Sacc%d" % d])
                                else:
                                    eprev = EX[:, 2 + d, cprev, hh:hh + 1]
                                    P.dve(lambda e, d=d, pD=pD, eprev=eprev: e.scalar_tensor_tensor(out=Sacc[d][:], in0=Sacc[d][:], scalar=eprev, in1=pD[:, 0:257],
                                                                                                    op0=ALU.mult, op1=ALU.add), r=[kD, "EX", "Sacc%d" % d], w=["Sacc%d" % d])
                                ecur = EX[:, 2 + d, c, hh:hh + 1]
                                P.act(lambda e, d=d, ecur=ecur: e.activation(out=Sbf[d][:], in_=Sacc[d][:], func=AF.Identity, scale=ecur), r=["Sacc%d" % d, "EX"], w=["Sbf%d" % d])
                            emit_pe_warm(P, pb[7], onesb, hT, 3)
                        caps.append(cap)
                    P.add_interleaved(caps)
                Hall = [g[:, 0:17 * 256].rearrange("p (c v) -> p c v", v=256) for g in Hm]
                for gi in range(2):
                    P.dve(lambda e, gi=gi: e.tensor_tensor(out=G[gi][:, 0:17 * 256], in0=Hm[gi][:, 0:17 * 256], in1=Hm[gi][:, 0:17 * 256], op=ALU.mult),
                          r=["G%d" % (2 + gi)], w=["G%d" % gi])
                    P.dve(lambda e, gi=gi: e.tensor_reduce(out=ssq[:, gi * 17:(gi + 1) * 17], in_=G[gi][:, 0:17 * 256].rearrange("p (c v) -> p c v", v=256), axis=AX.X, op=ALU.add),
                          r=["G%d" % gi], w=["ssq"])
                P.act(lambda e: e.activation(out=ssq[:], in_=ssq[:], func=AF.Sqrt, bias=epsc[:, 0:1], scale=1.0 / 256), r=["ssq", "epsc"], w=["ssq"])
                P.dve(lambda e: e.reciprocal(out=ssq[:], in_=ssq[:]), r=["ssq"], w=["ssq"])
                for gi in range(2):
                    P.dve(lambda e, gi=gi: e.tensor_tensor(out=Hall[gi], in0=Hall[gi], in1=ssq[:, gi * 17:(gi + 1) * 17].unsqueeze(2).to_broadcast([128, 17, 256]), op=ALU.mult),
                          r=["G%d" % (2 + gi), "ssq"], w=["G%d" % (2 + gi)])
                    P.dve(lambda e, gi=gi, hh=hh: e.tensor_tensor(out=Hall[gi], in0=Hall[gi], in1=mns[:, hh * 256:(hh + 1) * 256].unsqueeze(1).to_broadcast([128, 17, 256]), op=ALU.mult),
                          r=["G%d" % (2 + gi), "mns"], w=["G%d" % (2 + gi)])
                P.dma("pool", wvb[:], wo[:, :, hh * 256:(hh + 1) * 256], w=["wvb"])
                for c2 in range(NCH // 2):
                    pbi = c2 % 2; ob = obf[c2 % 2]; og = osig[c2 % 2]
                    for cc in range(2):
                        c = 2 * c2 + cc
                        for k in range(8):
                            P.pe(lambda e, c=c, cc=cc, k=k, pbi=pbi, hh=hh: e.matmul(pb[pbi][:, cc * 256:(cc + 1) * 256], lhsT=hT[:, k, c * 128:(c + 1) * 128],
                                                                                     rhs=wvb[:, k, :], start=(k == 0), stop=(k == 7)), r=["hT", "wvb"], w=["pb%d" % pbi])
                    P.act(lambda e, pbi=pbi, og=og: e.activation(out=og[:], in_=pb[pbi][:, :], func=AF.Sigmoid), r=["pb%d" % pbi], w=["tmp%d" % (c2 % 2)])
                    c0 = 2 * c2
                    hsrc = Hm[c0 // 17][:, (c0 % 17) * 256:(c0 % 17) * 256 + 512] if (c0 % 17) != 16 else None
                    if hsrc is not None:
                        P.dve(lambda e, ob=ob, og=og, hsrc=hsrc: e.tensor_tensor(out=ob[:], in0=og[:], in1=hsrc, op=ALU.mult),
                              r=["tmp%d" % (c2 % 2), "G%d" % (2 + c0 // 17)], w=["obf%d" % (c2 % 2)])
                    else:
                        for cc in range(2):
                            P.dve(lambda e, ob=ob, og=og, cc=cc, c0=c0: e.tensor_tensor(out=ob[:, cc * 256:(cc + 1) * 256], in0=og[:, cc * 256:(cc + 1) * 256], in1=Hv(c0 + cc), op=ALU.mult),
                                  r=["tmp%d" % (c2 % 2), "G2", "G3"], w=["obf%d" % (c2 % 2)])
                    ch0 = (2 * s + hh) * 2
                    emit_tok2fm(P, pb[7], identb, ob, "obf%d" % (c2 % 2), obT, "obT", out_mix[:, ch0:ch0 + 2, c0 * 128:(c0 + 2) * 128])
            xlb = XB[:, :]
            G4 = QK
            seg_tiles = [[(ti, t) for ti, t in enumerate(TOK_TILES) if t[0] < SEG_SPLIT], [(ti, t) for ti, t in enumerate(TOK_TILES) if t[0] >= SEG_SPLIT]]
            for nb in range(4):
                proj_fm(4 + nb, G[0], "G0")
                for sg_ in range(2):
                    a_, b_ = SEGS[sg_]
                    emit_conv_seg(P, G[0], "G0", G[1], "G1", cws[:, 4 + nb, :], cbs[:, 4 + nb:5 + nb], ["cws", "cbs"], sg_)
                    P.act(lambda e, a_=a_, b_=b_: e.activation(out=xlb[:, a_:b_], in_=G[1][:, a_:b_], func=AF.Copy), r=["G1/%d" % sg_],
                          w=["XB/%d" % sg_] + ["XB%d" % k for k in range(8)])
                for d in range(2):
                    for sg_ in range(2):
                        a_, b_ = SEGS[sg_]
                        for (wsb, bsb, dst, dkey, wkey, bkey) in [(lwab, lbas, G[2], "G2", "lwab", "lbas"), (lwxb, lbxs, G[3], "G3", "lwxb", "lbxs")]:
                            for ti, (t0, n, col) in seg_tiles[sg_]:
                                pbi = ti % 2
                                P.pe(lambda e, wsb=wsb, d=d, nb=nb, t0=t0, n=n, pbi=pbi: e.matmul(pb[pbi][:, :n], lhsT=wsb[:, d, nb, :], rhs=xlb[:, t0:t0 + n], start=True, stop=True),
                                     r=[wkey, "XB/%d" % sg_], w=["pb%d" % pbi])
                                P.act(lambda e, bsb=bsb, dst=dst, d=d, nb=nb, t0=t0, n=n, pbi=pbi: e.activation(out=dst[:, t0:t0 + n], in_=pb[pbi][:, :n], func=AF.Sigmoid,
                                                                                                          bias=bsb[:, d, nb:nb + 1], scale=1.0), r=["pb%d" % pbi, bkey], w=["%s/%d" % (dkey, sg_)])
                        P.act(lambda e, d=d, nb=nb, a_=a_, b_=b_: e.activation(out=G[2][:, a_:b_], in_=G[2][:, a_:b_], func=AF.Exp, scale=LC[:, d, nb:nb + 1]),
                              r=["G2/%d" % sg_, "LC"], w=["G2/%d" % sg_])
                    for sg_ in range(2):
                        a_, b_ = SEGS[sg_]
                        K2, K3, K0, K1 = "G2/%d" % sg_, "G3/%d" % sg_, "G0/%d" % sg_, "G1/%d" % sg_
                        P.dve(lambda e, a_=a_, b_=b_: e.tensor_tensor(out=G[3][:, a_:b_], in0=G[3][:, a_:b_], in1=G[1][:, a_:b_], op=ALU.mult), r=[K3, K1], w=[K3])
                        P.dve(lambda e, a_=a_, b_=b_: e.tensor_tensor(out=G[0][:, a_:b_], in0=G[2][:, a_:b_], in1=G[2][:, a_:b_], op=ALU.mult), r=[K2], w=[K0])
                        P.act(lambda e, a_=a_, b_=b_: e.activation(out=G[0][:, a_:b_], in_=G[0][:, a_:b_], func=AF.Sqrt, scale=-1.0, bias=1.0), r=[K0], w=[K0])
                        P.dve(lambda e, a_=a_, b_=b_: e.tensor_tensor(out=G[3][:, a_:b_], in0=G[3][:, a_:b_], in1=G[0][:, a_:b_], op=ALU.mult), r=[K3, K0], w=[K3])
                    S = SEG_SPLIT
                    if d == 0:
                        P.dve(lambda e: e.tensor_tensor_scan(out=G4[:, 0:S], data0=G[2][:, 0:S], data1=G[3][:, 0:S], initial=0.0, op0=ALU.mult, op1=ALU.add),
                              r=["G2/0", "G3/0"], w=["QK/0"])
                        P.dve(lambda e: e.tensor_tensor_scan(out=G4[:, S:T_ALL], data0=G[2][:, S:T_ALL], data1=G[3][:, S:T_ALL], initial=G4[:, S - 1:S],
                                                             op0=ALU.mult, op1=ALU.add), r=["G2/1", "G3/1", "QK/0"], w=["QK/1"])
                    else:
                        P.dve(lambda e: e.tensor_tensor_scan(out=G[0][:, 255::-1], data0=G[2][:, 255::-1], data1=G[3][:, 255::-1], initial=0.0, op0=ALU.mult, op1=ALU.add),
                              r=["G2/0", "G3/0"], w=["G0/0"])
                        P.dve(lambda e: e.tensor_tensor_scan(out=G[0][:, T_ALL - 1:S - 1:-1], data0=G[2][:, T_ALL - 1:S - 1:-1], data1=G[3][:, T_ALL - 1:S - 1:-1],
                                                             initial=G[0][:, 0:1], op0=ALU.mult, op1=ALU.add), r=["G2/1", "G3/1", "G0/0"], w=["G0/1"])
                        P.dve(lambda e: e.tensor_tensor_scan(out=G[0][:, S - 1:255:-1], data0=G[2][:, S - 1:255:-1], data1=G[3][:, S - 1:255:-1],
                                                             initial=G[0][:, S:S + 1], op0=ALU.mult, op1=ALU.add), r=["G2/0", "G3/0", "G0/1", "G0/0"], w=["G0/0"])
                        for sg_ in range(2):
                            a_, b_ = SEGS[sg_]
                            P.dve(lambda e, a_=a_, b_=b_: e.tensor_tensor(out=G4[:, a_:b_], in0=G4[:, a_:b_], in1=G[0][:, a_:b_], op=ALU.add),
                                  r=["QK/%d" % sg_, "G0/%d" % sg_], w=["QK/%d" % sg_])
                def gate_evac(ti, t0, n, pbi):
                    a = tmp[0]; b_ = tmp[1]; ob = hlb[ti % 2]; okey = "obf%d" % (ti % 2); pk = "pb%d" % pbi
                    P.dve(lambda e: e.tensor_copy(out=a[:, :n], in_=pb[pbi][:, :n]), r=[pk], w=["tmp0"])
                    P.dve(lambda e: e.tensor_tensor(out=b_[:, :n], in0=a[:, :n], in1=a[:, :n], op=ALU.mult), r=["tmp0"], w=["tmp1"])
                    P.dve(lambda e: e.tensor_scalar(out=b_[:, :n], in0=b_[:, :n], scalar1=0.044715, scalar2=1.0, op0=ALU.mult, op1=ALU.add), r=["tmp1"], w=["tmp1"])
                    P.dve(lambda e: e.tensor_tensor(out=b_[:, :n], in0=b_[:, :n], in1=a[:, :n], op=ALU.mult), r=["tmp1", "tmp0"], w=["tmp1"])
                    P.act(lambda e: e.activation(out=b_[:, :n], in_=b_[:, :n], func=AF.Sigmoid, scale=2.0 * 0.7978845608028654), r=["tmp1"], w=["tmp1"])
                    P.dve(lambda e: e.tensor_tensor(out=a[:, :n], in0=a[:, :n], in1=b_[:, :n], op=ALU.mult), r=["tmp0", "tmp1"], w=["tmp0"])
                    P.dve(lambda e: e.tensor_tensor(out=ob[:, :n], in0=a[:, :n], in1=G4[:, t0:t0 + n], op=ALU.mult), r=["tmp0", sk("QK", t0)], w=[okey])
                    P.dma("sp", out_mix[:, 8 + 4 * s + nb, t0:t0 + n], ob[:, :n], r=[okey], final=True)
                proj_fm(8 + nb, None, None, evac=gate_evac)

        for s_ in range(2):
            half(s_, drs[s_])
        P.emit()


def mix_common_consts(layer, I):
    d = {}
    d["modw"] = modblk(I["mod_w"][layer], 0, 2048)
    d["modb"] = colvec(I["mod_b"][layer][0:2048])
    d["g0"] = colvec(I["norm_g"][layer][0])
    r = np.arange(128)
    d["triU"] = (r[:, None] <= r[None, :]).astype(np.float32)
    d["triL"] = (r[:, None] >= r[None, :]).astype(np.float32)
    import ml_dtypes
    d["identb"] = np.eye(128, dtype=np.float32).astype(ml_dtypes.bfloat16)
    return d


def mix0_consts(I, s):
    W = I["ab_in_w"][0]
    h0, h1 = 2 * s, 2 * s + 1
    qcol = lambda h: np.arange(128 * h, 128 * h + 128)
    kcol = lambda h: 512 + np.arange(128 * h, 128 * h + 128)
    blocks = [qcol(h0), kcol(h0), qcol(h1), kcol(h1)]
    blocks += [3088 + 512 * s + 128 * n + np.arange(128) for n in range(4)]
    blocks += [4112 + 512 * s + 128 * n + np.arange(128) for n in range(4)]
    d = {}
    d["wfm"] = np.stack([kblk(W[:, c]) for c in blocks], 0)
    d["wv"] = np.stack([kblk(W[:, 1024 + 256 * h:1024 + 256 * h + 256]) for h in (h0, h1)], 0)
    d["wo"] = kblk(W[:, 2048 + 512 * s:2048 + 512 * s + 512])
    gcols = [3072 + t * 4 + h for t in range(4) for h in (h0, h1)]
    d["wg"] = kblk(W[:, gcols])
    gb = I["m_gate_b"][0]
    d["gbias"] = np.ascontiguousarray(np.broadcast_to(np.array([gb[t, h] for t in range(4) for h in (h0, h1)], np.float32)[None, :], (128, 8)))
    cw = np.zeros((128, 8, 4), np.float32); cb = np.zeros((128, 8), np.float32)
    for bi in range(4):
        cw[:, bi, :] = I["m_conv_w"][0][:, blocks[bi]].T
        cb[:, bi] = I["m_conv_b"][0][blocks[bi]]
    for n in range(4):
        ch = 512 * s + 128 * n + np.arange(128)
        cw[:, 4 + n, :] = I["l_conv_w"][0][:, ch].T
        cb[:, 4 + n] = I["l_conv_b"][0][ch]
    d["cw"] = cw; d["cb"] = cb
    d["mnorm"] = np.ascontiguousarray(np.broadcast_to(I["m_norm_w"][0][512 * s:512 * s + 512][None, :], (128, 512)))
    d["lwa"] = np.ascontiguousarray(I["l_wa"][0][:, 4 * s:4 * s + 4].transpose(2, 0, 1, 3))
    d["lwx"] = np.ascontiguousarray(I["l_wx"][0][:, 4 * s:4 * s + 4].transpose(2, 0, 1, 3))
    pc = lambda v: np.ascontiguousarray(v[:, 512 * s:512 * s + 512].reshape(2, 4, 128).transpose(2, 0, 1))
    d["lba"] = pc(I["l_ba"][0]); d["lbx"] = pc(I["l_bx"][0]); d["llam"] = pc(I["l_lam"][0])
    return d


def stream_fm(I_x, I_ctx, b):
    return fm(np.concatenate([I_ctx[b], I_x[b]], 0))


DK_SCALE = float(128.0 ** -0.5)


def phase_mix1(nc, P, pb, drs, xT, out_mix):
    dr = drs[0]
    modw = dr("modw", [16, 128, 8, 128])
    modb = dr("modb", [128, 16])
    cT = dr("cT", [128, 8, 2])
    g0 = dr("g0", [128, 8])
    triUd = dr("triU", [128, 128])
    triLd = dr("triL", [128, 128])
    identd = dr("identb", [128, 128], BF16)
    identfd = dr("identf", [128, 128])
    with ExitStack() as es:
        sb, ps = _mk(nc, es)
        hT = sb("hT", [128, 8, T_ALL], BF16)
        G = [sb("G%d" % i, [128, T_ALL]) for i in range(4)]
        QA = sb("QA", [128, T_ALL]); XB = sb("XB", [128, T_ALL], BF16)
        qab = QA[:].bitcast(BF16)
        qT = qab[:, 0:T_ALL]; kT = qab[:, T_ALL:2 * T_ALL]
        VT = G[1][:].bitcast(BF16); vtok = VT.rearrange("p (c v) -> p c v", v=256)
        ktok = G[0][:].bitcast(BF16)[:, 0:T_ALL].rearrange("p (c d) -> p c d", d=128)
        tmp = [sb("tmp%d" % i, [128, 512]) for i in range(4)]
        rstd = sb("rstd", [128, 512]); onesb = sb("onesb", [128, 128], BF16); onesf = sb("onesf", [128, 128]); epsc = sb("epsc", [128, 1])
        g0sb = sb("g0sb", [128, 8]); A0 = sb("A0", [128, 8, 2])
        triU = sb("triU_sb", [128, 128]); triL = sb("triL_sb", [128, 128]); identb = sb("identb_sb", [128, 128], BF16); identf = sb("identf_sb", [128, 128])
        wfb = [sb("wfb0", [128, 8, 128], BF16)] * 2
        wvb = sb("wvb", [128, 8, 256], BF16); wgab = sb("wgab", [128, 8, 32], BF16); wdtb = sb("wdtb", [128, 8, 16], BF16)
        alph = sb("alph_sb", [64, 256], BF16); gns = sb("gns", [128, 512]); cws = sb("cws", [128, 6, 4]); cbs = sb("cbs", [128, 6])
        dtbs = sb("dtbs", [128, 16]); Aneg = sb("Aneg", [128, 16]); sDs = sb("sDs", [128, 8])
        DEC = sb("DEC", [128, 6, NCH, 16])
        EBLg = sb("EBLg", [128, NCH])
        Sacc = [sb("Sacc%d" % i, [128, 256]) for i in range(2)]; Sbf = [sb("Sbf%d" % i, [128, 256], BF16) for i in range(2)]
        PT = [sb("PT%d" % i, [128, 128], BF16) for i in range(2)]
        CBm = [sb("CBm%d" % i, [128, 128]) for i in range(2)]; MT = [sb("MT%d" % i, [128, 512], BF16) for i in range(2)]
        obT = MT[0]
        xdt = [sb("xdt%d" % i, [128, 256], BF16) for i in range(2)]; xw = [sb("xw%d" % i, [128, 256], BF16) for i in range(2)]
        ssq = sb("ssq", [128, NCH]); obf = [sb("obf%d" % i, [128, 512], BF16) for i in range(2)]
        for (dst, src, key) in [(g0sb, g0, "g0sb"), (triU, triUd, "triU"), (triL, triLd, "triL"), (identb, identd, "identb"), (identf, identfd, "identf")]:
            P.dma("sp", dst[:], src, w=[key])
        P.dve(lambda e: e.memset(onesb[:], 1.0), w=["onesb"])
        P.dve(lambda e: e.memset(onesf[:], 1.0), w=["onesf"])
        P.dve(lambda e: e.memset(epsc[:], EPS), w=["epsc"])
        mwb = [(G[1][:, 1024 * i:1024 * (i + 1)].rearrange("p (a b) -> p a b", b=128), ["mwst%d" % i]) for i in range(4)]
        modT = emit_modulation(P, nc, sb, pb[7], modw, modb, cT, 16, mwb)
        xin = G[0][:, 0:4096].rearrange("p (k n) -> p k n", k=8)
        sqv = XB[:, 0:4096].rearrange("p (k n) -> p k n", k=8)
        XBK = ["XB"] + ["XB%d" % k for k in range(8)]
        emit_adaln_in(P, nc, sb, pb, xT, modT, g0sb, hT, xin, "G0", sqv, "XB", tmp, rstd, onesb, epsc, A0)
        def half(s, dr):
            wfm = dr("wfm", [10, 128, 8, 128])
            wga = dr("wga", [128, 8, 32])
            wv = dr("wv", [2, 128, 8, 256])
            wr = dr("wr", [128, 8, 512])
            wz = dr("wz", [128, 8, 512])
            wdt = dr("wdt", [128, 8, 16])
            alphd = dr("alph", [64, 256])
            gnorm = dr("gnorm", [128, 512])
            cw = dr("cw", [128, 6, 4])
            cb = dr("cb", [128, 6])
            dtbd = dr("dtb", [128, 16])
            alogd = dr("alog", [128, 16])
            sDd = dr("sD", [128, 8])
            for (dst, src, key) in [(cws, cw, "cws"), (cbs, cb, "cbs"), (gns, gnorm, "gns"), (dtbs, dtbd, "dtbs"), (Aneg, alogd, "Aneg"), (sDs, sDd, "sDs")]:
                P.dma("sp", dst[:], src, w=[key])
            for (dst, src, key) in [(wgab, wga, "wgab"), (wdtb, wdt, "wdtb"), (alph, alphd, "alph")]:
                P.dma("pool", dst[:], src, w=[key])
            P.act(lambda e: e.activation(out=Aneg[:], in_=Aneg[:], func=AF.Exp), r=["Aneg"], w=["Aneg"])
            P.dve(lambda e: e.tensor_scalar(out=Aneg[:], in0=Aneg[:], scalar1=-1.0, scalar2=None, op0=ALU.mult), r=["Aneg"], w=["Aneg"])
            wfcnt = [0]

            def proj_fm(blk, dst, dkey):
                sl = wfcnt[0] % 2; wfcnt[0] += 1
                if sl == 0:
                    wdst = wfb[0][:]; wk = "wfb0"; wl = lambda k: wfb[0][:, k, :]
                else:
                    wdst = wvb[:, :, 128:256]; wk = "wvb"; wl = lambda k: wvb[:, k, 128:256]
                P.dma("pool", wdst, wfm[blk], w=[wk])
                for ti, (t0, n, col) in enumerate(TOK_TILES):
                    pbi = ti % 2
                    for k in range(8):
                        P.pe(lambda e, k=k, t0=t0, n=n, pbi=pbi: e.matmul(pb[pbi][:, :n], lhsT=wl(k), rhs=hT[:, k, t0:t0 + n],
                                                                          start=(k == 0), stop=(k == 7)), r=[wk, "hT"], w=["pb%d" % pbi])
                    P.act(lambda e, t0=t0, n=n, pbi=pbi: e.activation(out=dst[:, t0:t0 + n], in_=pb[pbi][:, :n], func=AF.Copy), r=["pb%d" % pbi], w=[dkey])

            def proj_tok2(wsb, wkey, c2, pbi):
                for cc in range(2):
                    c = 2 * c2 + cc
                    for k in range(8):
                        P.pe(lambda e, c=c, cc=cc, k=k: e.matmul(pb[pbi][:, cc * 256:(cc + 1) * 256], lhsT=hT[:, k, c * 128:(c + 1) * 128], rhs=wsb[:, k, :],
                                                                 start=(k == 0), stop=(k == 7)), r=["hT", wkey], w=["pb%d" % pbi])

            masks = [triU, triL]; orders = [ORDER_F, ORDER_B]
            Hm = [G[2], G[3]]

            def Hv(c):
                g = Hm[c // 17]; cc = c % 17
                return g[:, cc * 256:(cc + 1) * 256]

            def headnorm_gate(normsb, nkey, nslice, gate_w_dram, func, ch0):
                Hall = [g[:, 0:17 * 256].rearrange("p (c v) -> p c v", v=256) for g in Hm]
                for gi in range(2):
                    P.dve(lambda e, gi=gi: e.tensor_tensor(out=G[gi][:, 0:17 * 256], in0=Hm[gi][:, 0:17 * 256], in1=Hm[gi][:, 0:17 * 256], op=ALU.mult),
                          r=["G%d" % (2 + gi)], w=["G%d" % gi])
                    P.dve(lambda e, gi=gi: e.tensor_reduce(out=ssq[:, gi * 17:(gi + 1) * 17], in_=G[gi][:, 0:17 * 256].rearrange("p (c v) -> p c v", v=256), axis=AX.X, op=ALU.add),
                          r=["G%d" % gi], w=["ssq"])
                P.act(lambda e: e.activation(out=ssq[:], in_=ssq[:], func=AF.Sqrt, bias=epsc[:, 0:1], scale=1.0 / 256), r=["ssq", "epsc"], w=["ssq"])
                P.dve(lambda e: e.reciprocal(out=ssq[:], in_=ssq[:]), r=["ssq"], w=["ssq"])
                for gi in range(2):
                    P.dve(lambda e, gi=gi: e.tensor_tensor(out=Hall[gi], in0=Hall[gi], in1=ssq[:, gi * 17:(gi + 1) * 17].unsqueeze(2).to_broadcast([128, 17, 256]), op=ALU.mult),
                          r=["G%d" % (2 + gi), "ssq"], w=["G%d" % (2 + gi)])
                    P.dve(lambda e, gi=gi: e.tensor_tensor(out=Hall[gi], in0=Hall[gi], in1=normsb[:, nslice].unsqueeze(1).to_broadcast([128, 17, 256]), op=ALU.mult),
                          r=["G%d" % (2 + gi), nkey], w=["G%d" % (2 + gi)])
                gate_out(gate_w_dram, func, ch0)

            def gate_out(gate_w_dram, func, ch0):
                P.dma("pool", wvb[:], gate_w_dram, w=["wvb"])
                for c2 in range(NCH // 2):
                    pbi = c2 % 2; ob = obf[c2 % 2]; og = tmp[c2 % 2]
                    proj_tok2(wvb, "wvb", c2, pbi)
                    P.act(lambda e, pbi=pbi, og=og: e.activation(out=og[:], in_=pb[pbi][:, :], func=func), r=["pb%d" % pbi], w=["tmp%d" % (c2 % 2)])
                    c0 = 2 * c2
                    for cc in range(2):
                        P.dve(lambda e, ob=ob, og=og, cc=cc, c0=c0: e.tensor_tensor(out=ob[:, cc * 256:(cc + 1) * 256], in0=og[:, cc * 256:(cc + 1) * 256], in1=Hv(c0 + cc), op=ALU.mult),
                              r=["tmp%d" % (c2 % 2), "G2", "G3"], w=["obf%d" % (c2 % 2)])
                    emit_tok2fm(P, pb[7], identb, ob, "obf%d" % (c2 % 2), obT, "MT0", out_mix[:, ch0:ch0 + 2, c0 * 128:(c0 + 2) * 128])

            P.dve(lambda e: e.memset(XB[0:64, :], 1.0), r=XBK, w=XBK)
            for d in range(2):
                for ti, (t0, n, col) in enumerate(TOK_TILES):
                    pbi = ti % 2
                    for k in range(8):
                        P.pe(lambda e, k=k, d=d, t0=t0, n=n, pbi=pbi: e.matmul(pb[pbi][0:16, :n], lhsT=wgab[:, k, d * 16:(d + 1) * 16], rhs=hT[:, k, t0:t0 + n],
                                                                              start=(k == 0), stop=(k == 7)), r=["wgab", "hT"], w=["pb%d" % pbi])
                    P.act(lambda e, d=d, t0=t0, n=n, pbi=pbi: e.activation(out=XB[32 * d:32 * d + 16, t0:t0 + n], in_=pb[pbi][0:16, :n], func=AF.Copy),
                          r=["pb%d" % pbi], w=XBK)
            for hh in range(2):
                P.dve(lambda e: e.memset(G[2][:, :], 0.0), w=["G2"])
                P.dve(lambda e: e.memset(G[3][:, :], 0.0), w=["G3"])
                for d in range(2):
                    proj_fm(2 * hh, G[0], "G0")
                    proj_fm(2 * hh + 1, G[1], "G1")
                    ecol = 127 if d == 0 else 0
                    for c4 in range(0, NCH, 4):
                        nn = min(4, NCH - c4); W = nn * 128; cs = slice(c4 * 128, c4 * 128 + W)
                        for cc in range(nn):
                            c = c4 + cc
                            P.pe(lambda e, c=c, cc=cc, d=d, hh=hh: e.matmul(pb[6][:, cc * 128:(cc + 1) * 128], lhsT=XB[32 * d:32 * d + 17, c * 128:(c + 1) * 128],
                                                                            rhs=alph[32 * d:32 * d + 17, hh * 128:(hh + 1) * 128], start=True, stop=True), r=XBK + ["alph"], w=["pb6"])
                        P.act(lambda e, W=W: e.activation(out=tmp[0][:, :W], in_=pb[6][:, :W], func=AF.Exp, scale=-1.0), r=["pb6"], w=["tmp0"])
                        P.act(lambda e, W=W: e.activation(out=tmp[0][:, :W], in_=tmp[0][:, :W], func=AF.Ln, bias=1.0), r=["tmp0"], w=["tmp0"])
                        for cc in range(nn):
                            P.pe(lambda e, cc=cc, d=d: e.matmul(pb[7][:, cc * 128:(cc + 1) * 128], lhsT=tmp[0][:, cc * 128:(cc + 1) * 128], rhs=masks[d][:], start=True, stop=True),
                                 r=["tmp0", "triU", "triL"], w=["pb7"])
                        P.act(lambda e, W=W: e.activation(out=tmp[1][:, :W], in_=pb[7][:, :W], func=AF.Exp, scale=-1.0 / 16), r=["pb7"], w=["tmp1"])
                        P.act(lambda e, W=W: e.activation(out=tmp[2][:, :W], in_=pb[7][:, :W], func=AF.Exp, scale=1.0 / 16), r=["pb7"], w=["tmp2"])
                        P.dve(lambda e, W=W, cs=cs: e.tensor_tensor(out=qT[:, cs], in0=G[0][:, cs], in1=tmp[1][:, :W], op=ALU.mult), r=["G0", "tmp1"], w=["QA"])
                        P.dve(lambda e, W=W, cs=cs: e.tensor_tensor(out=kT[:, cs], in0=G[1][:, cs], in1=tmp[2][:, :W], op=ALU.mult), r=["G1", "tmp2"], w=["QA"])
                        P.dve(lambda e, W=W, c4=c4, nn=nn, ecol=ecol: e.tensor_copy(out=EBLg[:, c4:c4 + nn], in_=tmp[1][:, ecol:W:128]), r=["tmp1"], w=["EBLg"])
                    if d == 0:
                        pass
                    P.dma("pool", wvb[:], wv[hh], w=["wvb"])
                    for c2 in range(NCH // 2):
                        pbi = c2 % 2
                        proj_tok2(wvb, "wvb", c2, pbi)
                        P.act(lambda e, c2=c2, pbi=pbi: e.activation(out=VT[:, c2 * 512:(c2 + 1) * 512], in_=pb[pbi][:, :], func=AF.Copy), r=["pb%d" % pbi], w=["G1"])
                    pT = pb[7][:].bitcast(BF16)
                    for c4 in range(0, NCH, 4):
                        nn = min(4, NCH - c4)
                        for cc in range(nn):
                            c = c4 + cc
                            P.pe(lambda e, c=c, cc=cc: e.transpose(pT[:, cc * 128:(cc + 1) * 128], kT[:, c * 128:(c + 1) * 128], identb[:]), r=["QA", "identb"], w=["pb7"])
                        P.act(lambda e, c4=c4, nn=nn: e.activation(out=ktok[:, c4:c4 + nn, :].rearrange("p c d -> p (c d)"), in_=pT[:, 0:nn * 128], func=AF.Copy),
                              r=["pb7"], w=["G0"])
                    for i in range(NCH):
                        c = orders[d][i]; cprev = orders[d][i - 1] if i > 0 else None
                        par = i % 2
                        qc = qT[:, c * 128:(c + 1) * 128]; kc = kT[:, c * 128:(c + 1) * 128]
                        pS = pb[0 + par]; pO = pb[2 + par]; pD = pb[4 + par]
                        kS, kO, kD = "pb%d" % par, "pb%d" % (2 + par), "pb%d" % (4 + par)
                        hk = "G%d" % (2 + c // 17)
                        P.pe(lambda e, pS=pS, kc=kc, qc=qc: e.matmul(pS[:, 0:128], lhsT=kc, rhs=qc, start=True, stop=True), r=["QA"], w=[kS])
                        P.dve(lambda e, par=par, pS=pS, d=d: e.tensor_tensor(out=PT[par][:], in0=pS[:, 0:128], in1=masks[d][:], op=ALU.mult),
                              r=[kS, "triU", "triL"], w=["PT%d" % par])
                        P.pe(lambda e, par=par, pO=pO, c=c, i=i: e.matmul(pO[:, 0:256], lhsT=PT[par][:], rhs=vtok[:, c, :], start=True, stop=(i == 0)), r=["PT%d" % par, "G1"], w=[kO])
                        if i > 0:
                            P.pe(lambda e, pO=pO, qc=qc: e.matmul(pO[:, 0:256], lhsT=qc, rhs=Sbf[0][:], start=False, stop=True), r=["QA", "Sbf0"], w=[kO])
                        P.dve(lambda e, pO=pO, c=c: e.scalar_tensor_tensor(out=Hv(c), in0=pO[:, 0:256], scalar=DK_SCALE, in1=Hv(c), op0=ALU.mult, op1=ALU.add),
                              r=[kO, hk], w=[hk])
                        if i < NCH - 1:
                            P.pe(lambda e, pD=pD, c=c: e.matmul(pD[:, 0:256], lhsT=ktok[:, c, :], rhs=vtok[:, c, :], start=True, stop=True), r=["G0", "G1"], w=[kD])
                            if i == 0:
                                P.dve(lambda e, pD=pD: e.tensor_copy(out=Sacc[0][:], in_=pD[:, 0:256]), r=[kD], w=["Sacc0"])
                            else:
                                eprev = EBLg[:, cprev:cprev + 1]
                                P.dve(lambda e, pD=pD, eprev=eprev: e.scalar_tensor_tensor(out=Sacc[0][:], in0=Sacc[0][:], scalar=eprev, in1=pD[:, 0:256],
                                                                                            op0=ALU.mult, op1=ALU.add), r=[kD, "EBLg", "Sacc0"], w=["Sacc0"])
                            ecur = EBLg[:, c:c + 1]
                            P.act(lambda e, ecur=ecur: e.activation(out=Sbf[0][:], in_=Sacc[0][:], func=AF.Identity, scale=ecur), r=["Sacc0", "EBLg"], w=["Sbf0"])
                        emit_pe_warm(P, pb[7], onesb, hT, 3)
                headnorm_gate(gns, "gns", slice(hh * 256, (hh + 1) * 256), wr[:, :, hh * 256:(hh + 1) * 256], AF.Silu, (2 * s + hh) * 2)
            DT, LA, B_S, EBS, EBL, W_S = [DEC[:, j] for j in range(6)]
            BL = EBL
            for half, pbx in [(0, pb[7]), (1, pb[6])]:
                for cc in range(17):
                    c = half * 17 + cc
                    for k in range(8):
                        P.pe(lambda e, c=c, cc=cc, k=k, pbx=pbx: e.matmul(pbx[:, cc * 16:(cc + 1) * 16], lhsT=hT[:, k, c * 128:(c + 1) * 128], rhs=wdtb[:, k, :],
                                                                          start=(k == 0), stop=(k == 7)), r=["hT", "wdtb"], w=["pb7" if half == 0 else "pb6"])
                P.dve(lambda e, half=half, pbx=pbx: e.tensor_tensor(out=DT[:, half * 17:(half + 1) * 17, :], in0=pbx[:, 0:272].rearrange("p (c g) -> p c g", g=16),
                                                                    in1=dtbs[:].unsqueeze(1).to_broadcast([128, 17, 16]), op=ALU.add), r=["pb7" if half == 0 else "pb6", "dtbs"], w=["DEC0"])
            P.act(lambda e: e.activation(out=DT, in_=DT, func=AF.Exp), r=["DEC0"], w=["DEC0"])
            P.act(lambda e: e.activation(out=DT, in_=DT, func=AF.Ln, bias=1.0), r=["DEC0"], w=["DEC0"])
            P.dve(lambda e: e.tensor_tensor(out=LA, in0=DT, in1=Aneg[:].unsqueeze(1).to_broadcast([128, NCH, 16]), op=ALU.mult), r=["DEC0", "Aneg"], w=["DEC1"])
            for d in range(2):
                pcs, pbl = pb[6], pb[7]
                P.pe(lambda e, d=d: e.matmul(pb[6][:, 0:272], lhsT=masks[d][:], rhs=LA[:, :, d * 8:(d + 1) * 8], start=True, stop=True), r=["triU", "triL", "DEC1"], w=["pb6"])
                P.pe(lambda e, d=d: e.matmul(pb[7][:, 0:272], lhsT=onesf[:], rhs=LA[:, :, d * 8:(d + 1) * 8], start=True, stop=True), r=["onesf", "DEC1"], w=["pb7"])
                P.dve(lambda e, d=d: e.tensor_copy(out=B_S[:, :, d * 8:(d + 1) * 8], in_=pb[6][:, 0:272].rearrange("p (c g) -> p c g", g=8)), r=["pb6"], w=["DEC2"])
                P.dve(lambda e, d=d: e.tensor_copy(out=BL[:, :, d * 8:(d + 1) * 8], in_=pb[7][:, 0:272].rearrange("p (c g) -> p c g", g=8)), r=["pb7"], w=["DEC5"])
            P.act(lambda e: e.activation(out=EBS, in_=B_S, func=AF.Exp), r=["DEC2"], w=["DEC4"])
            P.dve(lambda e: e.tensor_tensor(out=W_S, in0=BL, in1=B_S, op=ALU.subtract), r=["DEC2", "DEC5"], w=["DEC6"])
            P.act(lambda e: e.activation(out=EBL, in_=BL, func=AF.Exp), r=["DEC5", "DEC6"], w=["DEC5"])
            P.act(lambda e: e.activation(out=W_S, in_=W_S, func=AF.Exp), r=["DEC6"], w=["DEC6"])
            P.dve(lambda e: e.tensor_tensor(out=W_S, in0=W_S, in1=DT, op=ALU.mult), r=["DEC6", "DEC0"], w=["DEC6"])
            BT = qT; CT = kT
            btok = XB[:, :].rearrange("p (c n) -> p c n", n=128)
            for (blk, dstb) in [(8, BT), (9, CT)]:
                proj_fm(blk, G[3], "G3")
                emit_conv(P, G[3], "G3", G[2], "G2", cws[:, blk - 4, :], cbs[:, blk - 4:blk - 3], ["cws", "cbs"])
                P.act(lambda e, dstb=dstb: e.activation(out=dstb, in_=G[2][:, :], func=AF.Silu), r=["G2"], w=["QA"])
            pT = pb[7][:].bitcast(BF16)
            for c4 in range(0, NCH, 4):
                nn = min(4, NCH - c4)
                for cc in range(nn):
                    c = c4 + cc
                    P.pe(lambda e, c=c, cc=cc: e.transpose(pT[:, cc * 128:(cc + 1) * 128], BT[:, c * 128:(c + 1) * 128], identb[:]), r=["QA", "identb"], w=["pb7"])
                P.act(lambda e, c4=c4, nn=nn: e.activation(out=btok[:, c4:c4 + nn, :].rearrange("p c d -> p (c d)"), in_=pT[:, 0:nn * 128], func=AF.Copy),
                      r=["pb7"], w=XBK)
            xtok = [G[0][:, 0:17 * 256].rearrange("p (c v) -> p c v", v=256), G[1][:, 0:17 * 256].rearrange("p (c v) -> p c v", v=256)]

            def Xv(c):
                return [G[0], G[1]][c // 17][:, (c % 17) * 256:(c % 17 + 1) * 256]

            for hf in range(2):
                for xb in range(2):
                    blk = 4 + 2 * hf + xb
                    proj_fm(blk, G[3], "G3")
                    emit_conv(P, G[3], "G3", G[2], "G2", cws[:, blk - 4, :], cbs[:, blk - 4:blk - 3], ["cws", "cbs"])
                    P.act(lambda e: e.activation(out=G[3][:, :], in_=G[2][:, :], func=AF.Silu), r=["G2"], w=["G3"])
                    for c4 in range(0, NCH, 4):
                        nn = min(4, NCH - c4)
                        for cc in range(nn):
                            c = c4 + cc
                            P.pe(lambda e, c=c, cc=cc: e.transpose(pb[6][:, cc * 128:(cc + 1) * 128], G[3][:, c * 128:(c + 1) * 128], identf[:]), r=["G3", "identf"], w=["pb6"])
                        for cc in range(nn):
                            c = c4 + cc
                            P.act(lambda e, c=c, cc=cc, xb=xb: e.activation(out=Xv(c)[:, xb * 128:(xb + 1) * 128], in_=pb[6][:, cc * 128:(cc + 1) * 128], func=AF.Copy),
                                  r=["pb6"], w=["G%d" % (c // 17)])
                P.dve(lambda e: e.memset(G[2][:, :], 0.0), w=["G2"])
                P.dve(lambda e: e.memset(G[3][:, :], 0.0), w=["G3"])
                for i in range(NCH):
                    caps = []
                    for d in range(2):
                        with P.capture() as cap:
                            c = orders[d][i]
                            hs = slice(d * 8 + 4 * hf, d * 8 + 4 * hf + 4)
                            pA = pb[0 + d]; pB = pb[2 + d]; pC = pb[4 + d]
                            kA, kB, kC = "pb%d" % d, "pb%d" % (2 + d), "pb%d" % (4 + d)
                            hk = "G%d" % (2 + c // 17); xk = "G%d" % (c // 17)
                            Bc = BT[:, c * 128:(c + 1) * 128]; Cc = CT[:, c * 128:(c + 1) * 128]
                            P.pe(lambda e, pA=pA, Bc=Bc, Cc=Cc: e.matmul(pA[:, 0:128], lhsT=Bc, rhs=Cc, start=True, stop=True), r=["QA"], w=[kA])
                            P.dve(lambda e, d=d, pA=pA: e.tensor_tensor(out=CBm[d][:], in0=pA[:, 0:128], in1=masks[d][:], op=ALU.mult), r=[kA, "triU", "triL"], w=["CBm%d" % d])
                            R = tmp[d]; Rk = "tmp%d" % d
                            P.pool(lambda e, R=R, d=d, c=c, hs=hs: e.tensor_tensor(out=R[:].rearrange("p (e t) -> p e t", e=4), in0=masks[d][:].unsqueeze(1).to_broadcast([128, 4, 128]),
                                                                                  in1=LA[:, c, hs].unsqueeze(2).to_broadcast([128, 4, 128]), op=ALU.mult), r=["triU", "triL", "DEC1"], w=[Rk])
                            P.pe(lambda e, pB=pB, R=R: e.matmul(pB[:, :], lhsT=onesf[:], rhs=R[:], start=True, stop=True), r=["onesf", Rk], w=[kB])
                            Dl = tmp[2 + d]; Dk = "tmp%d" % (2 + d)
                            for e4 in range(4):
                                bcol = B_S[:, c, d * 8 + 4 * hf + e4:d * 8 + 4 * hf + e4 + 1]
                                P.dve(lambda e, pB=pB, Dl=Dl, e4=e4, bcol=bcol: e.tensor_scalar(out=Dl[:, e4 * 128:(e4 + 1) * 128], in0=pB[:, e4 * 128:(e4 + 1) * 128], scalar1=bcol, scalar2=0.0,
                                                                                                op0=ALU.subtract, op1=ALU.min), r=[kB, "DEC2"], w=[Dk])
                            P.act(lambda e, Dl=Dl: e.activation(out=Dl[:], in_=Dl[:], func=AF.Exp), r=[Dk], w=[Dk])
                            P.dve(lambda e, d=d, Dl=Dl: e.tensor_tensor(out=MT[d][:].rearrange("p (e t) -> p e t", e=4), in0=Dl[:].rearrange("p (e t) -> p e t", e=4),
                                                                        in1=CBm[d][:].unsqueeze(1).to_broadcast([128, 4, 128]), op=ALU.mult), r=[Dk, "CBm%d" % d], w=["MT%d" % d])
                            for e4 in range(4):
                                hi = d * 8 + 4 * hf + e4
                                P.act(lambda e, d=d, c=c, e4=e4, hi=hi: e.activation(out=xdt[d][:, e4 * 64:(e4 + 1) * 64], in_=Xv(c)[:, e4 * 64:(e4 + 1) * 64], func=AF.Identity,
                                                                                     scale=DT[:, c, hi:hi + 1]), r=[xk, "DEC0"], w=["xdt%d" % d])
                            for e4 in range(4):
                                hi = d * 8 + 4 * hf + e4
                                P.act(lambda e, d=d, c=c, e4=e4, hi=hi: e.activation(out=xw[d][:, e4 * 64:(e4 + 1) * 64], in_=Xv(c)[:, e4 * 64:(e4 + 1) * 64], func=AF.Identity,
                                                                                     scale=W_S[:, c, hi:hi + 1]), r=[xk, "DEC6"], w=["xw%d" % d])
                            for e4 in range(4):
                                P.pe(lambda e, pC=pC, d=d, e4=e4: e.matmul(pC[:, e4 * 64:(e4 + 1) * 64], lhsT=MT[d][:, e4 * 128:(e4 + 1) * 128], rhs=xdt[d][:, e4 * 64:(e4 + 1) * 64], start=True, stop=True),
                                     r=["MT%d" % d, "xdt%d" % d], w=[kC])
                            if i > 0:
                                P.pe(lambda e, pC=pC, d=d, Cc=Cc: e.matmul(pC[:, 256:512], lhsT=Cc, rhs=Sbf[d][:], start=True, stop=True), r=["QA", "Sbf%d" % d], w=[kC])
                                P.dve(lambda e, pC=pC, d=d, c=c, hs=hs, Dl=Dl: e.tensor_tensor(out=Dl[:, 0:256].rearrange("p (e q) -> p e q", e=4), in0=pC[:, 256:512].rearrange("p (e q) -> p e q", e=4),
                                                                                        in1=EBS[:, c, hs].unsqueeze(2).to_broadcast([128, 4, 64]), op=ALU.mult), r=[kC, "DEC4", "MT%d" % d], w=[Dk])
                                P.dve(lambda e, Dl=Dl, c=c: e.tensor_tensor(out=Hv(c), in0=Hv(c), in1=Dl[:, 0:256], op=ALU.add), r=[Dk, hk], w=[hk])
                            P.dve(lambda e, pC=pC, c=c: e.tensor_tensor(out=Hv(c), in0=Hv(c), in1=pC[:, 0:256], op=ALU.add), r=[kC, hk], w=[hk])
                            if i < NCH - 1:
                                P.pe(lambda e, pA=pA, c=c, d=d: e.matmul(pA[:, 256:512], lhsT=btok[:, c, :], rhs=xw[d][:], start=True, stop=True), r=XBK + ["xw%d" % d], w=[kA])
                                if i == 0:
                                    P.dve(lambda e, pA=pA, d=d: e.tensor_copy(out=Sacc[d][:], in_=pA[:, 256:512]), r=[kA], w=["Sacc%d" % d])
                                else:
                                    P.dve(lambda e, d=d, c=c, hs=hs: e.tensor_tensor(out=Sacc[d][:].rearrange("p (e q) -> p e q", e=4), in0=Sacc[d][:].rearrange("p (e q) -> p e q", e=4),
                                                                                     in1=EBL[:, c, hs].unsqueeze(2).to_broadcast([128, 4, 64]), op=ALU.mult), r=["Sacc%d" % d, "DEC5"], w=["Sacc%d" % d])
                                    P.dve(lambda e, pA=pA, d=d: e.tensor_tensor(out=Sacc[d][:], in0=Sacc[d][:], in1=pA[:, 256:512], op=ALU.add), r=[kA, "Sacc%d" % d], w=["Sacc%d" % d])
                                P.act(lambda e, d=d: e.activation(out=Sbf[d][:], in_=Sacc[d][:], func=AF.Copy), r=["Sacc%d" % d], w=["Sbf%d" % d])
                            emit_pe_warm(P, pb[7], onesb, hT, 3)
                        caps.append(cap)
                    P.add_interleaved(caps)
                for gi in range(2):
                    for e4 in range(4):
                        hv = Hm[gi][:, 0:17 * 256].rearrange("p (c v) -> p c v", v=256)[:, :, e4 * 64:(e4 + 1) * 64]
                        xv = [G[0], G[1]][gi][:, 0:17 * 256].rearrange("p (c v) -> p c v", v=256)[:, :, e4 * 64:(e4 + 1) * 64]
                        P.dve(lambda e, hv=hv, xv=xv, e4=e4, hf=hf: e.scalar_tensor_tensor(out=hv, in0=xv, scalar=sDs[:, 4 * hf + e4:4 * hf + e4 + 1], in1=hv, op0=ALU.mult, op1=ALU.add),
                              r=["G%d" % gi, "G%d" % (2 + gi), "sDs"], w=["G%d" % (2 + gi)])
                gate_out(wz[:, :, hf * 256:(hf + 1) * 256], AF.Silu, 8 + 4 * s + 2 * hf)

        for s_ in range(2):
            half(s_, drs[s_])
        P.emit()


def mix1_consts(I, s):
    import ml_dtypes
    W = I["cd_in_w"][0]
    h0, h1 = 2 * s, 2 * s + 1
    blocks = [np.arange(128 * h0, 128 * h0 + 128), 512 + np.arange(128 * h0, 128 * h0 + 128),
              np.arange(128 * h1, 128 * h1 + 128), 512 + np.arange(128 * h1, 128 * h1 + 128)]
    xoff = 4128
    blocks += [xoff + 512 * s + 128 * n + np.arange(128) for n in range(4)]
    blocks += [xoff + 1024 + 128 * s + np.arange(128), xoff + 1280 + 128 * s + np.arange(128)]
    d = {}
    d["wfm"] = np.stack([kblk(W[:, c]) for c in blocks], 0)
    d["wga"] = kblk(W[:, 3072:3104])
    d["wv"] = np.stack([kblk(W[:, 1024 + 256 * h:1024 + 256 * h + 256]) for h in (h0, h1)], 0)
    d["wr"] = kblk(W[:, 2048 + 512 * s:2048 + 512 * s + 512])
    d["wz"] = kblk(W[:, 3104 + 512 * s:3104 + 512 * s + 512])
    dtc = [5664 + dd * 16 + 8 * s + e for dd in range(2) for e in range(8)]
    d["wdt"] = kblk(W[:, dtc])
    al = np.zeros((64, 256), np.float32)
    for dd in range(2):
        al[32 * dd:32 * dd + 16] = I["g_alpha_w"][0][dd][:, 256 * s:256 * s + 256]
        al[32 * dd + 16] = I["g_alpha_b"][0][dd][256 * s:256 * s + 256]
    d["alph"] = al
    d["gnorm"] = np.ascontiguousarray(np.broadcast_to(I["g_norm_w"][0][512 * s:512 * s + 512][None, :], (128, 512)))
    cw = np.zeros((128, 6, 4), np.float32); cb = np.zeros((128, 6), np.float32)
    for bi in range(6):
        cols = blocks[4 + bi] - xoff
        cw[:, bi, :] = I["s_conv_w"][0][:, cols].T
        cb[:, bi] = I["s_conv_b"][0][cols]
    d["cw"] = cw; d["cb"] = cb
    bc = lambda v: np.ascontiguousarray(np.broadcast_to(np.asarray(v, np.float32)[None, :], (128, len(v))))
    d["dtb"] = bc([I["s_dt_bias"][0][dd, 8 * s + e] for dd in range(2) for e in range(8)])
    d["alog"] = bc([I["s_A_log"][0][dd, 8 * s + e] for dd in range(2) for e in range(8)])
    d["sD"] = bc(I["s_D"][0][8 * s:8 * s + 8])
    d["identf"] = np.eye(128, dtype=np.float32)
    return d


def build_fused():
    _PHASE[0] = 0
    nc = bass.Bass("TRN2", target_bir_lowering=False)
    mkdr = lambda sfx: (lambda name, shape, dt=F32: nc.dram_tensor(name + sfx, shape, dt, kind="ExternalInput").ap())
    xT = nc.dram_tensor("xT", [128, 8, T_ALL], F32, kind="ExternalInput").ap()
    msel = nc.dram_tensor("msel", [128, 2], F32, kind="ExternalInput").ap()
    xo = nc.dram_tensor("xo", [128, 8, 2048], F32, kind="ExternalOutput").ap()
    mix0s = nc.dram_tensor("mix0s", [128, 16, T_ALL], BF16, kind="Internal").ap()
    mix1s = nc.dram_tensor("mix1s", [128, 16, T_ALL], BF16, kind="Internal").ap()
    x1s = nc.dram_tensor("x1s", [128, 8, T_ALL], F32, kind="Internal").ap()
    with ExitStack() as es:
        P = Prog(nc, es)
        pb = [es.enter_context(nc.psum_tensor("pb%d" % i, [128, 512], F32)) for i in range(8)]
        phase_mix0(nc, P, pb, [mkdr("_m0s0"), mkdr("_m0s1")], xT, mix0s)
        phase_post(nc, P, pb, 0, mkdr("_p0"), TOK_TILES,
                   lambda t0, n: [xT[:, :, t0:t0 + n]], lambda t0, n: [mix0s[:, :, t0:t0 + n]], lambda t0, n: x1s[:, :, t0:t0 + n])
        phase_mix1(nc, P, pb, [mkdr("_m1s0"), mkdr("_m1s1")], x1s, mix1s)
        lat = lambda a, t0, n: [a[:, :, 256 + 2048 * r + t0:256 + 2048 * r + t0 + n] for r in range(2)]
        phase_post(nc, P, pb, 1, mkdr("_p1"), [(i * 512, 512, 0) for i in range(4)],
                   lambda t0, n: lat(x1s, t0, n), lambda t0, n: lat(mix1s, t0, n), lambda t0, n: xo[:, :, t0:t0 + n], msel_d=msel)
    return nc


def kernel(**I):
    I = {k: np.asarray(v) for k, v in I.items()}
    cores = list(range(8))
    shared = {}
    for h in range(2):
        d = dict(mix_common_consts(0, I)); d.update(mix0_consts(I, h))
        shared.update({k + "_m0s%d" % h: v for k, v in d.items()})
        d = dict(mix_common_consts(1, I)); d.update(mix1_consts(I, h))
        shared.update({k + "_m1s%d" % h: v for k, v in d.items()})
    shared.update({k + "_p0": v for k, v in post_consts(0, I).items()})
    shared.update({k + "_p1": v for k, v in post_consts(1, I).items()})
    in_maps = []
    for core in cores:
        b, s = core // 2, core % 2
        d = dict(shared)
        d["xT"] = stream_fm(I["x"], I["ctx"], b)
        cT = c_cols(I, b)
        for sfx in ("_m0s0", "_m0s1", "_m1s0", "_m1s1", "_p0", "_p1"):
            d["cT" + sfx] = cT
        m = np.zeros((128, 2), np.float32); m[:, s] = 1.0
        d["msel"] = m
        in_maps.append(d)
    res = run_bass_kernel_spmd(build_fused(), in_maps, core_ids=cores).results
    out = np.zeros((4, 4096, 1024), np.float32)
    for core in cores:
        b, s = core // 2, core % 2
        out[b, 2048 * s:2048 * s + 2048] = fm_inv(res[core]["xo"])
    return out
```

```python
import numpy as np
import concourse.bass as bass
import concourse.mybir as mybir
from concourse.bass_utils import run_bass_kernel_spmd
from concourse.alu_op_type import AluOpType as ALU
from contextlib import ExitStack

AF = mybir.ActivationFunctionType
F32 = mybir.dt.float32
BF16 = mybir.dt.bfloat16
AX = mybir.AxisListType

N_DMA_SLOTS = 40


class Prog:
    ENGS = ("pe", "act", "dve", "pool", "sp")

    def __init__(self, nc, es):
        self.nc = nc
        self.ops = []
        self.final = []
        self.esem = {e: es.enter_context(nc.semaphore("s_" + e)) for e in self.ENGS}
        self.dsem = [es.enter_context(nc.semaphore("d%d" % k)) for k in range(N_DMA_SLOTS)]
        self.cnt = {e: 0 for e in self.ENGS}
        self.dcnt = [0] * N_DMA_SLOTS
        self.nslot = 0

    SEG_KEYS = ("G0", "G1", "G2", "G3", "QK", "XB")

    @classmethod
    def _expand(cls, keys):
        out = []
        for k in keys:
            if k in cls.SEG_KEYS:
                out += [k + "/0", k + "/1"]
            else:
                out.append(k)
        return tuple(out)

    def op(self, eng, fn, r=(), w=(), dma=False):
        self.ops.append(dict(eng=eng, fn=fn, r=self._expand(r), w=self._expand(w), dma=dma))
        return len(self.ops) - 1

    def pe(self, fn, r=(), w=()):
        return self.op("pe", fn, r, w)

    def act(self, fn, r=(), w=()):
        return self.op("act", fn, r, w)

    def dve(self, fn, r=(), w=()):
        return self.op("dve", fn, r, w)

    def pool(self, fn, r=(), w=()):
        return self.op("pool", fn, r, w)

    def dma(self, q, out, in_, r=(), w=(), final=False):
        i = self.op(q, lambda e: e.dma_start(out=out, in_=in_), r, w, dma=True)
        return i

    class _Cap:
        def __init__(self, prog):
            self.prog = prog

        def __enter__(self):
            self.saved = self.prog.ops
            self.prog.ops = []
            self.ops = None
            return self

        def __exit__(self, *a):
            self.ops = self.prog.ops
            self.prog.ops = self.saved
            return False

    def capture(self):
        return Prog._Cap(self)

    def add_interleaved(self, caps):
        lists = [c.ops for c in caps]
        n = max(len(l) for l in lists)
        for j in range(n):
            for l in lists:
                if j < len(l):
                    self.ops.append(l[j])

    def emit(self):
        nc = self.nc
        ops = self.ops
        self.ops = []
        last_w = {}
        readers = {}
        slot_last = {}
        for i, o in enumerate(ops):
            deps = set()
            for k in o["r"]:
                if k in last_w:
                    deps.add(last_w[k])
                if k.startswith("pb"):
                    deps.update(j for j in readers.get(k, ()) if ops[j]["eng"] != o["eng"])
            for k in o["w"]:
                if k in last_w:
                    deps.add(last_w[k])
                deps.update(readers.get(k, ()))
            if o["dma"]:
                s = self.nslot % N_DMA_SLOTS
                self.nslot += 1
                o["slot"] = s
                if s in slot_last:
                    deps.add(slot_last[s])
                slot_last[s] = i
            deps.discard(i)
            if o["eng"] == "pe":
                deps = {d for d in deps if not (ops[d]["eng"] == "pe")}
            o["deps"] = deps
            for k in o["r"]:
                readers.setdefault(k, []).append(i)
            for k in o["w"]:
                last_w[k] = i
                readers[k] = []
        needed = set()
        for o in ops:
            needed.update(o["deps"])
        esem, dsem, cnt, dcnt = self.esem, self.dsem, self.cnt, self.dcnt
        for i, o in enumerate(ops):
            if o["dma"]:
                dcnt[o["slot"]] += 16
                o["tok"] = (("d", o["slot"]), dsem[o["slot"]], dcnt[o["slot"]])
            elif i in needed:
                cnt[o["eng"]] += 1
                o["tok"] = (("e", o["eng"]), esem[o["eng"]], cnt[o["eng"]])
            else:
                o["tok"] = None
        dfinal = list(dcnt)
        with nc.Block() as block:
            def run(engname, eng):
                waited = {}
                for i, o in enumerate(ops):
                    if o["eng"] != engname:
                        continue
                    need = {}
                    for d in o["deps"]:
                        key, sem, val = ops[d]["tok"]
                        if waited.get(key, 0) < val:
                            if need.get(key, (None, 0))[1] < val:
                                need[key] = (sem, val)
                    for key, (sem, val) in need.items():
                        eng.wait_ge(sem, val)
                        waited[key] = val
                    inst = o["fn"](eng)
                    if o["tok"] is not None:
                        inst.then_inc(o["tok"][1], 16 if o["dma"] else 1)
                if engname == "sp":
                    for k in range(N_DMA_SLOTS):
                        if dfinal[k] > 0:
                            eng.wait_ge(dsem[k], dfinal[k])

            @block.tensor
            def _(e):
                run("pe", e)

            @block.scalar
            def _(e):
                run("act", e)

            @block.vector
            def _(e):
                run("dve", e)

            @block.gpsimd
            def _(e):
                run("pool", e)

            @block.sync
            def _(e):
                run("sp", e)


D = 1024
EPS = 1e-6


_PHASE = [0]


def _mk(nc, es):
    _PHASE[0] += 1
    pfx = "p%d_" % _PHASE[0]
    sb = lambda name, shape, dt=F32: es.enter_context(nc.sbuf_tensor(pfx + name, shape, dt))
    ps = lambda name, shape, dt=F32: es.enter_context(nc.psum_tensor(pfx + name, shape, dt))
    return sb, ps


def emit_pe_warm(P, pbw, onesb, hT, n):
    for _ in range(n):
        P.pe(lambda e: e.matmul(pbw[:, 0:512], lhsT=onesb[:], rhs=hT[:, 0, 0:512], start=True, stop=True), r=["onesb", "hT"], w=["pb7"])


def emit_tok2fm(P, pb7, identb, ob, obkey, obT, obTkey, dst_ap):
    pT = pb7[:].bitcast(BF16)
    for cc in range(2):
        for chb in range(2):
            q = chb * 2 + cc
            P.pe(lambda e, cc=cc, chb=chb, q=q: e.transpose(pT[:, q * 128:(q + 1) * 128], ob[:, cc * 256 + chb * 128:cc * 256 + (chb + 1) * 128], identb[:]),
                 r=[obkey, "identb"], w=["pb7"])
    P.act(lambda e: e.activation(out=obT[:], in_=pT[:, 0:512], func=AF.Copy), r=["pb7"], w=[obTkey])
    P.dma("sp", dst_ap, obT[:].rearrange("p (b t) -> p b t", b=2), r=[obTkey])


def emit_modulation(P, nc, sb, pb7, modw, modb, cT, nch, mwbufs):
    cs = sb("cs", [128, 8, 2]); csr = sb("csr", [128, 8, 2]); mb = sb("mb", [128, nch]); modT = sb("modT", [128, nch, 2])
    mw = [b for b, _ in mwbufs]; mwk = [k for _, k in mwbufs]
    P.dma("sp", csr[:], cT, w=["csr"])
    P.dma("sp", mb[:], modb, w=["mb"])
    P.act(lambda e: e.activation(out=cs[:], in_=csr[:], func=AF.Silu), r=["csr"], w=["cs"])
    for ch in range(nch):
        s = ch % len(mw)
        P.dma("sp", mw[s], modw[ch], w=mwk[s])
        for k in range(8):
            P.pe(lambda e, s=s, k=k, ch=ch: e.matmul(pb7[:, 2 * ch:2 * ch + 2], lhsT=mw[s][:, k, :], rhs=cs[:, k, :],
                                                     start=(k == 0), stop=(k == 7)),
                 r=mwk[s] + ["cs"], w=["pb7"])
    P.dve(lambda e: e.tensor_tensor(out=modT[:], in0=pb7[:, 0:2 * nch].rearrange("p (c t) -> p c t", t=2),
                                    in1=mb[:].unsqueeze(2).to_broadcast([128, nch, 2]), op=ALU.add),
          r=["pb7", "mb"], w=["modT"])
    return modT


def phase_post(nc, P, pb, layer, dr, tiles, xsrc, mixsrc, dst, msel_d=None):
    moe = layer == 1
    NJ = 28 if moe else 22
    NE = 8 if moe else 1
    outw = dr("outw", [128, 16, 1024]); w1 = dr("w1", [NE * NJ, 128, 8, 256]); w2 = dr("w2", [NE * 8, 128, NJ, 128])
    modw = dr("modw", [32, 128, 8, 128]); modb = dr("modb", [128, 32]); cT = dr("cT", [128, 8, 2]); gT = dr("gT", [128, 3, 8])
    if moe:
        routw = dr("routw", [128, 8, 8]); routb = dr("routb", [128, 8]); identd = dr("ident", [128, 128]); seld = dr("sel", [8, 1024])
        snwd = dr("snw", [128, 8])
    with ExitStack() as es:
        sb, ps = _mk(nc, es)
        ow = sb("ow", [128, 16, 1024], BF16); mx = sb("mx", [128, 16, 512], BF16); xl = sb("xl", [128, 8, 512])
        y = sb("y", [128, 8, 512]); sq = sb("sq", [128, 8, 512], BF16); h2 = sb("h2", [128, 8, 512], BF16)
        hid = sb("hid", [128, NJ, 512], BF16)
        NW1, NW2 = (3, 3) if moe else (6, 6)
        w1b = [sb("w1b%d" % i, [128, 8, 256], BF16)[:] for i in range(NW1)]
        w2b = [sb("w2b%d" % i, [128, NJ, 128], BF16)[:] for i in range(NW2)]
        w1k = [["w1b%d" % i] for i in range(NW1)]; w2k = [["w2b%d" % i] for i in range(NW2)]
        tmp = [sb("tmp%d" % i, [128, 512]) for i in range(2)]
        sg = [sb("sg%d" % i, [128, 512]) for i in range(2)]
        rstd = sb("rstd", [128, 512]); gsb = sb("gsb", [128, 3, 8])
        onesb = sb("onesb", [128, 128], BF16); epsc = sb("epsc", [128, 1])
        A1 = sb("A1", [128, 8, 2]); B2 = sb("B2", [128, 8, 2]); A3 = sb("A3", [128, 8, 2])
        if moe:
            h2f = sb("h2f", [128, 8, 512]); Gb = sb("Gb", [128, 8, 512]); rw = sb("rw", [128, 8, 8]); rb = sb("rb", [128, 8])
            ident = sb("ident_sb", [128, 128]); sel = sb("sel_sb", [8, 1024]); Lg = sb("Lg", [128, 4, 8]); mx8 = sb("mx8", [128, 4, 8])
            msk = sb("msk", [128, 4, 8]); Eg = sb("Eg", [128, 4, 8]); den = sb("den", [128, 4]); gTs = sb("gTs", [8, 512])
            snw = sb("snw_sb", [128, 8])
            for i in range(2):
                v = h2f[:, 2 * i:2 * i + 2, :].rearrange("p a b -> p (a b)").bitcast(BF16).rearrange("p (k c) -> p k c", c=256)
                w1b.append(v); w1k.append(["h2f%d" % (2 * i), "h2f%d" % (2 * i + 1)])
            v = h2f[:, 4:8, :].rearrange("p a b -> p (a b)").bitcast(BF16)[:, 0:NJ * 128].rearrange("p (j c) -> p j c", c=128)
            w2b.append(v); w2k.append(["h2f%d" % m for m in range(4, 8)])
            NW1 = len(w1b); NW2 = len(w2b)
            P.dma("sp", snw[:], snwd, w=["snw"])
            P.dma("sp", rw[:], routw, w=["rw"]); P.dma("sp", rb[:], routb, w=["rb"])
            P.dma("sp", ident[:], identd, w=["ident"]); P.dma("sp", sel[:], seld, w=["sel"])
        P.dve(lambda e: e.memset(onesb[:], 1.0), w=["onesb"])
        P.dve(lambda e: e.memset(epsc[:], EPS), w=["epsc"])
        P.dma("sp", gsb[:], gT, w=["gsb"])
        if msel_d is not None:
            msel = sb("msel_sb", [128, 2])
            P.dma("sp", msel[:], msel_d, w=["msel"])
        P.dma("pool", ow[:], outw, w=["ow"])
        mwb = [(y[:, 2 * i:2 * i + 2, :].rearrange("p a (b c) -> p (a b) c", c=128), ["y%d" % (2 * i), "y%d" % (2 * i + 1)]) for i in range(4)]
        modT = emit_modulation(P, nc, sb, pb[7], modw, modb, cT, 32, mwb)
        gbc = lambda i: gsb[:, i, :].unsqueeze(2).to_broadcast([128, 8, 2])
        P.dve(lambda e: e.tensor_tensor(out=A1[:], in0=modT[:, 0:8, :], in1=gbc(0), op=ALU.mult), r=["modT", "gsb"], w=["A1"])
        P.dve(lambda e: e.scalar_tensor_tensor(out=B2[:], in0=modT[:, 16:24, :], scalar=1.0, in1=gbc(1), op0=ALU.add, op1=ALU.mult),
              r=["modT", "gsb"], w=["B2"])
        P.dve(lambda e: e.tensor_tensor(out=A3[:], in0=modT[:, 24:32, :], in1=gbc(2), op=ALU.mult), r=["modT", "gsb"], w=["A3"])
        w1cnt = [0]; w2cnt = [0]

        def stats(n, srckey):
            for m in range(8):
                P.pe(lambda e, m=m: e.matmul(pb[6][:, :n], lhsT=onesb[:], rhs=sq[:, m, :n], start=(m == 0), stop=(m == 7)),
                     r=["onesb", "sq%d" % m], w=["pb6"])
            P.act(lambda e: e.activation(out=rstd[:, :n], in_=pb[6][:, :n], func=AF.Sqrt, bias=epsc[:, 0:1], scale=1.0 / D),
                  r=["pb6", "epsc"], w=["rstd"])
            P.dve(lambda e: e.reciprocal(out=rstd[:, :n], in_=rstd[:, :n]), r=["rstd"], w=["rstd"])

        def resid_add(n, col, A, Akey):
            for m in range(8):
                t = tmp[m % 2]; tk = "tmp%d" % (m % 2)
                P.dve(lambda e, m=m, t=t: e.scalar_tensor_tensor(out=t[:, :n], in0=y[:, m, :n], scalar=A[:, m, col:col + 1], in1=rstd[:, :n],
                                                                   op0=ALU.mult, op1=ALU.mult), r=["y%d" % m, Akey, "rstd"], w=[tk])
                P.dve(lambda e, m=m, t=t: e.tensor_tensor(out=xl[:, m, :n], in0=xl[:, m, :n], in1=t[:, :n], op=ALU.add),
                      r=[tk, "xl%d" % m], w=["xl%d" % m])

        def do_tile(t0, n, col):
            xs_ = xsrc(t0, n); ms_ = mixsrc(t0, n)
            xlk = ["xl%d" % m for m in range(8)]; yk = ["y%d" % m for m in range(8)]; hk16 = ["hid%d" % j for j in range(16)]
            P.dma("sp", xl[:, :, :n], xs_[0], w=xlk)
            P.dma("sp", mx[:, :, :n], ms_[0], w=["mx"])
            if len(xs_) == 2:
                P.dma("sp", y[:, :, :n], xs_[1], w=yk)
                P.dma("sp", hid[:, 0:16, :n], ms_[1], w=hk16)
                P.dve(lambda e: e.tensor_scalar(out=xl[:, :, :n], in0=xl[:, :, :n], scalar1=msel[:, 0:1], scalar2=None, op0=ALU.mult), r=xlk + ["msel"], w=xlk)
                P.dve(lambda e: e.scalar_tensor_tensor(out=xl[:, :, :n], in0=y[:, :, :n], scalar=msel[:, 1:2], in1=xl[:, :, :n], op0=ALU.mult, op1=ALU.add),
                      r=xlk + yk + ["msel"], w=xlk)
                P.dve(lambda e: e.tensor_scalar(out=mx[:, :, :n], in0=mx[:, :, :n], scalar1=msel[:, 0:1], scalar2=None, op0=ALU.mult), r=["mx", "msel"], w=["mx"])
                P.dve(lambda e: e.scalar_tensor_tensor(out=mx[:, :, :n], in0=hid[:, 0:16, :n], scalar=msel[:, 1:2], in1=mx[:, :, :n], op0=ALU.mult, op1=ALU.add),
                      r=["mx", "msel"] + hk16, w=["mx"])
            if moe:
                for m in range(8):
                    P.act(lambda e, m=m: e.activation(out=sq[:, m, :n], in_=mx[:, 8 + m, :n], func=AF.Square), r=["mx"], w=["sq%d" % m])
                stats(n, "ssd")
                for m in range(8):
                    P.dve(lambda e, m=m: e.scalar_tensor_tensor(out=mx[:, 8 + m, :n], in0=mx[:, 8 + m, :n], scalar=snw[:, m:m + 1], in1=rstd[:, :n],
                                                                 op0=ALU.mult, op1=ALU.mult), r=["mx", "snw", "rstd"], w=["mx"])
            for m in range(8):
                pbm = pb[m % 2]; pk = "pb%d" % (m % 2)
                for k in range(16):
                    P.pe(lambda e, m=m, k=k, pbm=pbm: e.matmul(pbm[:, :n], lhsT=ow[:, k, m * 128:(m + 1) * 128], rhs=mx[:, k, :n],
                                                               start=(k == 0), stop=(k == 15)), r=["ow", "mx"], w=[pk])
                P.act(lambda e, m=m, pbm=pbm: e.activation(out=y[:, m, :n], in_=pbm[:, :n], func=AF.Copy), r=[pk], w=["y%d" % m])
                P.act(lambda e, m=m, pbm=pbm: e.activation(out=sq[:, m, :n], in_=pbm[:, :n], func=AF.Square), r=[pk], w=["sq%d" % m])
            stats(n, "y")
            resid_add(n, col, A1, "A1")
            for m in range(8):
                P.act(lambda e, m=m: e.activation(out=sq[:, m, :n], in_=xl[:, m, :n], func=AF.Square), r=["xl%d" % m], w=["sq%d" % m])
            stats(n, "xl")
            for m in range(8):
                t = tmp[m % 2]; tk = "tmp%d" % (m % 2)
                P.dve(lambda e, m=m, t=t: e.scalar_tensor_tensor(out=t[:, :n], in0=xl[:, m, :n], scalar=B2[:, m, col:col + 1], in1=rstd[:, :n],
                                                                   op0=ALU.mult, op1=ALU.mult), r=["xl%d" % m, "B2", "rstd"], w=[tk])
                if moe:
                    P.act(lambda e, m=m, t=t: e.activation(out=h2f[:, m, :n], in_=t[:, :n], func=AF.Identity, bias=modT[:, 8 + m, col:col + 1], scale=1.0),
                          r=[tk, "modT"], w=["h2f%d" % m])
                    P.dve(lambda e, m=m: e.tensor_copy(out=h2[:, m, :n], in_=h2f[:, m, :n]), r=["h2f%d" % m], w=["h2_%d" % m])
                else:
                    P.act(lambda e, m=m, t=t: e.activation(out=h2[:, m, :n], in_=t[:, :n], func=AF.Identity, bias=modT[:, 8 + m, col:col + 1], scale=1.0),
                          r=[tk, "modT"], w=["h2_%d" % m])
            h2keys = ["h2_%d" % m for m in range(8)]
            if moe:
                for s4 in range(4):
                    for k in range(8):
                        P.pe(lambda e, s4=s4, k=k: e.matmul(pb[7][:, s4 * 8:(s4 + 1) * 8], lhsT=h2f[:, k, s4 * 128:(s4 + 1) * 128], rhs=rw[:, k, :],
                                                            start=(k == 0), stop=(k == 7)), r=["h2f%d" % k, "rw"], w=["pb7"])
                P.dve(lambda e: e.tensor_tensor(out=Lg[:], in0=pb[7][:, 0:32].rearrange("p (s e) -> p s e", e=8),
                                                in1=rb[:].unsqueeze(1).to_broadcast([128, 4, 8]), op=ALU.add), r=["pb7", "rb"], w=["Lg"])
                for s4 in range(4):
                    P.dve(lambda e, s4=s4: e.max(out=mx8[:, s4, :], in_=Lg[:, s4, :]), r=["Lg"], w=["mx8"])
                P.dve(lambda e: e.tensor_tensor(out=msk[:], in0=Lg[:], in1=mx8[:, :, 1:2].to_broadcast([128, 4, 8]), op=ALU.is_ge),
                      r=["Lg", "mx8"], w=["msk"])
                P.dve(lambda e: e.tensor_tensor(out=Eg[:], in0=Lg[:], in1=mx8[:, :, 0:1].to_broadcast([128, 4, 8]), op=ALU.subtract),
                      r=["Lg", "mx8"], w=["Eg"])
                P.act(lambda e: e.activation(out=Eg[:], in_=Eg[:], func=AF.Exp), r=["Eg"], w=["Eg"])
                P.dve(lambda e: e.tensor_tensor(out=Eg[:], in0=Eg[:], in1=msk[:], op=ALU.mult), r=["Eg", "msk"], w=["Eg"])
                P.dve(lambda e: e.tensor_reduce(out=den[:], in_=Eg[:], axis=AX.X, op=ALU.add), r=["Eg"], w=["den"])
                P.dve(lambda e: e.reciprocal(out=den[:], in_=den[:]), r=["den"], w=["den"])
                P.dve(lambda e: e.tensor_tensor(out=Eg[:], in0=Eg[:], in1=den[:].unsqueeze(2).to_broadcast([128, 4, 8]), op=ALU.mult),
                      r=["Eg", "den"], w=["Eg"])
                for s4 in range(4):
                    P.pe(lambda e, s4=s4: e.transpose(pb[6][0:8, s4 * 128:(s4 + 1) * 128], Eg[:, s4, :], ident[:]), r=["Eg", "ident"], w=["pb6"])
                P.dve(lambda e: e.tensor_copy(out=gTs[:], in_=pb[6][0:8, :]), r=["pb6"], w=["gTs"])
                for ex in range(8):
                    pbm = pb[ex % 2]; pk = "pb%d" % (ex % 2)
                    P.pe(lambda e, ex=ex, pbm=pbm: e.matmul(pbm[:, :n], lhsT=sel[0:8, ex * 128:(ex + 1) * 128], rhs=gTs[0:8, :n], start=True, stop=True),
                         r=["sel", "gTs"], w=[pk])
                    P.act(lambda e, ex=ex, pbm=pbm: e.activation(out=Gb[:, ex, :n], in_=pbm[:, :n], func=AF.Copy), r=[pk], w=["Gb%d" % ex])
            for ex in range(NE):
                for j in range(NJ):
                    s = w1cnt[0] % NW1; w1cnt[0] += 1
                    P.dma("pool", w1b[s], w1[ex * NJ + j], w=w1k[s])
                    for half in range(2):
                        pbi = 2 + 2 * (j % 2) + half
                        for k in range(8):
                            P.pe(lambda e, s=s, k=k, half=half, pbi=pbi: e.matmul(pb[pbi][:, :n], lhsT=w1b[s][:, k, half * 128:(half + 1) * 128],
                                                                                   rhs=h2[:, k, :n], start=(k == 0), stop=(k == 7)),
                                 r=w1k[s] + ["h2_%d" % k], w=["pb%d" % pbi])
                    pg = 2 + 2 * (j % 2)
                    P.act(lambda e, j=j, pg=pg: e.activation(out=sg[j % 2][:, :n], in_=pb[pg][:, :n], func=AF.Silu), r=["pb%d" % pg], w=["sg%d" % (j % 2)])
                    P.dve(lambda e, j=j, pg=pg: e.tensor_tensor(out=hid[:, j, :n], in0=sg[j % 2][:, :n], in1=pb[pg + 1][:, :n], op=ALU.mult),
                          r=["sg%d" % (j % 2), "pb%d" % (pg + 1)], w=["hid%d" % j])
                for m in range(8):
                    s = w2cnt[0] % NW2; w2cnt[0] += 1
                    P.dma("pool", w2b[s], w2[ex * 8 + m], w=w2k[s])
                    pbm = pb[m % 2]; pk = "pb%d" % (m % 2)
                    for j in range(NJ):
                        P.pe(lambda e, s=s, j=j, pbm=pbm: e.matmul(pbm[:, :n], lhsT=w2b[s][:, j, :], rhs=hid[:, j, :n], start=(j == 0), stop=(j == NJ - 1)),
                             r=w2k[s] + ["hid%d" % j], w=[pk])
                    if not moe:
                        P.act(lambda e, m=m, pbm=pbm: e.activation(out=y[:, m, :n], in_=pbm[:, :n], func=AF.Copy), r=[pk], w=["y%d" % m])
                        P.act(lambda e, m=m, pbm=pbm: e.activation(out=sq[:, m, :n], in_=pbm[:, :n], func=AF.Square), r=[pk], w=["sq%d" % m])
                    elif ex == 0:
                        P.dve(lambda e, m=m, pbm=pbm, ex=ex: e.tensor_tensor(out=y[:, m, :n], in0=pbm[:, :n], in1=Gb[:, ex, :n], op=ALU.mult),
                              r=[pk, "Gb%d" % ex], w=["y%d" % m])
                    else:
                        t = tmp[m % 2]; tk = "tmp%d" % (m % 2)
                        P.dve(lambda e, m=m, pbm=pbm, ex=ex, t=t: e.tensor_tensor(out=t[:, :n], in0=pbm[:, :n], in1=Gb[:, ex, :n], op=ALU.mult),
                              r=[pk, "Gb%d" % ex], w=[tk])
                        P.dve(lambda e, m=m, t=t: e.tensor_tensor(out=y[:, m, :n], in0=y[:, m, :n], in1=t[:, :n], op=ALU.add),
                              r=[tk, "y%d" % m], w=["y%d" % m])
                        if ex == NE - 1:
                            P.act(lambda e, m=m: e.activation(out=sq[:, m, :n], in_=y[:, m, :n], func=AF.Square), r=["y%d" % m], w=["sq%d" % m])
            stats(n, "f")
            resid_add(n, col, A3, "A3")
            P.dma("sp", dst(t0, n), xl[:, :, :n], r=["xl%d" % m for m in range(8)], final=True)
        for (t0_, n_, col_) in tiles:
            do_tile(t0_, n_, col_)
        P.emit()


def fm(a):
    T, C = a.shape
    return np.ascontiguousarray(a.T.reshape(C // 128, 128, T).transpose(1, 0, 2))


def fm_inv(b):
    p, kc, T = b.shape
    return np.ascontiguousarray(b.transpose(2, 1, 0).reshape(T, kc * 128))


def kblk(w):
    K, N = w.shape
    return np.ascontiguousarray(w.reshape(K // 128, 128, N).transpose(1, 0, 2))


def w1blk(w, nj):
    return np.ascontiguousarray(w.reshape(8, 128, 2, nj, 128).transpose(3, 1, 0, 2, 4).reshape(nj, 128, 8, 256))


def w2blk(w, nj):
    return np.ascontiguousarray(w.reshape(nj, 128, 8, 128).transpose(2, 1, 0, 3))


def modblk(w, c0, c1):
    n = (c1 - c0) // 128
    return np.ascontiguousarray(w[:, c0:c1].reshape(8, 128, n, 128).transpose(2, 1, 0, 3))


def colvec(v):
    return np.ascontiguousarray(v.reshape(-1, 128).T)


def post_consts(layer, I):
    moe = layer == 1
    d = {}
    if moe:
        d["outw"] = kblk(I["cd_out_w"][0])
        d["w1"] = np.concatenate([w1blk(I["moe_w1"][0, e], 28) for e in range(8)], axis=0)
        d["w2"] = np.concatenate([w2blk(I["moe_w2"][0, e], 28) for e in range(8)], axis=0)
        d["routw"] = kblk(I["router_w"][0])
        d["routb"] = np.ascontiguousarray(np.broadcast_to(I["router_b"][0][None, :], (128, 8)))
        d["ident"] = np.eye(128, dtype=np.float32)
        sel = np.zeros((8, 1024), np.float32)
        for e in range(8):
            sel[e, e * 128:(e + 1) * 128] = 1.0
        d["sel"] = sel
        d["snw"] = colvec(I["s_norm_w"][0])
    else:
        d["outw"] = kblk(I["ab_out_w"][0])
        d["w1"] = w1blk(I["ffn_w1"][0], 22)
        d["w2"] = w2blk(I["ffn_w2"][0], 22)
    d["modw"] = modblk(I["mod_w"][layer], 2048, 6144)
    d["modb"] = colvec(I["mod_b"][layer][2048:6144])
    d["gT"] = np.ascontiguousarray(I["norm_g"][layer][1:4].reshape(3, 8, 128).transpose(2, 0, 1))
    return d


def c_cols(I, b):
    return np.ascontiguousarray(np.stack([I["c"][b], I["c_ctx"]], 0).reshape(2, 8, 128).transpose(2, 1, 0))


T_ALL = 4352
NCH = 34
ORDER_F = list(range(NCH))
ORDER_B = [1, 0] + list(range(NCH - 1, 1, -1))
TOK_TILES = [(0, 256, 1)] + [(256 + 512 * i, 512, 0) for i in range(8)]
LN_DK = float(np.log(128.0 ** -0.5))


def emit_adaln_in(P, nc, sb, pb, xT, modT, g0sb, hT, xin, xinkey, sq, sqkey, tmp, rstd, onesb, epsc, A0):
    P.dve(lambda e: e.scalar_tensor_tensor(out=A0[:], in0=modT[:, 8:16, :], scalar=1.0, in1=g0sb[:].unsqueeze(2).to_broadcast([128, 8, 2]),
                                           op0=ALU.add, op1=ALU.mult), r=["modT", "g0sb"], w=["A0"])

    def tile(t0, n, col):
        P.dma("sp", xin[:, :, :n], xT[:, :, t0:t0 + n], w=[xinkey])
        for k in range(8):
            P.act(lambda e, k=k: e.activation(out=sq[:, k, :n], in_=xin[:, k, :n], func=AF.Square), r=[xinkey], w=[sqkey + str(k)])
        for k in range(8):
            P.pe(lambda e, k=k: e.matmul(pb[6][:, :n], lhsT=onesb[:], rhs=sq[:, k, :n], start=(k == 0), stop=(k == 7)),
                 r=["onesb", sqkey + str(k)], w=["pb6"])
        P.act(lambda e: e.activation(out=rstd[:, :n], in_=pb[6][:, :n], func=AF.Sqrt, bias=epsc[:, 0:1], scale=1.0 / D), r=["pb6", "epsc"], w=["rstd"])
        P.dve(lambda e: e.reciprocal(out=rstd[:, :n], in_=rstd[:, :n]), r=["rstd"], w=["rstd"])
        for k in range(8):
            t = tmp[k % 2]; tk = "tmp%d" % (k % 2)
            P.dve(lambda e, k=k, t=t: e.scalar_tensor_tensor(out=t[:, :n], in0=xin[:, k, :n], scalar=A0[:, k, col:col + 1], in1=rstd[:, :n],
                                                               op0=ALU.mult, op1=ALU.mult), r=[xinkey, "A0", "rstd"], w=[tk])
            P.act(lambda e, k=k, t=t: e.activation(out=hT[:, k, t0:t0 + n], in_=t[:, :n], func=AF.Identity, bias=modT[:, k, col:col + 1], scale=1.0),
                  r=[tk, "modT"], w=["hT"])
    for (t0, n, col) in TOK_TILES:
        tile(t0, n, col)


def emit_conv(P, src, skey, dst, dkey, wcol, bcol, wkeys):
    P.dve(lambda e: e.tensor_scalar(out=dst[:, :], in0=src[:, :], scalar1=wcol[:, 2:3], scalar2=bcol, op0=ALU.mult, op1=ALU.add),
          r=[skey] + wkeys, w=[dkey])
    views = [(lambda a: a[:, 0:256].rearrange("p (r w) -> p r w", w=256), 256),
             (lambda a: a[:, 256:T_ALL].rearrange("p (r w) -> p r w", w=64), 64)]
    for (j, d) in [(0, -2), (1, -1), (3, 1)]:
        for vf, W in views:
            sv = vf(src); dv = vf(dst)
            if d < 0:
                o_ = dv[:, :, -d:W]; i_ = sv[:, :, 0:W + d]
            else:
                o_ = dv[:, :, 0:W - d]; i_ = sv[:, :, d:W]
            P.dve(lambda e, o_=o_, i_=i_, j=j: e.scalar_tensor_tensor(out=o_, in0=i_, scalar=wcol[:, j:j + 1], in1=o_, op0=ALU.mult, op1=ALU.add),
                  r=[skey, dkey] + wkeys, w=[dkey])


SEG_SPLIT = 2304
SEGS = [(0, SEG_SPLIT), (SEG_SPLIT, T_ALL)]


def sk(key, t0):
    return key + ("/0" if t0 < SEG_SPLIT else "/1")


def emit_conv_seg(P, src, skey, dst, dkey, wcol, bcol, wkeys, seg):
    a, b = SEGS[seg]
    sk_, dk_ = skey + "/%d" % seg, dkey + "/%d" % seg
    P.dve(lambda e: e.tensor_scalar(out=dst[:, a:b], in0=src[:, a:b], scalar1=wcol[:, 2:3], scalar2=bcol, op0=ALU.mult, op1=ALU.add),
          r=[sk_] + wkeys, w=[dk_])
    if seg == 0:
        views = [(lambda t: t[:, 0:256].rearrange("p (r w) -> p r w", w=256), 256),
                 (lambda t: t[:, 256:SEG_SPLIT].rearrange("p (r w) -> p r w", w=64), 64)]
    else:
        views = [(lambda t: t[:, SEG_SPLIT:T_ALL].rearrange("p (r w) -> p r w", w=64), 64)]
    for (j, d) in [(0, -2), (1, -1), (3, 1)]:
        for vf, W in views:
            sv = vf(src); dv = vf(dst)
            if d < 0:
                o_ = dv[:, :, -d:W]; i_ = sv[:, :, 0:W + d]
            else:
                o_ = dv[:, :, 0:W - d]; i_ = sv[:, :, d:W]
            P.dve(lambda e, o_=o_, i_=i_, j=j: e.scalar_tensor_tensor(out=o_, in0=i_, scalar=wcol[:, j:j + 1], in1=o_, op0=ALU.mult, op1=ALU.add),
                  r=[sk_, dk_] + wkeys, w=[dk_])


def phase_mix0(nc, P, pb, drs, xT, out_mix):
    dr = drs[0]
    modw = dr("modw", [16, 128, 8, 128])
    modb = dr("modb", [128, 16])
    cT = dr("cT", [128, 8, 2])
    g0 = dr("g0", [128, 8])
    triUd = dr("triU", [128, 128])
    triLd = dr("triL", [128, 128])
    identd = dr("identb", [128, 128], BF16)
    with ExitStack() as es:
        sb, ps = _mk(nc, es)
        obT = sb("obT", [128, 512], BF16)
        hT = sb("hT", [128, 8, T_ALL], BF16)
        G = [sb("G%d" % i, [128, T_ALL]) for i in range(4)]
        QK = sb("QK", [128, T_ALL]); XB = sb("XB", [128, T_ALL], BF16)
        qkb = QK[:].bitcast(BF16)
        qT = qkb[:, 0:T_ALL]; kT = qkb[:, T_ALL:2 * T_ALL]
        VT = G[1][:].bitcast(BF16)
        vtok = VT.rearrange("p (c v) -> p c v", v=256)
        ktok = G[0][:].bitcast(BF16)[:, 0:T_ALL].rearrange("p (c d) -> p c d", d=128)
        tmp = [sb("tmp%d" % i, [128, 512]) for i in range(2)]
        rstd = sb("rstd", [128, 512]); onesb = sb("onesb", [128, 128], BF16); onesf = sb("onesf", [128, 128]); epsc = sb("epsc", [128, 1])
        g0sb = sb("g0sb", [128, 8]); A0 = sb("A0", [128, 8, 2])
        triU = sb("triU_sb", [128, 128]); triL = sb("triL_sb", [128, 128]); identb = sb("identb_sb", [128, 128], BF16)
        wfb = [sb("wfb%d" % i, [128, 8, 128], BF16) for i in range(4)]
        wvb = sb("wvb", [128, 8, 256], BF16); wgb = sb("wgb", [128, 8, 8], BF16)
        gbs = sb("gbs", [128, 8]); cws = sb("cws", [128, 8, 4]); cbs = sb("cbs", [128, 8]); mns = sb("mns", [128, 512])
        lwab = sb("lwab", [128, 2, 4, 128], BF16); lwxb = sb("lwxb", [128, 2, 4, 128], BF16)
        lbas = sb("lbas", [128, 2, 4]); lbxs = sb("lbxs", [128, 2, 4]); lams = sb("lams", [128, 2, 4]); LC = sb("LC", [128, 2, 4])
        GA = sb("GA", [128, NCH, 8]); Lsp = sb("Lsp", [128, NCH, 4]); EX = sb("EX", [128, 4, NCH, 2]); WV = sb("WV", [128, 2, NCH, 2])
        Sacc = [sb("Sacc%d" % i, [128, 257]) for i in range(2)]; Sbf = [sb("Sbf%d" % i, [128, 257], BF16) for i in range(2)]
        PT = [sb("PT%d" % i, [128, 128], BF16) for i in range(2)]; vs = [sb("vs%d" % i, [128, 257], BF16) for i in range(4)]
        dn = [sb("dn%d" % i, [128, 4]) for i in range(2)]
        ssq = sb("ssq", [128, NCH]); osig = tmp; obf = [sb("obf%d" % i, [128, 512], BF16) for i in range(2)]
        hlb = obf
        for (dst, src, key) in [(g0sb, g0, "g0sb"), (triU, triUd, "triU"), (triL, triLd, "triL"), (identb, identd, "identb")]:
            P.dma("sp", dst[:], src, w=[key])
        P.dve(lambda e: e.memset(onesb[:], 1.0), w=["onesb"])
        P.dve(lambda e: e.memset(onesf[:], 1.0), w=["onesf"])
        P.dve(lambda e: e.memset(epsc[:], EPS), w=["epsc"])
        mwb = [(G[1][:, 1024 * i:1024 * (i + 1)].rearrange("p (a b) -> p a b", b=128), ["mwst%d" % i]) for i in range(4)]
        modT = emit_modulation(P, nc, sb, pb[7], modw, modb, cT, 16, mwb)
        xin = G[0][:, 0:4096].rearrange("p (k n) -> p k n", k=8)
        sqv = XB[:, 0:4096].rearrange("p (k n) -> p k n", k=8)
        emit_adaln_in(P, nc, sb, pb, xT, modT, g0sb, hT, xin, "G0", sqv, "XB", tmp, rstd, onesb, epsc, A0)
        def half(s, dr):
            wfm = dr("wfm", [12, 128, 8, 128])
            wv = dr("wv", [2, 128, 8, 256])
            wo = dr("wo", [128, 8, 512])
            wg = dr("wg", [128, 8, 8])
            gbias = dr("gbias", [128, 8])
            cw = dr("cw", [128, 8, 4])
            cb = dr("cb", [128, 8])
            mnorm = dr("mnorm", [128, 512])
            lwa = dr("lwa", [128, 2, 4, 128])
            lwx = dr("lwx", [128, 2, 4, 128])
            lba = dr("lba", [128, 2, 4])
            lbx = dr("lbx", [128, 2, 4])
            llam = dr("llam", [128, 2, 4])
            for (dst, src, key) in [(gbs, gbias, "gbs"), (cws, cw, "cws"), (cbs, cb, "cbs"), (mns, mnorm, "mns"), (lbas, lba, "lbas"), (lbxs, lbx, "lbxs"), (lams, llam, "lams")]:
                P.dma("sp", dst[:], src, w=[key])
            for (dst, src, key) in [(wgb, wg, "wgb"), (lwab, lwa, "lwab"), (lwxb, lwx, "lwxb")]:
                P.dma("pool", dst[:], src, w=[key])
            P.act(lambda e: e.activation(out=LC[:], in_=lams[:], func=AF.Exp, scale=-1.0), r=["lams"], w=["LC"])
            P.act(lambda e: e.activation(out=LC[:], in_=LC[:], func=AF.Ln, bias=1.0), r=["LC"], w=["LC"])
            P.dve(lambda e: e.tensor_scalar(out=LC[:], in0=LC[:], scalar1=-8.0, scalar2=None, op0=ALU.mult), r=["LC"], w=["LC"])
            wfcnt = [0]

            def proj_fm(blk, dst, dkey, evac=None, pbo=0):
                s = wfcnt[0] % 4; wfcnt[0] += 1
                P.dma("pool", wfb[s][:], wfm[blk], w=["wfb%d" % s])
                for ti, (t0, n, col) in enumerate(TOK_TILES):
                    pbi = pbo + ti % 2
                    for k in range(8):
                        P.pe(lambda e, k=k, s=s, t0=t0, n=n, pbi=pbi: e.matmul(pb[pbi][:, :n], lhsT=wfb[s][:, k, :], rhs=hT[:, k, t0:t0 + n],
                                                                              start=(k == 0), stop=(k == 7)), r=["wfb%d" % s, "hT"], w=["pb%d" % pbi])
                    if evac is None:
                        P.act(lambda e, t0=t0, n=n, pbi=pbi: e.activation(out=dst[:, t0:t0 + n], in_=pb[pbi][:, :n], func=AF.Copy), r=["pb%d" % pbi], w=[sk(dkey, t0)])
                    else:
                        evac(ti, t0, n, pbi)

            for c in range(NCH):
                for k in range(8):
                    P.pe(lambda e, c=c, k=k: e.matmul(pb[7][:, c * 8:(c + 1) * 8], lhsT=hT[:, k, c * 128:(c + 1) * 128], rhs=wgb[:, k, :],
                                                      start=(k == 0), stop=(k == 7)), r=["hT", "wgb"], w=["pb7"])
            P.dve(lambda e: e.tensor_tensor(out=GA[:], in0=pb[7][:, 0:NCH * 8].rearrange("p (c g) -> p c g", g=8),
                                            in1=gbs[:].unsqueeze(1).to_broadcast([128, NCH, 8]), op=ALU.add), r=["pb7", "gbs"], w=["GA"])
            P.act(lambda e: e.activation(out=Lsp[:], in_=GA[:, :, 4:8], func=AF.Exp, scale=-1.0), r=["GA"], w=["Lsp"])
            P.act(lambda e: e.activation(out=Lsp[:], in_=Lsp[:], func=AF.Ln, bias=1.0), r=["Lsp"], w=["Lsp"])
            NG = NCH * 2
            for gi, (lh, sl) in enumerate([(triU, slice(0, 2)), (triL, slice(2, 4)), (onesf, slice(0, 2)), (onesf, slice(2, 4))]):
                P.pe(lambda e, gi=gi, lh=lh, sl=sl: e.matmul(pb[6][:, gi * NG:(gi + 1) * NG], lhsT=lh[:], rhs=Lsp[:, :, sl], start=True, stop=True),
                     r=["triU", "triL", "onesf", "Lsp"], w=["pb6"])
            P.act(lambda e: e.activation(out=EX[:].rearrange("p a c h -> p (a c h)"), in_=pb[6][:, 0:4 * NG], func=AF.Exp, scale=-1.0), r=["pb6"], w=["EX"])
            for d in range(2):
                P.dve(lambda e, d=d: e.tensor_tensor(out=WV[:, d], in0=GA[:, :, 2 * d:2 * d + 2],
                                                     in1=pb[6][:, d * NG:(d + 1) * NG].rearrange("p (c h) -> p c h", h=2), op=ALU.add), r=["GA", "pb6", "EX"], w=["WV"])
            lnc = sb("lnc%d" % s, [128, 1])
            P.dve(lambda e: e.memset(lnc[:], LN_DK), w=["lnc"])
            P.act(lambda e: e.activation(out=WV[:], in_=WV[:], func=AF.Exp, bias=lnc[:, 0:1], scale=1.0), r=["WV", "lnc"], w=["WV"])
            masks = [triU, triL]; orders = [ORDER_F, ORDER_B]
            Hm = [G[2], G[3]]

            def Hv(c):
                g = Hm[c // 17]; cc = c % 17
                return g[:, cc * 256:(cc + 1) * 256]

            for hh in range(2):
                caps = []
                for (blk, dstb, ga, gb, pbo) in [(2 * hh, qT, 0, 1, 0), (2 * hh + 1, kT, 2, 3, 2)]:
                    with P.capture() as cap:
                        proj_fm(blk, G[ga], "G%d" % ga, pbo=pbo)
                        for sg_ in range(2):
                            a_, b_ = SEGS[sg_]
                            emit_conv_seg(P, G[ga], "G%d" % ga, G[gb], "G%d" % gb, cws[:, blk, :], cbs[:, blk:blk + 1], ["cws", "cbs"], sg_)
                            P.act(lambda e, dstb=dstb, gb=gb, a_=a_, b_=b_: e.activation(out=dstb[:, a_:b_], in_=G[gb][:, a_:b_], func=AF.Silu),
                                  r=["G%d/%d" % (gb, sg_)], w=["QK"])
                    caps.append(cap)
                P.add_interleaved(caps)
                P.dma("pool", wvb[:], wv[hh], w=["wvb"])
                for c2 in range(NCH // 2):
                    pbi = c2 % 2
                    for cc in range(2):
                        c = 2 * c2 + cc
                        for k in range(8):
                            P.pe(lambda e, c=c, cc=cc, k=k, pbi=pbi: e.matmul(pb[pbi][:, cc * 256:(cc + 1) * 256], lhsT=hT[:, k, c * 128:(c + 1) * 128], rhs=wvb[:, k, :],
                                                                              start=(k == 0), stop=(k == 7)), r=["hT", "wvb"], w=["pb%d" % pbi])
                    P.act(lambda e, c2=c2, pbi=pbi: e.activation(out=VT[:, c2 * 512:(c2 + 1) * 512], in_=pb[pbi][:, :], func=AF.Copy), r=["pb%d" % pbi], w=["G1"])
                pT = pb[7][:].bitcast(BF16)
                for c4 in range(0, NCH, 4):
                    nn = min(4, NCH - c4)
                    for cc in range(nn):
                        c = c4 + cc
                        P.pe(lambda e, c=c, cc=cc: e.transpose(pT[:, cc * 128:(cc + 1) * 128], kT[:, c * 128:(c + 1) * 128], identb[:]), r=["QK", "identb"], w=["pb7"])
                    P.act(lambda e, c4=c4, nn=nn: e.activation(out=ktok[:, c4:c4 + nn, :].rearrange("p c d -> p (c d)"), in_=pT[:, 0:nn * 128], func=AF.Copy),
                          r=["pb7"], w=["G0"])
                P.dve(lambda e: e.memset(G[2][:, :], 0.0), w=["G2"])
                P.dve(lambda e: e.memset(G[3][:, :], 0.0), w=["G3"])
                vcnt = 0
                for i in range(NCH):
                    caps = []
                    for d in range(2):
                        with P.capture() as cap:
                            c = orders[d][i]; cprev = orders[d][i - 1] if i > 0 else None
                            qc = qT[:, c * 128:(c + 1) * 128]; kc = kT[:, c * 128:(c + 1) * 128]
                            pS = pb[0 + d]; pO = pb[2 + d]; pD = pb[4 + d]
                            kS, kO, kD = "pb%d" % d, "pb%d" % (2 + d), "pb%d" % (4 + d)
                            v = vs[vcnt % 4]; vk = "vs%d" % (vcnt % 4); vcnt += 1
                            wcol = WV[:, d, c, hh:hh + 1]
                            hk = "G%d" % (2 + c // 17)
                            P.pe(lambda e, pS=pS, kc=kc, qc=qc: e.matmul(pS[:, 0:128], lhsT=kc, rhs=qc, start=True, stop=True), r=["QK"], w=[kS])
                            P.dve(lambda e, d=d, pS=pS: e.tensor_tensor(out=PT[d][:], in0=pS[:, 0:128], in1=masks[d][:], op=ALU.mult),
                                  r=[kS, "triU", "triL"], w=["PT%d" % d])
                            P.act(lambda e, v=v, c=c, wcol=wcol: e.activation(out=v[:, 0:256], in_=vtok[:, c, :], func=AF.Identity, scale=wcol),
                                  r=["G1", "WV"], w=[vk])
                            P.act(lambda e, v=v, wcol=wcol: e.activation(out=v[:, 256:257], in_=wcol, func=AF.Identity), r=["WV"], w=[vk])
                            P.pe(lambda e, d=d, pO=pO, v=v, i=i: e.matmul(pO[:, 0:257], lhsT=PT[d][:], rhs=v[:], start=True, stop=(i == 0)), r=["PT%d" % d, vk], w=[kO])
                            if i > 0:
                                P.pe(lambda e, d=d, pO=pO, qc=qc: e.matmul(pO[:, 0:257], lhsT=qc, rhs=Sbf[d][:], start=False, stop=True), r=["QK", "Sbf%d" % d], w=[kO])
                            ebc = EX[:, d, c, hh:hh + 1]
                            dd = dn[d]; dk_ = "dn%d" % d
                            P.dve(lambda e, dd=dd, pO=pO: e.tensor_copy(out=dd[:, 1:2], in_=pO[:, 256:257]), r=[kO], w=[dk_])
                            P.dve(lambda e, dd=dd: e.scalar_tensor_tensor(out=dd[:, 0:1], in0=dd[:, 1:2], scalar=-1.0, in1=dd[:, 1:2], op0=ALU.mult, op1=ALU.max), r=[dk_], w=[dk_])
                            P.dve(lambda e, dd=dd: e.reciprocal(out=dd[:, 2:3], in_=dd[:, 0:1]), r=[dk_], w=[dk_])
                            P.dve(lambda e, dd=dd, ebc=ebc: e.tensor_tensor(out=dd[:, 3:4], in0=dd[:, 2:3], in1=ebc, op=ALU.min), r=[dk_, "EX"], w=[dk_])
                            P.dve(lambda e, dd=dd, pO=pO, c=c: e.scalar_tensor_tensor(out=Hv(c), in0=pO[:, 0:256], scalar=dd[:, 3:4], in1=Hv(c), op0=ALU.mult, op1=ALU.add),
                                  r=[kO, dk_, hk], w=[hk])
                            if i < NCH - 1:
                                P.pe(lambda e, pD=pD, c=c, v=v: e.matmul(pD[:, 0:257], lhsT=ktok[:, c, :], rhs=v[:], start=True, stop=True), r=["G0", vk], w=[kD])
                                if i == 0:
                                    P.dve(lambda e, d=d, pD=pD: e.tensor_copy(out=Sacc[d][:], in_=pD[:, 0:257]), r=[kD], w=["Sacc%d" % d])
                                else:
                                    eprev = EX[:, 2 + d, cprev, hh:hh + 1]
                                    P.dve(lambda e, d=d, pD=pD, eprev=eprev: e.scalar_tensor_tensor(out=Sacc[d][:], in0=Sacc[d][:], scalar=eprev, in1=pD[:, 0:257],
                                                                                                    op0=ALU.mult, op1=ALU.add), r=[kD, "EX", "Sacc%d" % d], w=["Sacc%d" % d])
                                ecur = EX[:, 2 + d, c, hh:hh + 1]
                                P.act(lambda e, d=d, ecur=ecur: e.activation(out=Sbf[d][:], in_=Sacc[d][:], func=AF.Identity, scale=ecur), r=["Sacc%d" % d, "EX"], w=["Sbf%d" % d])
                            emit_pe_warm(P, pb[7], onesb, hT, 3)
                        caps.append(cap)
                    P.add_interleaved(caps)
                Hall = [g[:, 0:17 * 256].rearrange("p (c v) -> p c v", v=256) for g in Hm]
                for gi in range(2):
                    P.dve(lambda e, gi=gi: e.tensor_tensor(out=G[gi][:, 0:17 * 256], in0=Hm[gi][:, 0:17 * 256], in1=Hm[gi][:, 0:17 * 256], op=ALU.mult),
                          r=["G%d" % (2 + gi)], w=["G%d" % gi])
                    P.dve(lambda e, gi=gi: e.tensor_reduce(out=ssq[:, gi * 17:(gi + 1) * 17], in_=G[gi][:, 0:17 * 256].rearrange("p (c v) -> p c v", v=256), axis=AX.X, op=ALU.add),
                          r=["G%d" % gi], w=["ssq"])
                P.act(lambda e: e.activation(out=ssq[:], in_=ssq[:], func=AF.Sqrt, bias=epsc[:, 0:1], scale=1.0 / 256), r=["ssq", "epsc"], w=["ssq"])
                P.dve(lambda e: e.reciprocal(out=ssq[:], in_=ssq[:]), r=["ssq"], w=["ssq"])
                for gi in range(2):
                    P.dve(lambda e, gi=gi: e.tensor_tensor(out=Hall[gi], in0=Hall[gi], in1=ssq[:, gi * 17:(gi + 1) * 17].unsqueeze(2).to_broadcast([128, 17, 256]), op=ALU.mult),
                          r=["G%d" % (2 + gi), "ssq"], w=["G%d" % (2 + gi)])
                    P.dve(lambda e, gi=gi, hh=hh: e.tensor_tensor(out=Hall[gi], in0=Hall[gi], in1=mns[:, hh * 256:(hh + 1) * 256].unsqueeze(1).to_broadcast([128, 17, 256]), op=ALU.mult),
                          r=["G%d" % (2 + gi), "mns"], w=["G%d" % (2 + gi)])
                P.dma("pool", wvb[:], wo[:, :, hh * 256:(hh + 1) * 256], w=["wvb"])
                for c2 in range(NCH // 2):
                    pbi = c2 % 2; ob = obf[c2 % 2]; og = osig[c2 % 2]
                    for cc in range(2):
                        c = 2 * c2 + cc
                        for k in range(8):
                            P.pe(lambda e, c=c, cc=cc, k=k, pbi=pbi, hh=hh: e.matmul(pb[pbi][:, cc * 256:(cc + 1) * 256], lhsT=hT[:, k, c * 128:(c + 1) * 128],
                                                                                     rhs=wvb[:, k, :], start=(k == 0), stop=(k == 7)), r=["hT", "wvb"], w=["pb%d" % pbi])
                    P.act(lambda e, pbi=pbi, og=og: e.activation(out=og[:], in_=pb[pbi][:, :], func=AF.Sigmoid), r=["pb%d" % pbi], w=["tmp%d" % (c2 % 2)])
                    c0 = 2 * c2
                    hsrc = Hm[c0 // 17][:, (c0 % 17) * 256:(c0 % 17) * 256 + 512] if (c0 % 17) != 16 else None
                    if hsrc is not None:
                        P.dve(lambda e, ob=ob, og=og, hsrc=hsrc: e.tensor_tensor(out=ob[:], in0=og[:], in1=hsrc, op=ALU.mult),
                              r=["tmp%d" % (c2 % 2), "G%d" % (2 + c0 // 17)], w=["obf%d" % (c2 % 2)])
                    else:
                        for cc in range(2):
                            P.dve(lambda e, ob=ob, og=og, cc=cc, c0=c0: e.tensor_tensor(out=ob[:, cc * 256:(cc + 1) * 256], in0=og[:, cc * 256:(cc + 1) * 256], in1=Hv(c0 + cc), op=ALU.mult),
                                  r=["tmp%d" % (c2 % 2), "G2", "G3"], w=["obf%d" % (c2 % 2)])
                    ch0 = (2 * s + hh) * 2
                    emit_tok2fm(P, pb[7], identb, ob, "obf%d" % (c2 % 2), obT, "obT", out_mix[:, ch0:ch0 + 2, c0 * 128:(c0 + 2) * 128])
            xlb = XB[:, :]
            G4 = QK
            seg_tiles = [[(ti, t) for ti, t in enumerate(TOK_TILES) if t[0] < SEG_SPLIT], [(ti, t) for ti, t in enumerate(TOK_TILES) if t[0] >= SEG_SPLIT]]
            for nb in range(4):
                proj_fm(4 + nb, G[0], "G0")
                for sg_ in range(2):
                    a_, b_ = SEGS[sg_]
                    emit_conv_seg(P, G[0], "G0", G[1], "G1", cws[:, 4 + nb, :], cbs[:, 4 + nb:5 + nb], ["cws", "cbs"], sg_)
                    P.act(lambda e, a_=a_, b_=b_: e.activation(out=xlb[:, a_:b_], in_=G[1][:, a_:b_], func=AF.Copy), r=["G1/%d" % sg_],
                          w=["XB/%d" % sg_] + ["XB%d" % k for k in range(8)])
                for d in range(2):
                    for sg_ in range(2):
                        a_, b_ = SEGS[sg_]
                        for (wsb, bsb, dst, dkey, wkey, bkey) in [(lwab, lbas, G[2], "G2", "lwab", "lbas"), (lwxb, lbxs, G[3], "G3", "lwxb", "lbxs")]:
                            for ti, (t0, n, col) in seg_tiles[sg_]:
                                pbi = ti % 2
                                P.pe(lambda e, wsb=wsb, d=d, nb=nb, t0=t0, n=n, pbi=pbi: e.matmul(pb[pbi][:, :n], lhsT=wsb[:, d, nb, :], rhs=xlb[:, t0:t0 + n], start=True, stop=True),
                                     r=[wkey, "XB/%d" % sg_], w=["pb%d" % pbi])
                                P.act(lambda e, bsb=bsb, dst=dst, d=d, nb=nb, t0=t0, n=n, pbi=pbi: e.activation(out=dst[:, t0:t0 + n], in_=pb[pbi][:, :n], func=AF.Sigmoid,
                                                                                                          bias=bsb[:, d, nb:nb + 1], scale=1.0), r=["pb%d" % pbi, bkey], w=["%s/%d" % (dkey, sg_)])
                        P.act(lambda e, d=d, nb=nb, a_=a_, b_=b_: e.activation(out=G[2][:, a_:b_], in_=G[2][:, a_:b_], func=AF.Exp, scale=LC[:, d, nb:nb + 1]),
                              r=["G2/%d" % sg_, "LC"], w=["G2/%d" % sg_])
                    for sg_ in range(2):
                        a_, b_ = SEGS[sg_]
                        K2, K3, K0, K1 = "G2/%d" % sg_, "G3/%d" % sg_, "G0/%d" % sg_, "G1/%d" % sg_
                        P.dve(lambda e, a_=a_, b_=b_: e.tensor_tensor(out=G[3][:, a_:b_], in0=G[3][:, a_:b_], in1=G[1][:, a_:b_], op=ALU.mult), r=[K3, K1], w=[K3])
                        P.dve(lambda e, a_=a_, b_=b_: e.tensor_tensor(out=G[0][:, a_:b_], in0=G[2][:, a_:b_], in1=G[2][:, a_:b_], op=ALU.mult), r=[K2], w=[K0])
                        P.act(lambda e, a_=a_, b_=b_: e.activation(out=G[0][:, a_:b_], in_=G[0][:, a_:b_], func=AF.Sqrt, scale=-1.0, bias=1.0), r=[K0], w=[K0])
                        P.dve(lambda e, a_=a_, b_=b_: e.tensor_tensor(out=G[3][:, a_:b_], in0=G[3][:, a_:b_], in1=G[0][:, a_:b_], op=ALU.mult), r=[K3, K0], w=[K3])
                    S = SEG_SPLIT
                    if d == 0:
                        P.dve(lambda e: e.tensor_tensor_scan(out=G4[:, 0:S], data0=G[2][:, 0:S], data1=G[3][:, 0:S], initial=0.0, op0=ALU.mult, op1=ALU.add),
                              r=["G2/0", "G3/0"], w=["QK/0"])
                        P.dve(lambda e: e.tensor_tensor_scan(out=G4[:, S:T_ALL], data0=G[2][:, S:T_ALL], data1=G[3][:, S:T_ALL], initial=G4[:, S - 1:S],
                                                             op0=ALU.mult, op1=ALU.add), r=["G2/1", "G3/1", "QK/0"], w=["QK/1"])
                    else:
                        P.dve(lambda e: e.tensor_tensor_scan(out=G[0][:, 255::-1], data0=G[2][:, 255::-1], data1=G[3][:, 255::-1], initial=0.0, op0=ALU.mult, op1=ALU.add),
                              r=["G2/0", "G3/0"], w=["G0/0"])
                        P.dve(lambda e: e.tensor_tensor_scan(out=G[0][:, T_ALL - 1:S - 1:-1], data0=G[2][:, T_ALL - 1:S - 1:-1], data1=G[3][:, T_ALL - 1:S - 1:-1],
                                                             initial=G[0][:, 0:1], op0=ALU.mult, op1=ALU.add), r=["G2/1", "G3/1", "G0/0"], w=["G0/1"])
                        P.dve(lambda e: e.tensor_tensor_scan(out=G[0][:, S - 1:255:-1], data0=G[2][:, S - 1:255:-1], data1=G[3][:, S - 1:255:-1],
                                                             initial=G[0][:, S:S + 1], op0=ALU.mult, op1=ALU.add), r=["G2/0", "G3/0", "G0/1", "G0/0"], w=["G0/0"])
                        for sg_ in range(2):
                            a_, b_ = SEGS[sg_]
                            P.dve(lambda e, a_=a_, b_=b_: e.tensor_tensor(out=G4[:, a_:b_], in0=G4[:, a_:b_], in1=G[0][:, a_:b_], op=ALU.add),
                                  r=["QK/%d" % sg_, "G0/%d" % sg_], w=["QK/%d" % sg_])
                def gate_evac(ti, t0, n, pbi):
                    a = tmp[0]; b_ = tmp[1]; ob = hlb[ti % 2]; okey = "obf%d" % (ti % 2); pk = "pb%d" % pbi
                    P.dve(lambda e: e.tensor_copy(out=a[:, :n], in_=pb[pbi][:, :n]), r=[pk], w=["tmp0"])
                    P.dve(lambda e: e.tensor_tensor(out=b_[:, :n], in0=a[:, :n], in1=a[:, :n], op=ALU.mult), r=["tmp0"], w=["tmp1"])
                    P.dve(lambda e: e.tensor_scalar(out=b_[:, :n], in0=b_[:, :n], scalar1=0.044715, scalar2=1.0, op0=ALU.mult, op1=ALU.add), r=["tmp1"], w=["tmp1"])
                    P.dve(lambda e: e.tensor_tensor(out=b_[:, :n], in0=b_[:, :n], in1=a[:, :n], op=ALU.mult), r=["tmp1", "tmp0"], w=["tmp1"])
                    P.act(lambda e: e.activation(out=b_[:, :n], in_=b_[:, :n], func=AF.Sigmoid, scale=2.0 * 0.7978845608028654), r=["tmp1"], w=["tmp1"])
                    P.dve(lambda e: e.tensor_tensor(out=a[:, :n], in0=a[:, :n], in1=b_[:, :n], op=ALU.mult), r=["tmp0", "tmp1"], w=["tmp0"])
                    P.dve(lambda e: e.tensor_tensor(out=ob[:, :n], in0=a[:, :n], in1=G4[:, t0:t0 + n], op=ALU.mult), r=["tmp0", sk("QK", t0)], w=[okey])
                    P.dma("sp", out_mix[:, 8 + 4 * s + nb, t0:t0 + n], ob[:, :n], r=[okey], final=True)
                proj_fm(8 + nb, None, None, evac=gate_evac)

        for s_ in range(2):
            half(s_, drs[s_])
        P.emit()


def mix_common_consts(layer, I):
    d = {}
    d["modw"] = modblk(I["mod_w"][layer], 0, 2048)
    d["modb"] = colvec(I["mod_b"][layer][0:2048])
    d["g0"] = colvec(I["norm_g"][layer][0])
    r = np.arange(128)
    d["triU"] = (r[:, None] <= r[None, :]).astype(np.float32)
    d["triL"] = (r[:, None] >= r[None, :]).astype(np.float32)
    import ml_dtypes
    d["identb"] = np.eye(128, dtype=np.float32).astype(ml_dtypes.bfloat16)
    return d


def mix0_consts(I, s):
    W = I["ab_in_w"][0]
    h0, h1 = 2 * s, 2 * s + 1
    qcol = lambda h: np.arange(128 * h, 128 * h + 128)
    kcol = lambda h: 512 + np.arange(128 * h, 128 * h + 128)
    blocks = [qcol(h0), kcol(h0), qcol(h1), kcol(h1)]
    blocks += [3088 + 512 * s + 128 * n + np.arange(128) for n in range(4)]
    blocks += [4112 + 512 * s + 128 * n + np.arange(128) for n in range(4)]
    d = {}
    d["wfm"] = np.stack([kblk(W[:, c]) for c in blocks], 0)
    d["wv"] = np.stack([kblk(W[:, 1024 + 256 * h:1024 + 256 * h + 256]) for h in (h0, h1)], 0)
    d["wo"] = kblk(W[:, 2048 + 512 * s:2048 + 512 * s + 512])
    gcols = [3072 + t * 4 + h for t in range(4) for h in (h0, h1)]
    d["wg"] = kblk(W[:, gcols])
    gb = I["m_gate_b"][0]
    d["gbias"] = np.ascontiguousarray(np.broadcast_to(np.array([gb[t, h] for t in range(4) for h in (h0, h1)], np.float32)[None, :], (128, 8)))
    cw = np.zeros((128, 8, 4), np.float32); cb = np.zeros((128, 8), np.float32)
    for bi in range(4):
        cw[:, bi, :] = I["m_conv_w"][0][:, blocks[bi]].T
        cb[:, bi] = I["m_conv_b"][0][blocks[bi]]
    for n in range(4):
        ch = 512 * s + 128 * n + np.arange(128)
        cw[:, 4 + n, :] = I["l_conv_w"][0][:, ch].T
        cb[:, 4 + n] = I["l_conv_b"][0][ch]
    d["cw"] = cw; d["cb"] = cb
    d["mnorm"] = np.ascontiguousarray(np.broadcast_to(I["m_norm_w"][0][512 * s:512 * s + 512][None, :], (128, 512)))
    d["lwa"] = np.ascontiguousarray(I["l_wa"][0][:, 4 * s:4 * s + 4].transpose(2, 0, 1, 3))
    d["lwx"] = np.ascontiguousarray(I["l_wx"][0][:, 4 * s:4 * s + 4].transpose(2, 0, 1, 3))
    pc = lambda v: np.ascontiguousarray(v[:, 512 * s:512 * s + 512].reshape(2, 4, 128).transpose(2, 0, 1))
    d["lba"] = pc(I["l_ba"][0]); d["lbx"] = pc(I["l_bx"][0]); d["llam"] = pc(I["l_lam"][0])
    return d


def stream_fm(I_x, I_ctx, b):
    return fm(np.concatenate([I_ctx[b], I_x[b]], 0))


DK_SCALE = float(128.0 ** -0.5)


def phase_mix1(nc, P, pb, drs, xT, out_mix):
    dr = drs[0]
    modw = dr("modw", [16, 128, 8, 128])
    modb = dr("modb", [128, 16])
    cT = dr("cT", [128, 8, 2])
    g0 = dr("g0", [128, 8])
    triUd = dr("triU", [128, 128])
    triLd = dr("triL", [128, 128])
    identd = dr("identb", [128, 128], BF16)
    identfd = dr("identf", [128, 128])
    with ExitStack() as es:
        sb, ps = _mk(nc, es)
        hT = sb("hT", [128, 8, T_ALL], BF16)
        G = [sb("G%d" % i, [128, T_ALL]) for i in range(4)]
        QA = sb("QA", [128, T_ALL]); XB = sb("XB", [128, T_ALL], BF16)
        qab = QA[:].bitcast(BF16)
        qT = qab[:, 0:T_ALL]; kT = qab[:, T_ALL:2 * T_ALL]
        VT = G[1][:].bitcast(BF16); vtok = VT.rearrange("p (c v) -> p c v", v=256)
        ktok = G[0][:].bitcast(BF16)[:, 0:T_ALL].rearrange("p (c d) -> p c d", d=128)
        tmp = [sb("tmp%d" % i, [128, 512]) for i in range(4)]
        rstd = sb("rstd", [128, 512]); onesb = sb("onesb", [128, 128], BF16); onesf = sb("onesf", [128, 128]); epsc = sb("epsc", [128, 1])
        g0sb = sb("g0sb", [128, 8]); A0 = sb("A0", [128, 8, 2])
        triU = sb("triU_sb", [128, 128]); triL = sb("triL_sb", [128, 128]); identb = sb("identb_sb", [128, 128], BF16); identf = sb("identf_sb", [128, 128])
        wfb = [sb("wfb0", [128, 8, 128], BF16)] * 2
        wvb = sb("wvb", [128, 8, 256], BF16); wgab = sb("wgab", [128, 8, 32], BF16); wdtb = sb("wdtb", [128, 8, 16], BF16)
        alph = sb("alph_sb", [64, 256], BF16); gns = sb("gns", [128, 512]); cws = sb("cws", [128, 6, 4]); cbs = sb("cbs", [128, 6])
        dtbs = sb("dtbs", [128, 16]); Aneg = sb("Aneg", [128, 16]); sDs = sb("sDs", [128, 8])
        DEC = sb("DEC", [128, 6, NCH, 16])
        EBLg = sb("EBLg", [128, NCH])
        Sacc = [sb("Sacc%d" % i, [128, 256]) for i in range(2)]; Sbf = [sb("Sbf%d" % i, [128, 256], BF16) for i in range(2)]
        PT = [sb("PT%d" % i, [128, 128], BF16) for i in range(2)]
        CBm = [sb("CBm%d" % i, [128, 128]) for i in range(2)]; MT = [sb("MT%d" % i, [128, 512], BF16) for i in range(2)]
        obT = MT[0]
        xdt = [sb("xdt%d" % i, [128, 256], BF16) for i in range(2)]; xw = [sb("xw%d" % i, [128, 256], BF16) for i in range(2)]
        ssq = sb("ssq", [128, NCH]); obf = [sb("obf%d" % i, [128, 512], BF16) for i in range(2)]
        for (dst, src, key) in [(g0sb, g0, "g0sb"), (triU, triUd, "triU"), (triL, triLd, "triL"), (identb, identd, "identb"), (identf, identfd, "identf")]:
            P.dma("sp", dst[:], src, w=[key])
        P.dve(lambda e: e.memset(onesb[:], 1.0), w=["onesb"])
        P.dve(lambda e: e.memset(onesf[:], 1.0), w=["onesf"])
        P.dve(lambda e: e.memset(epsc[:], EPS), w=["epsc"])
        mwb = [(G[1][:, 1024 * i:1024 * (i + 1)].rearrange("p (a b) -> p a b", b=128), ["mwst%d" % i]) for i in range(4)]
        modT = emit_modulation(P, nc, sb, pb[7], modw, modb, cT, 16, mwb)
        xin = G[0][:, 0:4096].rearrange("p (k n) -> p k n", k=8)
        sqv = XB[:, 0:4096].rearrange("p (k n) -> p k n", k=8)
        XBK = ["XB"] + ["XB%d" % k for k in range(8)]
        emit_adaln_in(P, nc, sb, pb, xT, modT, g0sb, hT, xin, "G0", sqv, "XB", tmp, rstd, onesb, epsc, A0)
        def half(s, dr):
            wfm = dr("wfm", [10, 128, 8, 128])
            wga = dr("wga", [128, 8, 32])
            wv = dr("wv", [2, 128, 8, 256])
            wr = dr("wr", [128, 8, 512])
            wz = dr("wz", [128, 8, 512])
            wdt = dr("wdt", [128, 8, 16])
            alphd = dr("alph", [64, 256])
            gnorm = dr("gnorm", [128, 512])
            cw = dr("cw", [128, 6, 4])
            cb = dr("cb", [128, 6])
            dtbd = dr("dtb", [128, 16])
            alogd = dr("alog", [128, 16])
            sDd = dr("sD", [128, 8])
            for (dst, src, key) in [(cws, cw, "cws"), (cbs, cb, "cbs"), (gns, gnorm, "gns"), (dtbs, dtbd, "dtbs"), (Aneg, alogd, "Aneg"), (sDs, sDd, "sDs")]:
                P.dma("sp", dst[:], src, w=[key])
            for (dst, src, key) in [(wgab, wga, "wgab"), (wdtb, wdt, "wdtb"), (alph, alphd, "alph")]:
                P.dma("pool", dst[:], src, w=[key])
            P.act(lambda e: e.activation(out=Aneg[:], in_=Aneg[:], func=AF.Exp), r=["Aneg"], w=["Aneg"])
            P.dve(lambda e: e.tensor_scalar(out=Aneg[:], in0=Aneg[:], scalar1=-1.0, scalar2=None, op0=ALU.mult), r=["Aneg"], w=["Aneg"])
            wfcnt = [0]

            def proj_fm(blk, dst, dkey):
                sl = wfcnt[0] % 2; wfcnt[0] += 1
                if sl == 0:
                    wdst = wfb[0][:]; wk = "wfb0"; wl = lambda k: wfb[0][:, k, :]
                else:
                    wdst = wvb[:, :, 128:256]; wk = "wvb"; wl = lambda k: wvb[:, k, 128:256]
                P.dma("pool", wdst, wfm[blk], w=[wk])
                for ti, (t0, n, col) in enumerate(TOK_TILES):
                    pbi = ti % 2
                    for k in range(8):
                        P.pe(lambda e, k=k, t0=t0, n=n, pbi=pbi: e.matmul(pb[pbi][:, :n], lhsT=wl(k), rhs=hT[:, k, t0:t0 + n],
                                                                          start=(k == 0), stop=(k == 7)), r=[wk, "hT"], w=["pb%d" % pbi])
                    P.act(lambda e, t0=t0, n=n, pbi=pbi: e.activation(out=dst[:, t0:t0 + n], in_=pb[pbi][:, :n], func=AF.Copy), r=["pb%d" % pbi], w=[sk(dkey, t0)])

            def proj_tok2(wsb, wkey, c2, pbi):
                for cc in range(2):
                    c = 2 * c2 + cc
                    for k in range(8):
                        P.pe(lambda e, c=c, cc=cc, k=k: e.matmul(pb[pbi][:, cc * 256:(cc + 1) * 256], lhsT=hT[:, k, c * 128:(c + 1) * 128], rhs=wsb[:, k, :],
                                                                 start=(k == 0), stop=(k == 7)), r=["hT", wkey], w=["pb%d" % pbi])

            masks = [triU, triL]; orders = [ORDER_F, ORDER_B]
            Hm = [G[2], G[3]]

            def Hv(c):
                g = Hm[c // 17]; cc = c % 17
                return g[:, cc * 256:(cc + 1) * 256]

            def headnorm_gate(normsb, nkey, nslice, gate_w_dram, func, ch0):
                Hall = [g[:, 0:17 * 256].rearrange("p (c v) -> p c v", v=256) for g in Hm]
                for gi in range(2):
                    P.dve(lambda e, gi=gi: e.tensor_tensor(out=G[gi][:, 0:17 * 256], in0=Hm[gi][:, 0:17 * 256], in1=Hm[gi][:, 0:17 * 256], op=ALU.mult),
                          r=["G%d" % (2 + gi)], w=["G%d" % gi])
                    P.dve(lambda e, gi=gi: e.tensor_reduce(out=ssq[:, gi * 17:(gi + 1) * 17], in_=G[gi][:, 0:17 * 256].rearrange("p (c v) -> p c v", v=256), axis=AX.X, op=ALU.add),
                          r=["G%d" % gi], w=["ssq"])
                P.act(lambda e: e.activation(out=ssq[:], in_=ssq[:], func=AF.Sqrt, bias=epsc[:, 0:1], scale=1.0 / 256), r=["ssq", "epsc"], w=["ssq"])
                P.dve(lambda e: e.reciprocal(out=ssq[:], in_=ssq[:]), r=["ssq"], w=["ssq"])
                for gi in range(2):
                    P.dve(lambda e, gi=gi: e.tensor_tensor(out=Hall[gi], in0=Hall[gi], in1=ssq[:, gi * 17:(gi + 1) * 17].unsqueeze(2).to_broadcast([128, 17, 256]), op=ALU.mult),
                          r=["G%d" % (2 + gi), "ssq"], w=["G%d" % (2 + gi)])
                    P.dve(lambda e, gi=gi: e.tensor_tensor(out=Hall[gi], in0=Hall[gi], in1=normsb[:, nslice].unsqueeze(1).to_broadcast([128, 17, 256]), op=ALU.mult),
                          r=["G%d" % (2 + gi), nkey], w=["G%d" % (2 + gi)])
                gate_out(gate_w_dram, func, ch0)

            def gate_out(gate_w_dram, func, ch0):
                P.dma("pool", wvb[:], gate_w_dram, w=["wvb"])
                for c2 in range(NCH // 2):
                    pbi = c2 % 2; ob = obf[c2 % 2]; og = tmp[c2 % 2]
                    proj_tok2(wvb, "wvb", c2, pbi)
                    P.act(lambda e, pbi=pbi, og=og: e.activation(out=og[:], in_=pb[pbi][:, :], func=func), r=["pb%d" % pbi], w=["tmp%d" % (c2 % 2)])
                    c0 = 2 * c2
                    for cc in range(2):
                        P.dve(lambda e, ob=ob, og=og, cc=cc, c0=c0: e.tensor_tensor(out=ob[:, cc * 256:(cc + 1) * 256], in0=og[:, cc * 256:(cc + 1) * 256], in1=Hv(c0 + cc), op=ALU.mult),
                              r=["tmp%d" % (c2 % 2), "G2", "G3"], w=["obf%d" % (c2 % 2)])
                    emit_tok2fm(P, pb[7], identb, ob, "obf%d" % (c2 % 2), obT, "MT0", out_mix[:, ch0:ch0 + 2, c0 * 128:(c0 + 2) * 128])

            P.dve(lambda e: e.memset(XB[0:64, :], 1.0), r=XBK, w=XBK)
            for d in range(2):
                for ti, (t0, n, col) in enumerate(TOK_TILES):
                    pbi = ti % 2
                    for k in range(8):
                        P.pe(lambda e, k=k, d=d, t0=t0, n=n, pbi=pbi: e.matmul(pb[pbi][0:16, :n], lhsT=wgab[:, k, d * 16:(d + 1) * 16], rhs=hT[:, k, t0:t0 + n],
                                                                              start=(k == 0), stop=(k == 7)), r=["wgab", "hT"], w=["pb%d" % pbi])
                    P.act(lambda e, d=d, t0=t0, n=n, pbi=pbi: e.activation(out=XB[32 * d:32 * d + 16, t0:t0 + n], in_=pb[pbi][0:16, :n], func=AF.Copy),
                          r=["pb%d" % pbi], w=XBK)
            for hh in range(2):
                P.dve(lambda e: e.memset(G[2][:, :], 0.0), w=["G2"])
                P.dve(lambda e: e.memset(G[3][:, :], 0.0), w=["G3"])
                for d in range(2):
                    proj_fm(2 * hh, G[0], "G0")
                    proj_fm(2 * hh + 1, G[1], "G1")
                    ecol = 127 if d == 0 else 0
                    for c4 in range(0, NCH, 4):
                        nn = min(4, NCH - c4); W = nn * 128; cs = slice(c4 * 128, c4 * 128 + W)
                        for cc in range(nn):
                            c = c4 + cc
                            P.pe(lambda e, c=c, cc=cc, d=d, hh=hh: e.matmul(pb[6][:, cc * 128:(cc + 1) * 128], lhsT=XB[32 * d:32 * d + 17, c * 128:(c + 1) * 128],
                                                                            rhs=alph[32 * d:32 * d + 17, hh * 128:(hh + 1) * 128], start=True, stop=True), r=XBK + ["alph"], w=["pb6"])
                        P.act(lambda e, W=W: e.activation(out=tmp[0][:, :W], in_=pb[6][:, :W], func=AF.Exp, scale=-1.0), r=["pb6"], w=["tmp0"])
                        P.act(lambda e, W=W: e.activation(out=tmp[0][:, :W], in_=tmp[0][:, :W], func=AF.Ln, bias=1.0), r=["tmp0"], w=["tmp0"])
                        for cc in range(nn):
                            P.pe(lambda e, cc=cc, d=d: e.matmul(pb[7][:, cc * 128:(cc + 1) * 128], lhsT=tmp[0][:, cc * 128:(cc + 1) * 128], rhs=masks[d][:], start=True, stop=True),
                                 r=["tmp0", "triU", "triL"], w=["pb7"])
                        P.act(lambda e, W=W: e.activation(out=tmp[1][:, :W], in_=pb[7][:, :W], func=AF.Exp, scale=-1.0 / 16), r=["pb7"], w=["tmp1"])
                        P.act(lambda e, W=W: e.activation(out=tmp[2][:, :W], in_=pb[7][:, :W], func=AF.Exp, scale=1.0 / 16), r=["pb7"], w=["tmp2"])
                        P.dve(lambda e, W=W, cs=cs: e.tensor_tensor(out=qT[:, cs], in0=G[0][:, cs], in1=tmp[1][:, :W], op=ALU.mult), r=["G0", "tmp1"], w=["QA"])
                        P.dve(lambda e, W=W, cs=cs: e.tensor_tensor(out=kT[:, cs], in0=G[1][:, cs], in1=tmp[2][:, :W], op=ALU.mult), r=["G1", "tmp2"], w=["QA"])
                        P.dve(lambda e, W=W, c4=c4, nn=nn, ecol=ecol: e.tensor_copy(out=EBLg[:, c4:c4 + nn], in_=tmp[1][:, ecol:W:128]), r=["tmp1"], w=["EBLg"])
                    if d == 0:
                        pass
                    P.dma("pool", wvb[:], wv[hh], w=["wvb"])
                    for c2 in range(NCH // 2):
                        pbi = c2 % 2
                        proj_tok2(wvb, "wvb", c2, pbi)
                        P.act(lambda e, c2=c2, pbi=pbi: e.activation(out=VT[:, c2 * 512:(c2 + 1) * 512], in_=pb[pbi][:, :], func=AF.Copy), r=["pb%d" % pbi], w=["G1"])
                    pT = pb[7][:].bitcast(BF16)
                    for c4 in range(0, NCH, 4):
                        nn = min(4, NCH - c4)
                        for cc in range(nn):
                            c = c4 + cc
                            P.pe(lambda e, c=c, cc=cc: e.transpose(pT[:, cc * 128:(cc + 1) * 128], kT[:, c * 128:(c + 1) * 128], identb[:]), r=["QA", "identb"], w=["pb7"])
                        P.act(lambda e, c4=c4, nn=nn: e.activation(out=ktok[:, c4:c4 + nn, :].rearrange("p c d -> p (c d)"), in_=pT[:, 0:nn * 128], func=AF.Copy),
                              r=["pb7"], w=["G0"])
                    for i in range(NCH):
                        c = orders[d][i]; cprev = orders[d][i - 1] if i > 0 else None
                        par = i % 2
                        qc = qT[:, c * 128:(c + 1) * 128]; kc = kT[:, c * 128:(c + 1) * 128]
                        pS = pb[0 + par]; pO = pb[2 + par]; pD = pb[4 + par]
                        kS, kO, kD = "pb%d" % par, "pb%d" % (2 + par), "pb%d" % (4 + par)
                        hk = "G%d" % (2 + c // 17)
                        P.pe(lambda e, pS=pS, kc=kc, qc=qc: e.matmul(pS[:, 0:128], lhsT=kc, rhs=qc, start=True, stop=True), r=["QA"], w=[kS])
                        P.dve(lambda e, par=par, pS=pS, d=d: e.tensor_tensor(out=PT[par][:], in0=pS[:, 0:128], in1=masks[d][:], op=ALU.mult),
                              r=[kS, "triU", "triL"], w=["PT%d" % par])
                        P.pe(lambda e, par=par, pO=pO, c=c, i=i: e.matmul(pO[:, 0:256], lhsT=PT[par][:], rhs=vtok[:, c, :], start=True, stop=(i == 0)), r=["PT%d" % par, "G1"], w=[kO])
                        if i > 0:
                            P.pe(lambda e, pO=pO, qc=qc: e.matmul(pO[:, 0:256], lhsT=qc, rhs=Sbf[0][:], start=False, stop=True), r=["QA", "Sbf0"], w=[kO])
                        P.dve(lambda e, pO=pO, c=c: e.scalar_tensor_tensor(out=Hv(c), in0=pO[:, 0:256], scalar=DK_SCALE, in1=Hv(c), op0=ALU.mult, op1=ALU.add),
                              r=[kO, hk], w=[hk])
                        if i < NCH - 1:
                            P.pe(lambda e, pD=pD, c=c: e.matmul(pD[:, 0:256], lhsT=ktok[:, c, :], rhs=vtok[:, c, :], start=True, stop=True), r=["G0", "G1"], w=[kD])
                            if i == 0:
                                P.dve(lambda e, pD=pD: e.tensor_copy(out=Sacc[0][:], in_=pD[:, 0:256]), r=[kD], w=["Sacc0"])
                            else:
                                eprev = EBLg[:, cprev:cprev + 1]
                                P.dve(lambda e, pD=pD, eprev=eprev: e.scalar_tensor_tensor(out=Sacc[0][:], in0=Sacc[0][:], scalar=eprev, in1=pD[:, 0:256],
                                                                                            op0=ALU.mult, op1=ALU.add), r=[kD, "EBLg", "Sacc0"], w=["Sacc0"])
                            ecur = EBLg[:, c:c + 1]
                            P.act(lambda e, ecur=ecur: e.activation(out=Sbf[0][:], in_=Sacc[0][:], func=AF.Identity, scale=ecur), r=["Sacc0", "EBLg"], w=["Sbf0"])
                        emit_pe_warm(P, pb[7], onesb, hT, 3)
                headnorm_gate(gns, "gns", slice(hh * 256, (hh + 1) * 256), wr[:, :, hh * 256:(hh + 1) * 256], AF.Silu, (2 * s + hh) * 2)
            DT, LA, B_S, EBS, EBL, W_S = [DEC[:, j] for j in range(6)]
            BL = EBL
            for half, pbx in [(0, pb[7]), (1, pb[6])]:
                for cc in range(17):
                    c = half * 17 + cc
                    for k in range(8):
                        P.pe(lambda e, c=c, cc=cc, k=k, pbx=pbx: e.matmul(pbx[:, cc * 16:(cc + 1) * 16], lhsT=hT[:, k, c * 128:(c + 1) * 128], rhs=wdtb[:, k, :],
                                                                          start=(k == 0), stop=(k == 7)), r=["hT", "wdtb"], w=["pb7" if half == 0 else "pb6"])
                P.dve(lambda e, half=half, pbx=pbx: e.tensor_tensor(out=DT[:, half * 17:(half + 1) * 17, :], in0=pbx[:, 0:272].rearrange("p (c g) -> p c g", g=16),
                                                                    in1=dtbs[:].unsqueeze(1).to_broadcast([128, 17, 16]), op=ALU.add), r=["pb7" if half == 0 else "pb6", "dtbs"], w=["DEC0"])
            P.act(lambda e: e.activation(out=DT, in_=DT, func=AF.Exp), r=["DEC0"], w=["DEC0"])
            P.act(lambda e: e.activation(out=DT, in_=DT, func=AF.Ln, bias=1.0), r=["DEC0"], w=["DEC0"])
            P.dve(lambda e: e.tensor_tensor(out=LA, in0=DT, in1=Aneg[:].unsqueeze(1).to_broadcast([128, NCH, 16]), op=ALU.mult), r=["DEC0", "Aneg"], w=["DEC1"])
            for d in range(2):
                pcs, pbl = pb[6], pb[7]
                P.pe(lambda e, d=d: e.matmul(pb[6][:, 0:272], lhsT=masks[d][:], rhs=LA[:, :, d * 8:(d + 1) * 8], start=True, stop=True), r=["triU", "triL", "DEC1"], w=["pb6"])
                P.pe(lambda e, d=d: e.matmul(pb[7][:, 0:272], lhsT=onesf[:], rhs=LA[:, :, d * 8:(d + 1) * 8], start=True, stop=True), r=["onesf", "DEC1"], w=["pb7"])
                P.dve(lambda e, d=d: e.tensor_copy(out=B_S[:, :, d * 8:(d + 1) * 8], in_=pb[6][:, 0:272].rearrange("p (c g) -> p c g", g=8)), r=["pb6"], w=["DEC2"])
                P.dve(lambda e, d=d: e.tensor_copy(out=BL[:, :, d * 8:(d + 1) * 8], in_=pb[7][:, 0:272].rearrange("p (c g) -> p c g", g=8)), r=["pb7"], w=["DEC5"])
            P.act(lambda e: e.activation(out=EBS, in_=B_S, func=AF.Exp), r=["DEC2"], w=["DEC4"])
            P.dve(lambda e: e.tensor_tensor(out=W_S, in0=BL, in1=B_S, op=ALU.subtract), r=["DEC2", "DEC5"], w=["DEC6"])
            P.act(lambda e: e.activation(out=EBL, in_=BL, func=AF.Exp), r=["DEC5", "DEC6"], w=["DEC5"])
            P.act(lambda e: e.activation(out=W_S, in_=W_S, func=AF.Exp), r=["DEC6"], w=["DEC6"])
            P.dve(lambda e: e.tensor_tensor(out=W_S, in0=W_S, in1=DT, op=ALU.mult), r=["DEC6", "DEC0"], w=["DEC6"])
            BT = qT; CT = kT
            btok = XB[:, :].rearrange("p (c n) -> p c n", n=128)
            for (blk, dstb) in [(8, BT), (9, CT)]:
                proj_fm(blk, G[3], "G3")
                for sg_ in range(2):
                    a_, b_ = SEGS[sg_]
                    emit_conv_seg(P, G[3], "G3", G[2], "G2", cws[:, blk - 4, :], cbs[:, blk - 4:blk - 3], ["cws", "cbs"], sg_)
                    P.act(lambda e, dstb=dstb, a_=a_, b_=b_: e.activation(out=dstb[:, a_:b_], in_=G[2][:, a_:b_], func=AF.Silu), r=["G2/%d" % sg_], w=["QA"])
            pT = pb[7][:].bitcast(BF16)
            for c4 in range(0, NCH, 4):
                nn = min(4, NCH - c4)
                for cc in range(nn):
                    c = c4 + cc
                    P.pe(lambda e, c=c, cc=cc: e.transpose(pT[:, cc * 128:(cc + 1) * 128], BT[:, c * 128:(c + 1) * 128], identb[:]), r=["QA", "identb"], w=["pb7"])
                P.act(lambda e, c4=c4, nn=nn: e.activation(out=btok[:, c4:c4 + nn, :].rearrange("p c d -> p (c d)"), in_=pT[:, 0:nn * 128], func=AF.Copy),
                      r=["pb7"], w=XBK)
            xtok = [G[0][:, 0:17 * 256].rearrange("p (c v) -> p c v", v=256), G[1][:, 0:17 * 256].rearrange("p (c v) -> p c v", v=256)]

            def Xv(c):
                return [G[0], G[1]][c // 17][:, (c % 17) * 256:(c % 17 + 1) * 256]

            for hf in range(2):
                for xb in range(2):
                    blk = 4 + 2 * hf + xb
                    proj_fm(blk, G[3], "G3")
                    for sg_ in range(2):
                        a_, b_ = SEGS[sg_]
                        emit_conv_seg(P, G[3], "G3", G[2], "G2", cws[:, blk - 4, :], cbs[:, blk - 4:blk - 3], ["cws", "cbs"], sg_)
                        P.act(lambda e, a_=a_, b_=b_: e.activation(out=G[3][:, a_:b_], in_=G[2][:, a_:b_], func=AF.Silu), r=["G2/%d" % sg_], w=["G3/%d" % sg_])
                    for c4 in range(0, NCH, 4):
                        nn = min(4, NCH - c4)
                        for cc in range(nn):
                            c = c4 + cc
                            P.pe(lambda e, c=c, cc=cc: e.transpose(pb[6][:, cc * 128:(cc + 1) * 128], G[3][:, c * 128:(c + 1) * 128], identf[:]), r=["G3", "identf"], w=["pb6"])
                        for cc in range(nn):
                            c = c4 + cc
                            P.act(lambda e, c=c, cc=cc, xb=xb: e.activation(out=Xv(c)[:, xb * 128:(xb + 1) * 128], in_=pb[6][:, cc * 128:(cc + 1) * 128], func=AF.Copy),
                                  r=["pb6"], w=["G%d" % (c // 17)])
                P.dve(lambda e: e.memset(G[2][:, :], 0.0), w=["G2"])
                P.dve(lambda e: e.memset(G[3][:, :], 0.0), w=["G3"])
                for i in range(NCH):
                    caps = []
                    for d in range(2):
                        with P.capture() as cap:
                            c = orders[d][i]
                            hs = slice(d * 8 + 4 * hf, d * 8 + 4 * hf + 4)
                            pA = pb[0 + d]; pB = pb[2 + d]; pC = pb[4 + d]
                            kA, kB, kC = "pb%d" % d, "pb%d" % (2 + d), "pb%d" % (4 + d)
                            hk = "G%d" % (2 + c // 17); xk = "G%d" % (c // 17)
                            Bc = BT[:, c * 128:(c + 1) * 128]; Cc = CT[:, c * 128:(c + 1) * 128]
                            P.pe(lambda e, pA=pA, Bc=Bc, Cc=Cc: e.matmul(pA[:, 0:128], lhsT=Bc, rhs=Cc, start=True, stop=True), r=["QA"], w=[kA])
                            P.dve(lambda e, d=d, pA=pA: e.tensor_tensor(out=CBm[d][:], in0=pA[:, 0:128], in1=masks[d][:], op=ALU.mult), r=[kA, "triU", "triL"], w=["CBm%d" % d])
                            R = tmp[d]; Rk = "tmp%d" % d
                            P.pool(lambda e, R=R, d=d, c=c, hs=hs: e.tensor_tensor(out=R[:].rearrange("p (e t) -> p e t", e=4), in0=masks[d][:].unsqueeze(1).to_broadcast([128, 4, 128]),
                                                                                  in1=LA[:, c, hs].unsqueeze(2).to_broadcast([128, 4, 128]), op=ALU.mult), r=["triU", "triL", "DEC1"], w=[Rk])
                            P.pe(lambda e, pB=pB, R=R: e.matmul(pB[:, :], lhsT=onesf[:], rhs=R[:], start=True, stop=True), r=["onesf", Rk], w=[kB])
                            Dl = tmp[2 + d]; Dk = "tmp%d" % (2 + d)
                            for e4 in range(4):
                                bcol = B_S[:, c, d * 8 + 4 * hf + e4:d * 8 + 4 * hf + e4 + 1]
                                P.dve(lambda e, pB=pB, Dl=Dl, e4=e4, bcol=bcol: e.tensor_scalar(out=Dl[:, e4 * 128:(e4 + 1) * 128], in0=pB[:, e4 * 128:(e4 + 1) * 128], scalar1=bcol, scalar2=0.0,
                                                                                                op0=ALU.subtract, op1=ALU.min), r=[kB, "DEC2"], w=[Dk])
                            P.act(lambda e, Dl=Dl: e.activation(out=Dl[:], in_=Dl[:], func=AF.Exp), r=[Dk], w=[Dk])
                            P.dve(lambda e, d=d, Dl=Dl: e.tensor_tensor(out=MT[d][:].rearrange("p (e t) -> p e t", e=4), in0=Dl[:].rearrange("p (e t) -> p e t", e=4),
                                                                        in1=CBm[d][:].unsqueeze(1).to_broadcast([128, 4, 128]), op=ALU.mult), r=[Dk, "CBm%d" % d], w=["MT%d" % d])
                            for e4 in range(4):
                                hi = d * 8 + 4 * hf + e4
                                P.act(lambda e, d=d, c=c, e4=e4, hi=hi: e.activation(out=xdt[d][:, e4 * 64:(e4 + 1) * 64], in_=Xv(c)[:, e4 * 64:(e4 + 1) * 64], func=AF.Identity,
                                                                                     scale=DT[:, c, hi:hi + 1]), r=[xk, "DEC0"], w=["xdt%d" % d])
                            for e4 in range(4):
                                hi = d * 8 + 4 * hf + e4
                                P.act(lambda e, d=d, c=c, e4=e4, hi=hi: e.activation(out=xw[d][:, e4 * 64:(e4 + 1) * 64], in_=Xv(c)[:, e4 * 64:(e4 + 1) * 64], func=AF.Identity,
                                                                                     scale=W_S[:, c, hi:hi + 1]), r=[xk, "DEC6"], w=["xw%d" % d])
                            for e4 in range(4):
                                P.pe(lambda e, pC=pC, d=d, e4=e4: e.matmul(pC[:, e4 * 64:(e4 + 1) * 64], lhsT=MT[d][:, e4 * 128:(e4 + 1) * 128], rhs=xdt[d][:, e4 * 64:(e4 + 1) * 64], start=True, stop=True),
                                     r=["MT%d" % d, "xdt%d" % d], w=[kC])
                            if i > 0:
                                P.pe(lambda e, pC=pC, d=d, Cc=Cc: e.matmul(pC[:, 256:512], lhsT=Cc, rhs=Sbf[d][:], start=True, stop=True), r=["QA", "Sbf%d" % d], w=[kC])
                                P.dve(lambda e, pC=pC, d=d, c=c, hs=hs, Dl=Dl: e.tensor_tensor(out=Dl[:, 0:256].rearrange("p (e q) -> p e q", e=4), in0=pC[:, 256:512].rearrange("p (e q) -> p e q", e=4),
                                                                                        in1=EBS[:, c, hs].unsqueeze(2).to_broadcast([128, 4, 64]), op=ALU.mult), r=[kC, "DEC4", "MT%d" % d], w=[Dk])
                                P.dve(lambda e, Dl=Dl, c=c: e.tensor_tensor(out=Hv(c), in0=Hv(c), in1=Dl[:, 0:256], op=ALU.add), r=[Dk, hk], w=[hk])
                            P.dve(lambda e, pC=pC, c=c: e.tensor_tensor(out=Hv(c), in0=Hv(c), in1=pC[:, 0:256], op=ALU.add), r=[kC, hk], w=[hk])
                            if i < NCH - 1:
                                P.pe(lambda e, pA=pA, c=c, d=d: e.matmul(pA[:, 256:512], lhsT=btok[:, c, :], rhs=xw[d][:], start=True, stop=True), r=XBK + ["xw%d" % d], w=[kA])
                                if i == 0:
                                    P.dve(lambda e, pA=pA, d=d: e.tensor_copy(out=Sacc[d][:], in_=pA[:, 256:512]), r=[kA], w=["Sacc%d" % d])
                                else:
                                    P.dve(lambda e, d=d, c=c, hs=hs: e.tensor_tensor(out=Sacc[d][:].rearrange("p (e q) -> p e q", e=4), in0=Sacc[d][:].rearrange("p (e q) -> p e q", e=4),
                                                                                     in1=EBL[:, c, hs].unsqueeze(2).to_broadcast([128, 4, 64]), op=ALU.mult), r=["Sacc%d" % d, "DEC5"], w=["Sacc%d" % d])
                                    P.dve(lambda e, pA=pA, d=d: e.tensor_tensor(out=Sacc[d][:], in0=Sacc[d][:], in1=pA[:, 256:512], op=ALU.add), r=[kA, "Sacc%d" % d], w=["Sacc%d" % d])
                                P.act(lambda e, d=d: e.activation(out=Sbf[d][:], in_=Sacc[d][:], func=AF.Copy), r=["Sacc%d" % d], w=["Sbf%d" % d])
                            emit_pe_warm(P, pb[7], onesb, hT, 3)
                        caps.append(cap)
                    P.add_interleaved(caps)
                for gi in range(2):
                    for e4 in range(4):
                        hv = Hm[gi][:, 0:17 * 256].rearrange("p (c v) -> p c v", v=256)[:, :, e4 * 64:(e4 + 1) * 64]
                        xv = [G[0], G[1]][gi][:, 0:17 * 256].rearrange("p (c v) -> p c v", v=256)[:, :, e4 * 64:(e4 + 1) * 64]
                        P.dve(lambda e, hv=hv, xv=xv, e4=e4, hf=hf: e.scalar_tensor_tensor(out=hv, in0=xv, scalar=sDs[:, 4 * hf + e4:4 * hf + e4 + 1], in1=hv, op0=ALU.mult, op1=ALU.add),
                              r=["G%d" % gi, "G%d" % (2 + gi), "sDs"], w=["G%d" % (2 + gi)])
                gate_out(wz[:, :, hf * 256:(hf + 1) * 256], AF.Silu, 8 + 4 * s + 2 * hf)

        for s_ in range(2):
            half(s_, drs[s_])
        P.emit()


def mix1_consts(I, s):
    import ml_dtypes
    W = I["cd_in_w"][0]
    h0, h1 = 2 * s, 2 * s + 1
    blocks = [np.arange(128 * h0, 128 * h0 + 128), 512 + np.arange(128 * h0, 128 * h0 + 128),
              np.arange(128 * h1, 128 * h1 + 128), 512 + np.arange(128 * h1, 128 * h1 + 128)]
    xoff = 4128
    blocks += [xoff + 512 * s + 128 * n + np.arange(128) for n in range(4)]
    blocks += [xoff + 1024 + 128 * s + np.arange(128), xoff + 1280 + 128 * s + np.arange(128)]
    d = {}
    d["wfm"] = np.stack([kblk(W[:, c]) for c in blocks], 0)
    d["wga"] = kblk(W[:, 3072:3104])
    d["wv"] = np.stack([kblk(W[:, 1024 + 256 * h:1024 + 256 * h + 256]) for h in (h0, h1)], 0)
    d["wr"] = kblk(W[:, 2048 + 512 * s:2048 + 512 * s + 512])
    d["wz"] = kblk(W[:, 3104 + 512 * s:3104 + 512 * s + 512])
    dtc = [5664 + dd * 16 + 8 * s + e for dd in range(2) for e in range(8)]
    d["wdt"] = kblk(W[:, dtc])
    al = np.zeros((64, 256), np.float32)
    for dd in range(2):
        al[32 * dd:32 * dd + 16] = I["g_alpha_w"][0][dd][:, 256 * s:256 * s + 256]
        al[32 * dd + 16] = I["g_alpha_b"][0][dd][256 * s:256 * s + 256]
    d["alph"] = al
    d["gnorm"] = np.ascontiguousarray(np.broadcast_to(I["g_norm_w"][0][512 * s:512 * s + 512][None, :], (128, 512)))
    cw = np.zeros((128, 6, 4), np.float32); cb = np.zeros((128, 6), np.float32)
    for bi in range(6):
        cols = blocks[4 + bi] - xoff
        cw[:, bi, :] = I["s_conv_w"][0][:, cols].T
        cb[:, bi] = I["s_conv_b"][0][cols]
    d["cw"] = cw; d["cb"] = cb
    bc = lambda v: np.ascontiguousarray(np.broadcast_to(np.asarray(v, np.float32)[None, :], (128, len(v))))
    d["dtb"] = bc([I["s_dt_bias"][0][dd, 8 * s + e] for dd in range(2) for e in range(8)])
    d["alog"] = bc([I["s_A_log"][0][dd, 8 * s + e] for dd in range(2) for e in range(8)])
    d["sD"] = bc(I["s_D"][0][8 * s:8 * s + 8])
    d["identf"] = np.eye(128, dtype=np.float32)
    return d


def build_fused():
    _PHASE[0] = 0
    nc = bass.Bass("TRN2", target_bir_lowering=False)
    mkdr = lambda sfx: (lambda name, shape, dt=F32: nc.dram_tensor(name + sfx, shape, dt, kind="ExternalInput").ap())
    xT = nc.dram_tensor("xT", [128, 8, T_ALL], F32, kind="ExternalInput").ap()
    msel = nc.dram_tensor("msel", [128, 2], F32, kind="ExternalInput").ap()
    xo = nc.dram_tensor("xo", [128, 8, 2048], F32, kind="ExternalOutput").ap()
    mix0s = nc.dram_tensor("mix0s", [128, 16, T_ALL], BF16, kind="Internal").ap()
    mix1s = nc.dram_tensor("mix1s", [128, 16, T_ALL], BF16, kind="Internal").ap()
    x1s = nc.dram_tensor("x1s", [128, 8, T_ALL], F32, kind="Internal").ap()
    with ExitStack() as es:
        P = Prog(nc, es)
        pb = [es.enter_context(nc.psum_tensor("pb%d" % i, [128, 512], F32)) for i in range(8)]
        phase_mix0(nc, P, pb, [mkdr("_m0s0"), mkdr("_m0s1")], xT, mix0s)
        phase_post(nc, P, pb, 0, mkdr("_p0"), TOK_TILES,
                   lambda t0, n: [xT[:, :, t0:t0 + n]], lambda t0, n: [mix0s[:, :, t0:t0 + n]], lambda t0, n: x1s[:, :, t0:t0 + n])
        phase_mix1(nc, P, pb, [mkdr("_m1s0"), mkdr("_m1s1")], x1s, mix1s)
        lat = lambda a, t0, n: [a[:, :, 256 + 2048 * r + t0:256 + 2048 * r + t0 + n] for r in range(2)]
        phase_post(nc, P, pb, 1, mkdr("_p1"), [(i * 512, 512, 0) for i in range(4)],
                   lambda t0, n: lat(x1s, t0, n), lambda t0, n: lat(mix1s, t0, n), lambda t0, n: xo[:, :, t0:t0 + n], msel_d=msel)
    return nc


def kernel(**I):
    I = {k: np.asarray(v) for k, v in I.items()}
    cores = list(range(8))
    shared = {}
    for h in range(2):
        d = dict(mix_common_consts(0, I)); d.update(mix0_consts(I, h))
        shared.update({k + "_m0s%d" % h: v for k, v in d.items()})
        d = dict(mix_common_consts(1, I)); d.update(mix1_consts(I, h))
        shared.update({k + "_m1s%d" % h: v for k, v in d.items()})
    shared.update({k + "_p0": v for k, v in post_consts(0, I).items()})
    shared.update({k + "_p1": v for k, v in post_consts(1, I).items()})
    in_maps = []
    for core in cores:
        b, s = core // 2, core % 2
        d = dict(shared)
        d["xT"] = stream_fm(I["x"], I["ctx"], b)
        cT = c_cols(I, b)
        for sfx in ("_m0s0", "_m0s1", "_m1s0", "_m1s1", "_p0", "_p1"):
            d["cT" + sfx] = cT
        m = np.zeros((128, 2), np.float32); m[:, s] = 1.0
        d["msel"] = m
        in_maps.append(d)
    res = run_bass_kernel_spmd(build_fused(), in_maps, core_ids=cores).results
    out = np.zeros((4, 4096, 1024), np.float32)
    for core in cores:
        b, s = core // 2, core % 2
        out[b, 2048 * s:2048 * s + 2048] = fm_inv(res[core]["xo"])
    return out
```

```python
import numpy as np
import concourse.bass as bass
import concourse.mybir as mybir
from concourse.bass_utils import run_bass_kernel_spmd
from concourse.alu_op_type import AluOpType as ALU
from contextlib import ExitStack

AF = mybir.ActivationFunctionType
F32 = mybir.dt.float32
BF16 = mybir.dt.bfloat16
AX = mybir.AxisListType

N_DMA_SLOTS = 40


class Prog:
    ENGS = ("pe", "act", "dve", "pool", "sp")

    def __init__(self, nc, es):
        self.nc = nc
        self.ops = []
        self.final = []
        self.esem = {e: es.enter_context(nc.semaphore("s_" + e)) for e in self.ENGS}
        self.dsem = [es.enter_context(nc.semaphore("d%d" % k)) for k in range(N_DMA_SLOTS)]
        self.cnt = {e: 0 for e in self.ENGS}
        self.dcnt = [0] * N_DMA_SLOTS
        self.nslot = 0

    SEG_KEYS = ("G0", "G1", "G2", "G3", "QK", "XB")

    @classmethod
    def _expand(cls, keys):
        out = []
        for k in keys:
            if k in cls.SEG_KEYS:
                out += [k + "/0", k + "/1"]
            else:
                out.append(k)
        return tuple(out)

    def op(self, eng, fn, r=(), w=(), dma=False):
        self.ops.append(dict(eng=eng, fn=fn, r=self._expand(r), w=self._expand(w), dma=dma))
        return len(self.ops) - 1

    def pe(self, fn, r=(), w=()):
        return self.op("pe", fn, r, w)

    def act(self, fn, r=(), w=()):
        return self.op("act", fn, r, w)

    def dve(self, fn, r=(), w=()):
        return self.op("dve", fn, r, w)

    def pool(self, fn, r=(), w=()):
        return self.op("pool", fn, r, w)

    def dma(self, q, out, in_, r=(), w=(), final=False):
        i = self.op(q, lambda e: e.dma_start(out=out, in_=in_), r, w, dma=True)
        return i

    class _Cap:
        def __init__(self, prog):
            self.prog = prog

        def __enter__(self):
            self.saved = self.prog.ops
            self.prog.ops = []
            self.ops = None
            return self

        def __exit__(self, *a):
            self.ops = self.prog.ops
            self.prog.ops = self.saved
            return False

    def capture(self):
        return Prog._Cap(self)

    def add_interleaved(self, caps):
        lists = [c.ops for c in caps]
        n = max(len(l) for l in lists)
        for j in range(n):
            for l in lists:
                if j < len(l):
                    self.ops.append(l[j])

    def emit(self):
        nc = self.nc
        ops = self.ops
        self.ops = []
        last_w = {}
        readers = {}
        slot_last = {}
        for i, o in enumerate(ops):
            deps = set()
            for k in o["r"]:
                if k in last_w:
                    deps.add(last_w[k])
                if k.startswith("pb"):
                    deps.update(j for j in readers.get(k, ()) if ops[j]["eng"] != o["eng"])
            for k in o["w"]:
                if k in last_w:
                    deps.add(last_w[k])
                deps.update(readers.get(k, ()))
            if o["dma"]:
                s = self.nslot % N_DMA_SLOTS
                self.nslot += 1
                o["slot"] = s
                if s in slot_last:
                    deps.add(slot_last[s])
                slot_last[s] = i
            deps.discard(i)
            if o["eng"] == "pe":
                deps = {d for d in deps if not (ops[d]["eng"] == "pe")}
            o["deps"] = deps
            for k in o["r"]:
                readers.setdefault(k, []).append(i)
            for k in o["w"]:
                last_w[k] = i
                readers[k] = []
        needed = set()
        for o in ops:
            needed.update(o["deps"])
        esem, dsem, cnt, dcnt = self.esem, self.dsem, self.cnt, self.dcnt
        for i, o in enumerate(ops):
            if o["dma"]:
                dcnt[o["slot"]] += 16
                o["tok"] = (("d", o["slot"]), dsem[o["slot"]], dcnt[o["slot"]])
            elif i in needed:
                cnt[o["eng"]] += 1
                o["tok"] = (("e", o["eng"]), esem[o["eng"]], cnt[o["eng"]])
            else:
                o["tok"] = None
        dfinal = list(dcnt)
        with nc.Block() as block:
            def run(engname, eng):
                waited = {}
                for i, o in enumerate(ops):
                    if o["eng"] != engname:
                        continue
                    need = {}
                    for d in o["deps"]:
                        key, sem, val = ops[d]["tok"]
                        if waited.get(key, 0) < val:
                            if need.get(key, (None, 0))[1] < val:
                                need[key] = (sem, val)
                    for key, (sem, val) in need.items():
                        eng.wait_ge(sem, val)
                        waited[key] = val
                    inst = o["fn"](eng)
                    if o["tok"] is not None:
                        inst.then_inc(o["tok"][1], 16 if o["dma"] else 1)
                if engname == "sp":
                    for k in range(N_DMA_SLOTS):
                        if dfinal[k] > 0:
                            eng.wait_ge(dsem[k], dfinal[k])

            @block.tensor
            def _(e):
                run("pe", e)

            @block.scalar
            def _(e):
                run("act", e)

            @block.vector
            def _(e):
                run("dve", e)

            @block.gpsimd
            def _(e):
                run("pool", e)

            @block.sync
            def _(e):
                run("sp", e)


D = 1024
EPS = 1e-6


_PHASE = [0]


def _mk(nc, es):
    _PHASE[0] += 1
    pfx = "p%d_" % _PHASE[0]
    sb = lambda name, shape, dt=F32: es.enter_context(nc.sbuf_tensor(pfx + name, shape, dt))
    ps = lambda name, shape, dt=F32: es.enter_context(nc.psum_tensor(pfx + name, shape, dt))
    return sb, ps


def emit_pe_warm(P, pbw, onesb, hT, n):
    for _ in range(n):
        P.pe(lambda e: e.matmul(pbw[:, 0:512], lhsT=onesb[:], rhs=hT[:, 0, 0:512], start=True, stop=True), r=["onesb", "hT"], w=["pb7"])


def emit_tok2fm(P, pb7, identb, ob, obkey, obT, obTkey, dst_ap):
    pT = pb7[:].bitcast(BF16)
    for cc in range(2):
        for chb in range(2):
            q = chb * 2 + cc
            P.pe(lambda e, cc=cc, chb=chb, q=q: e.transpose(pT[:, q * 128:(q + 1) * 128], ob[:, cc * 256 + chb * 128:cc * 256 + (chb + 1) * 128], identb[:]),
                 r=[obkey, "identb"], w=["pb7"])
    P.act(lambda e: e.activation(out=obT[:], in_=pT[:, 0:512], func=AF.Copy), r=["pb7"], w=[obTkey])
    P.dma("sp", dst_ap, obT[:].rearrange("p (b t) -> p b t", b=2), r=[obTkey])


def emit_modulation(P, nc, sb, pb7, modw, modb, cT, nch, mwbufs):
    cs = sb("cs", [128, 8, 2]); csr = sb("csr", [128, 8, 2]); mb = sb("mb", [128, nch]); modT = sb("modT", [128, nch, 2])
    mw = [b for b, _ in mwbufs]; mwk = [k for _, k in mwbufs]
    P.dma("sp", csr[:], cT, w=["csr"])
    P.dma("sp", mb[:], modb, w=["mb"])
    P.act(lambda e: e.activation(out=cs[:], in_=csr[:], func=AF.Silu), r=["csr"], w=["cs"])
    for ch in range(nch):
        s = ch % len(mw)
        P.dma("sp", mw[s], modw[ch], w=mwk[s])
        for k in range(8):
            P.pe(lambda e, s=s, k=k, ch=ch: e.matmul(pb7[:, 2 * ch:2 * ch + 2], lhsT=mw[s][:, k, :], rhs=cs[:, k, :],
                                                     start=(k == 0), stop=(k == 7)),
                 r=mwk[s] + ["cs"], w=["pb7"])
    P.dve(lambda e: e.tensor_tensor(out=modT[:], in0=pb7[:, 0:2 * nch].rearrange("p (c t) -> p c t", t=2),
                                    in1=mb[:].unsqueeze(2).to_broadcast([128, nch, 2]), op=ALU.add),
          r=["pb7", "mb"], w=["modT"])
    return modT


def phase_post(nc, P, pb, layer, dr, tiles, xsrc, mixsrc, dst, msel_d=None):
    moe = layer == 1
    NJ = 28 if moe else 22
    NE = 8 if moe else 1
    outw = dr("outw", [128, 16, 1024]); w1 = dr("w1", [NE * NJ, 128, 8, 256]); w2 = dr("w2", [NE * 8, 128, NJ, 128])
    modw = dr("modw", [32, 128, 8, 128]); modb = dr("modb", [128, 32]); cT = dr("cT", [128, 8, 2]); gT = dr("gT", [128, 3, 8])
    if moe:
        routw = dr("routw", [128, 8, 8]); routb = dr("routb", [128, 8]); identd = dr("ident", [128, 128]); seld = dr("sel", [8, 1024])
        snwd = dr("snw", [128, 8])
    with ExitStack() as es:
        sb, ps = _mk(nc, es)
        ow = sb("ow", [128, 16, 1024], BF16); mx = sb("mx", [128, 16, 512], BF16); xl = sb("xl", [128, 8, 512])
        y = sb("y", [128, 8, 512]); sq = sb("sq", [128, 8, 512], BF16); h2 = sb("h2", [128, 8, 512], BF16)
        hid = sb("hid", [128, NJ, 512], BF16)
        NW1, NW2 = (3, 3) if moe else (6, 6)
        w1b = [sb("w1b%d" % i, [128, 8, 256], BF16)[:] for i in range(NW1)]
        w2b = [sb("w2b%d" % i, [128, NJ, 128], BF16)[:] for i in range(NW2)]
        w1k = [["w1b%d" % i] for i in range(NW1)]; w2k = [["w2b%d" % i] for i in range(NW2)]
        tmp = [sb("tmp%d" % i, [128, 512]) for i in range(2)]
        sg = [sb("sg%d" % i, [128, 512]) for i in range(2)]
        rstd = sb("rstd", [128, 512]); gsb = sb("gsb", [128, 3, 8])
        onesb = sb("onesb", [128, 128], BF16); epsc = sb("epsc", [128, 1])
        A1 = sb("A1", [128, 8, 2]); B2 = sb("B2", [128, 8, 2]); A3 = sb("A3", [128, 8, 2])
        if moe:
            h2f = sb("h2f", [128, 8, 512]); Gb = sb("Gb", [128, 8, 512]); rw = sb("rw", [128, 8, 8]); rb = sb("rb", [128, 8])
            ident = sb("ident_sb", [128, 128]); sel = sb("sel_sb", [8, 1024]); Lg = sb("Lg", [128, 4, 8]); mx8 = sb("mx8", [128, 4, 8])
            msk = sb("msk", [128, 4, 8]); Eg = sb("Eg", [128, 4, 8]); den = sb("den", [128, 4]); gTs = sb("gTs", [8, 512])
            snw = sb("snw_sb", [128, 8])
            for i in range(2):
                v = h2f[:, 2 * i:2 * i + 2, :].rearrange("p a b -> p (a b)").bitcast(BF16).rearrange("p (k c) -> p k c", c=256)
                w1b.append(v); w1k.append(["h2f%d" % (2 * i), "h2f%d" % (2 * i + 1)])
            v = h2f[:, 4:8, :].rearrange("p a b -> p (a b)").bitcast(BF16)[:, 0:NJ * 128].rearrange("p (j c) -> p j c", c=128)
            w2b.append(v); w2k.append(["h2f%d" % m for m in range(4, 8)])
            NW1 = len(w1b); NW2 = len(w2b)
            P.dma("sp", snw[:], snwd, w=["snw"])
            P.dma("sp", rw[:], routw, w=["rw"]); P.dma("sp", rb[:], routb, w=["rb"])
            P.dma("sp", ident[:], identd, w=["ident"]); P.dma("sp", sel[:], seld, w=["sel"])
        P.dve(lambda e: e.memset(onesb[:], 1.0), w=["onesb"])
        P.dve(lambda e: e.memset(epsc[:], EPS), w=["epsc"])
        P.dma("sp", gsb[:], gT, w=["gsb"])
        if msel_d is not None:
            msel = sb("msel_sb", [128, 2])
            P.dma("sp", msel[:], msel_d, w=["msel"])
        P.dma("pool", ow[:], outw, w=["ow"])
        mwb = [(y[:, 2 * i:2 * i + 2, :].rearrange("p a (b c) -> p (a b) c", c=128), ["y%d" % (2 * i), "y%d" % (2 * i + 1)]) for i in range(4)]
        modT = emit_modulation(P, nc, sb, pb[7], modw, modb, cT, 32, mwb)
        gbc = lambda i: gsb[:, i, :].unsqueeze(2).to_broadcast([128, 8, 2])
        P.dve(lambda e: e.tensor_tensor(out=A1[:], in0=modT[:, 0:8, :], in1=gbc(0), op=ALU.mult), r=["modT", "gsb"], w=["A1"])
        P.dve(lambda e: e.scalar_tensor_tensor(out=B2[:], in0=modT[:, 16:24, :], scalar=1.0, in1=gbc(1), op0=ALU.add, op1=ALU.mult),
              r=["modT", "gsb"], w=["B2"])
        P.dve(lambda e: e.tensor_tensor(out=A3[:], in0=modT[:, 24:32, :], in1=gbc(2), op=ALU.mult), r=["modT", "gsb"], w=["A3"])
        w1cnt = [0]; w2cnt = [0]

        def stats(n, srckey):
            for m in range(8):
                P.pe(lambda e, m=m: e.matmul(pb[6][:, :n], lhsT=onesb[:], rhs=sq[:, m, :n], start=(m == 0), stop=(m == 7)),
                     r=["onesb", "sq%d" % m], w=["pb6"])
            P.act(lambda e: e.activation(out=rstd[:, :n], in_=pb[6][:, :n], func=AF.Sqrt, bias=epsc[:, 0:1], scale=1.0 / D),
                  r=["pb6", "epsc"], w=["rstd"])
            P.dve(lambda e: e.reciprocal(out=rstd[:, :n], in_=rstd[:, :n]), r=["rstd"], w=["rstd"])

        def resid_add(n, col, A, Akey):
            for m in range(8):
                t = tmp[m % 2]; tk = "tmp%d" % (m % 2)
                P.dve(lambda e, m=m, t=t: e.scalar_tensor_tensor(out=t[:, :n], in0=y[:, m, :n], scalar=A[:, m, col:col + 1], in1=rstd[:, :n],
                                                                   op0=ALU.mult, op1=ALU.mult), r=["y%d" % m, Akey, "rstd"], w=[tk])
                P.dve(lambda e, m=m, t=t: e.tensor_tensor(out=xl[:, m, :n], in0=xl[:, m, :n], in1=t[:, :n], op=ALU.add),
                      r=[tk, "xl%d" % m], w=["xl%d" % m])

        def do_tile(t0, n, col):
            xs_ = xsrc(t0, n); ms_ = mixsrc(t0, n)
            xlk = ["xl%d" % m for m in range(8)]; yk = ["y%d" % m for m in range(8)]; hk16 = ["hid%d" % j for j in range(16)]
            P.dma("sp", xl[:, :, :n], xs_[0], w=xlk)
            P.dma("sp", mx[:, :, :n], ms_[0], w=["mx"])
            if len(xs_) == 2:
                P.dma("sp", y[:, :, :n], xs_[1], w=yk)
                P.dma("sp", hid[:, 0:16, :n], ms_[1], w=hk16)
                P.dve(lambda e: e.tensor_scalar(out=xl[:, :, :n], in0=xl[:, :, :n], scalar1=msel[:, 0:1], scalar2=None, op0=ALU.mult), r=xlk + ["msel"], w=xlk)
                P.dve(lambda e: e.scalar_tensor_tensor(out=xl[:, :, :n], in0=y[:, :, :n], scalar=msel[:, 1:2], in1=xl[:, :, :n], op0=ALU.mult, op1=ALU.add),
                      r=xlk + yk + ["msel"], w=xlk)
                P.dve(lambda e: e.tensor_scalar(out=mx[:, :, :n], in0=mx[:, :, :n], scalar1=msel[:, 0:1], scalar2=None, op0=ALU.mult), r=["mx", "msel"], w=["mx"])
                P.dve(lambda e: e.scalar_tensor_tensor(out=mx[:, :, :n], in0=hid[:, 0:16, :n], scalar=msel[:, 1:2], in1=mx[:, :, :n], op0=ALU.mult, op1=ALU.add),
                      r=["mx", "msel"] + hk16, w=["mx"])
            if moe:
                for m in range(8):
                    P.act(lambda e, m=m: e.activation(out=sq[:, m, :n], in_=mx[:, 8 + m, :n], func=AF.Square), r=["mx"], w=["sq%d" % m])
                stats(n, "ssd")
                for m in range(8):
                    P.dve(lambda e, m=m: e.scalar_tensor_tensor(out=mx[:, 8 + m, :n], in0=mx[:, 8 + m, :n], scalar=snw[:, m:m + 1], in1=rstd[:, :n],
                                                                 op0=ALU.mult, op1=ALU.mult), r=["mx", "snw", "rstd"], w=["mx"])
            for m in range(8):
                pbm = pb[m % 2]; pk = "pb%d" % (m % 2)
                for k in range(16):
                    P.pe(lambda e, m=m, k=k, pbm=pbm: e.matmul(pbm[:, :n], lhsT=ow[:, k, m * 128:(m + 1) * 128], rhs=mx[:, k, :n],
                                                               start=(k == 0), stop=(k == 15)), r=["ow", "mx"], w=[pk])
                P.act(lambda e, m=m, pbm=pbm: e.activation(out=y[:, m, :n], in_=pbm[:, :n], func=AF.Copy), r=[pk], w=["y%d" % m])
                P.act(lambda e, m=m, pbm=pbm: e.activation(out=sq[:, m, :n], in_=pbm[:, :n], func=AF.Square), r=[pk], w=["sq%d" % m])
            stats(n, "y")
            resid_add(n, col, A1, "A1")
            for m in range(8):
                P.act(lambda e, m=m: e.activation(out=sq[:, m, :n], in_=xl[:, m, :n], func=AF.Square), r=["xl%d" % m], w=["sq%d" % m])
            stats(n, "xl")
            for m in range(8):
                t = tmp[m % 2]; tk = "tmp%d" % (m % 2)
                P.dve(lambda e, m=m, t=t: e.scalar_tensor_tensor(out=t[:, :n], in0=xl[:, m, :n], scalar=B2[:, m, col:col + 1], in1=rstd[:, :n],
                                                                   op0=ALU.mult, op1=ALU.mult), r=["xl%d" % m, "B2", "rstd"], w=[tk])
                if moe:
                    P.act(lambda e, m=m, t=t: e.activation(out=h2f[:, m, :n], in_=t[:, :n], func=AF.Identity, bias=modT[:, 8 + m, col:col + 1], scale=1.0),
                          r=[tk, "modT"], w=["h2f%d" % m])
                    P.dve(lambda e, m=m: e.tensor_copy(out=h2[:, m, :n], in_=h2f[:, m, :n]), r=["h2f%d" % m], w=["h2_%d" % m])
                else:
                    P.act(lambda e, m=m, t=t: e.activation(out=h2[:, m, :n], in_=t[:, :n], func=AF.Identity, bias=modT[:, 8 + m, col:col + 1], scale=1.0),
                          r=[tk, "modT"], w=["h2_%d" % m])
            h2keys = ["h2_%d" % m for m in range(8)]
            if moe:
                for s4 in range(4):
                    for k in range(8):
                        P.pe(lambda e, s4=s4, k=k: e.matmul(pb[7][:, s4 * 8:(s4 + 1) * 8], lhsT=h2f[:, k, s4 * 128:(s4 + 1) * 128], rhs=rw[:, k, :],
                                                            start=(k == 0), stop=(k == 7)), r=["h2f%d" % k, "rw"], w=["pb7"])
                P.dve(lambda e: e.tensor_tensor(out=Lg[:], in0=pb[7][:, 0:32].rearrange("p (s e) -> p s e", e=8),
                                                in1=rb[:].unsqueeze(1).to_broadcast([128, 4, 8]), op=ALU.add), r=["pb7", "rb"], w=["Lg"])
                for s4 in range(4):
                    P.dve(lambda e, s4=s4: e.max(out=mx8[:, s4, :], in_=Lg[:, s4, :]), r=["Lg"], w=["mx8"])
                P.dve(lambda e: e.tensor_tensor(out=msk[:], in0=Lg[:], in1=mx8[:, :, 1:2].to_broadcast([128, 4, 8]), op=ALU.is_ge),
                      r=["Lg", "mx8"], w=["msk"])
                P.dve(lambda e: e.tensor_tensor(out=Eg[:], in0=Lg[:], in1=mx8[:, :, 0:1].to_broadcast([128, 4, 8]), op=ALU.subtract),
                      r=["Lg", "mx8"], w=["Eg"])
                P.act(lambda e: e.activation(out=Eg[:], in_=Eg[:], func=AF.Exp), r=["Eg"], w=["Eg"])
                P.dve(lambda e: e.tensor_tensor(out=Eg[:], in0=Eg[:], in1=msk[:], op=ALU.mult), r=["Eg", "msk"], w=["Eg"])
                P.dve(lambda e: e.tensor_reduce(out=den[:], in_=Eg[:], axis=AX.X, op=ALU.add), r=["Eg"], w=["den"])
                P.dve(lambda e: e.reciprocal(out=den[:], in_=den[:]), r=["den"], w=["den"])
                P.dve(lambda e: e.tensor_tensor(out=Eg[:], in0=Eg[:], in1=den[:].unsqueeze(2).to_broadcast([128, 4, 8]), op=ALU.mult),
                      r=["Eg", "den"], w=["Eg"])
                for s4 in range(4):
                    P.pe(lambda e, s4=s4: e.transpose(pb[6][0:8, s4 * 128:(s4 + 1) * 128], Eg[:, s4, :], ident[:]), r=["Eg", "ident"], w=["pb6"])
                P.dve(lambda e: e.tensor_copy(out=gTs[:], in_=pb[6][0:8, :]), r=["pb6"], w=["gTs"])
                for ex in range(8):
                    pbm = pb[ex % 2]; pk = "pb%d" % (ex % 2)
                    P.pe(lambda e, ex=ex, pbm=pbm: e.matmul(pbm[:, :n], lhsT=sel[0:8, ex * 128:(ex + 1) * 128], rhs=gTs[0:8, :n], start=True, stop=True),
                         r=["sel", "gTs"], w=[pk])
                    P.act(lambda e, ex=ex, pbm=pbm: e.activation(out=Gb[:, ex, :n], in_=pbm[:, :n], func=AF.Copy), r=[pk], w=["Gb%d" % ex])
            for ex in range(NE):
                for j in range(NJ):
                    s = w1cnt[0] % NW1; w1cnt[0] += 1
                    P.dma("pool", w1b[s], w1[ex * NJ + j], w=w1k[s])
                    for half in range(2):
                        pbi = 2 + 2 * (j % 2) + half
                        for k in range(8):
                            P.pe(lambda e, s=s, k=k, half=half, pbi=pbi: e.matmul(pb[pbi][:, :n], lhsT=w1b[s][:, k, half * 128:(half + 1) * 128],
                                                                                   rhs=h2[:, k, :n], start=(k == 0), stop=(k == 7)),
                                 r=w1k[s] + ["h2_%d" % k], w=["pb%d" % pbi])
                    pg = 2 + 2 * (j % 2)
                    P.act(lambda e, j=j, pg=pg: e.activation(out=sg[j % 2][:, :n], in_=pb[pg][:, :n], func=AF.Silu), r=["pb%d" % pg], w=["sg%d" % (j % 2)])
                    P.dve(lambda e, j=j, pg=pg: e.tensor_tensor(out=hid[:, j, :n], in0=sg[j % 2][:, :n], in1=pb[pg + 1][:, :n], op=ALU.mult),
                          r=["sg%d" % (j % 2), "pb%d" % (pg + 1)], w=["hid%d" % j])
                for m in range(8):
                    s = w2cnt[0] % NW2; w2cnt[0] += 1
                    P.dma("pool", w2b[s], w2[ex * 8 + m], w=w2k[s])
                    pbm = pb[m % 2]; pk = "pb%d" % (m % 2)
                    for j in range(NJ):
                        P.pe(lambda e, s=s, j=j, pbm=pbm: e.matmul(pbm[:, :n], lhsT=w2b[s][:, j, :], rhs=hid[:, j, :n], start=(j == 0), stop=(j == NJ - 1)),
                             r=w2k[s] + ["hid%d" % j], w=[pk])
                    if not moe:
                        P.act(lambda e, m=m, pbm=pbm: e.activation(out=y[:, m, :n], in_=pbm[:, :n], func=AF.Copy), r=[pk], w=["y%d" % m])
                        P.act(lambda e, m=m, pbm=pbm: e.activation(out=sq[:, m, :n], in_=pbm[:, :n], func=AF.Square), r=[pk], w=["sq%d" % m])
                    elif ex == 0:
                        P.dve(lambda e, m=m, pbm=pbm, ex=ex: e.tensor_tensor(out=y[:, m, :n], in0=pbm[:, :n], in1=Gb[:, ex, :n], op=ALU.mult),
                              r=[pk, "Gb%d" % ex], w=["y%d" % m])
                    else:
                        t = tmp[m % 2]; tk = "tmp%d" % (m % 2)
                        P.dve(lambda e, m=m, pbm=pbm, ex=ex, t=t: e.tensor_tensor(out=t[:, :n], in0=pbm[:, :n], in1=Gb[:, ex, :n], op=ALU.mult),
                              r=[pk, "Gb%d" % ex], w=[tk])
                        P.dve(lambda e, m=m, t=t: e.tensor_tensor(out=y[:, m, :n], in0=y[:, m, :n], in1=t[:, :n], op=ALU.add),
                              r=[tk, "y%d" % m], w=["y%d" % m])
                        if ex == NE - 1:
                            P.act(lambda e, m=m: e.activation(out=sq[:, m, :n], in_=y[:, m, :n], func=AF.Square), r=["y%d" % m], w=["sq%d" % m])
            stats(n, "f")
            resid_add(n, col, A3, "A3")
            P.dma("sp", dst(t0, n), xl[:, :, :n], r=["xl%d" % m for m in range(8)], final=True)
        for (t0_, n_, col_) in tiles:
            do_tile(t0_, n_, col_)
        P.emit()


def fm(a):
    T, C = a.shape
    return np.ascontiguousarray(a.T.reshape(C // 128, 128, T).transpose(1, 0, 2))


def fm_inv(b):
    p, kc, T = b.shape
    return np.ascontiguousarray(b.transpose(2, 1, 0).reshape(T, kc * 128))


def kblk(w):
    K, N = w.shape
    return np.ascontiguousarray(w.reshape(K // 128, 128, N).transpose(1, 0, 2))


def w1blk(w, nj):
    return np.ascontiguousarray(w.reshape(8, 128, 2, nj, 128).transpose(3, 1, 0, 2, 4).reshape(nj, 128, 8, 256))


def w2blk(w, nj):
    return np.ascontiguousarray(w.reshape(nj, 128, 8, 128).transpose(2, 1, 0, 3))


def modblk(w, c0, c1):
    n = (c1 - c0) // 128
    return np.ascontiguousarray(w[:, c0:c1].reshape(8, 128, n, 128).transpose(2, 1, 0, 3))


def colvec(v):
    return np.ascontiguousarray(v.reshape(-1, 128).T)


def post_consts(layer, I):
    moe = layer == 1
    d = {}
    if moe:
        d["outw"] = kblk(I["cd_out_w"][0])
        d["w1"] = np.concatenate([w1blk(I["moe_w1"][0, e], 28) for e in range(8)], axis=0)
        d["w2"] = np.concatenate([w2blk(I["moe_w2"][0, e], 28) for e in range(8)], axis=0)
        d["routw"] = kblk(I["router_w"][0])
        d["routb"] = np.ascontiguousarray(np.broadcast_to(I["router_b"][0][None, :], (128, 8)))
        d["ident"] = np.eye(128, dtype=np.float32)
        sel = np.zeros((8, 1024), np.float32)
        for e in range(8):
            sel[e, e * 128:(e + 1) * 128] = 1.0
        d["sel"] = sel
        d["snw"] = colvec(I["s_norm_w"][0])
    else:
        d["outw"] = kblk(I["ab_out_w"][0])
        d["w1"] = w1blk(I["ffn_w1"][0], 22)
        d["w2"] = w2blk(I["ffn_w2"][0], 22)
    d["modw"] = modblk(I["mod_w"][layer], 2048, 6144)
    d["modb"] = colvec(I["mod_b"][layer][2048:6144])
    d["gT"] = np.ascontiguousarray(I["norm_g"][layer][1:4].reshape(3, 8, 128).transpose(2, 0, 1))
    return d


def c_cols(I, b):
    return np.ascontiguousarray(np.stack([I["c"][b], I["c_ctx"]], 0).reshape(2, 8, 128).transpose(2, 1, 0))


T_ALL = 4352
NCH = 34
ORDER_F = list(range(NCH))
ORDER_B = [1, 0] + list(range(NCH - 1, 1, -1))
TOK_TILES = [(0, 256, 1)] + [(256 + 512 * i, 512, 0) for i in range(8)]
LN_DK = float(np.log(128.0 ** -0.5))


def emit_adaln_in(P, nc, sb, pb, xT, modT, g0sb, hT, xin, xinkey, sq, sqkey, tmp, rstd, onesb, epsc, A0):
    P.dve(lambda e: e.scalar_tensor_tensor(out=A0[:], in0=modT[:, 8:16, :], scalar=1.0, in1=g0sb[:].unsqueeze(2).to_broadcast([128, 8, 2]),
                                           op0=ALU.add, op1=ALU.mult), r=["modT", "g0sb"], w=["A0"])

    def tile(t0, n, col):
        P.dma("sp", xin[:, :, :n], xT[:, :, t0:t0 + n], w=[xinkey])
        for k in range(8):
            P.act(lambda e, k=k: e.activation(out=sq[:, k, :n], in_=xin[:, k, :n], func=AF.Square), r=[xinkey], w=[sqkey + str(k)])
        for k in range(8):
            P.pe(lambda e, k=k: e.matmul(pb[6][:, :n], lhsT=onesb[:], rhs=sq[:, k, :n], start=(k == 0), stop=(k == 7)),
                 r=["onesb", sqkey + str(k)], w=["pb6"])
        P.act(lambda e: e.activation(out=rstd[:, :n], in_=pb[6][:, :n], func=AF.Sqrt, bias=epsc[:, 0:1], scale=1.0 / D), r=["pb6", "epsc"], w=["rstd"])
        P.dve(lambda e: e.reciprocal(out=rstd[:, :n], in_=rstd[:, :n]), r=["rstd"], w=["rstd"])
        for k in range(8):
            t = tmp[k % 2]; tk = "tmp%d" % (k % 2)
            P.dve(lambda e, k=k, t=t: e.scalar_tensor_tensor(out=t[:, :n], in0=xin[:, k, :n], scalar=A0[:, k, col:col + 1], in1=rstd[:, :n],
                                                               op0=ALU.mult, op1=ALU.mult), r=[xinkey, "A0", "rstd"], w=[tk])
            P.act(lambda e, k=k, t=t: e.activation(out=hT[:, k, t0:t0 + n], in_=t[:, :n], func=AF.Identity, bias=modT[:, k, col:col + 1], scale=1.0),
                  r=[tk, "modT"], w=["hT"])
    for (t0, n, col) in TOK_TILES:
        tile(t0, n, col)


def emit_conv(P, src, skey, dst, dkey, wcol, bcol, wkeys):
    P.dve(lambda e: e.tensor_scalar(out=dst[:, :], in0=src[:, :], scalar1=wcol[:, 2:3], scalar2=bcol, op0=ALU.mult, op1=ALU.add),
          r=[skey] + wkeys, w=[dkey])
    views = [(lambda a: a[:, 0:256].rearrange("p (r w) -> p r w", w=256), 256),
             (lambda a: a[:, 256:T_ALL].rearrange("p (r w) -> p r w", w=64), 64)]
    for (j, d) in [(0, -2), (1, -1), (3, 1)]:
        for vf, W in views:
            sv = vf(src); dv = vf(dst)
            if d < 0:
                o_ = dv[:, :, -d:W]; i_ = sv[:, :, 0:W + d]
            else:
                o_ = dv[:, :, 0:W - d]; i_ = sv[:, :, d:W]
            P.dve(lambda e, o_=o_, i_=i_, j=j: e.scalar_tensor_tensor(out=o_, in0=i_, scalar=wcol[:, j:j + 1], in1=o_, op0=ALU.mult, op1=ALU.add),
                  r=[skey, dkey] + wkeys, w=[dkey])


SEG_SPLIT = 2304
SEGS = [(0, SEG_SPLIT), (SEG_SPLIT, T_ALL)]


def sk(key, t0):
    return key + ("/0" if t0 < SEG_SPLIT else "/1")


def emit_conv_seg(P, src, skey, dst, dkey, wcol, bcol, wkeys, seg):
    a, b = SEGS[seg]
    sk_, dk_ = skey + "/%d" % seg, dkey + "/%d" % seg
    P.dve(lambda e: e.tensor_scalar(out=dst[:, a:b], in0=src[:, a:b], scalar1=wcol[:, 2:3], scalar2=bcol, op0=ALU.mult, op1=ALU.add),
          r=[sk_] + wkeys, w=[dk_])
    if seg == 0:
        views = [(lambda t: t[:, 0:256].rearrange("p (r w) -> p r w", w=256), 256),
                 (lambda t: t[:, 256:SEG_SPLIT].rearrange("p (r w) -> p r w", w=64), 64)]
    else:
        views = [(lambda t: t[:, SEG_SPLIT:T_ALL].rearrange("p (r w) -> p r w", w=64), 64)]
    for (j, d) in [(0, -2), (1, -1), (3, 1)]:
        for vf, W in views:
            sv = vf(src); dv = vf(dst)
            if d < 0:
                o_ = dv[:, :, -d:W]; i_ = sv[:, :, 0:W + d]
            else:
                o_ = dv[:, :, 0:W - d]; i_ = sv[:, :, d:W]
            P.dve(lambda e, o_=o_, i_=i_, j=j: e.scalar_tensor_tensor(out=o_, in0=i_, scalar=wcol[:, j:j + 1], in1=o_, op0=ALU.mult, op1=ALU.add),
                  r=[sk_, dk_] + wkeys, w=[dk_])


def phase_mix0(nc, P, pb, drs, xT, out_mix):
    dr = drs[0]
    modw = dr("modw", [16, 128, 8, 128])
    modb = dr("modb", [128, 16])
    cT = dr("cT", [128, 8, 2])
    g0 = dr("g0", [128, 8])
    triUd = dr("triU", [128, 128])
    triLd = dr("triL", [128, 128])
    identd = dr("identb", [128, 128], BF16)
    with ExitStack() as es:
        sb, ps = _mk(nc, es)
        obT = sb("obT", [128, 512], BF16)
        hT = sb("hT", [128, 8, T_ALL], BF16)
        G = [sb("G%d" % i, [128, T_ALL]) for i in range(4)]
        QK = sb("QK", [128, T_ALL]); XB = sb("XB", [128, T_ALL], BF16)
        qkb = QK[:].bitcast(BF16)
        qT = qkb[:, 0:T_ALL]; kT = qkb[:, T_ALL:2 * T_ALL]
        VT = G[1][:].bitcast(BF16)
        vtok = VT.rearrange("p (c v) -> p c v", v=256)
        ktok = G[0][:].bitcast(BF16)[:, 0:T_ALL].rearrange("p (c d) -> p c d", d=128)
        tmp = [sb("tmp%d" % i, [128, 512]) for i in range(2)]
        rstd = sb("rstd", [128, 512]); onesb = sb("onesb", [128, 128], BF16); onesf = sb("onesf", [128, 128]); epsc = sb("epsc", [128, 1])
        g0sb = sb("g0sb", [128, 8]); A0 = sb("A0", [128, 8, 2])
        triU = sb("triU_sb", [128, 128]); triL = sb("triL_sb", [128, 128]); identb = sb("identb_sb", [128, 128], BF16)
        wfb = [sb("wfb%d" % i, [128, 8, 128], BF16) for i in range(4)]
        wvb = sb("wvb", [128, 8, 256], BF16); wgb = sb("wgb", [128, 8, 8], BF16)
        gbs = sb("gbs", [128, 8]); cws = sb("cws", [128, 8, 4]); cbs = sb("cbs", [128, 8]); mns = sb("mns", [128, 512])
        lwab = sb("lwab", [128, 2, 4, 128], BF16); lwxb = sb("lwxb", [128, 2, 4, 128], BF16)
        lbas = sb("lbas", [128, 2, 4]); lbxs = sb("lbxs", [128, 2, 4]); lams = sb("lams", [128, 2, 4]); LC = sb("LC", [128, 2, 4])
        GA = sb("GA", [128, NCH, 8]); Lsp = sb("Lsp", [128, NCH, 4]); EX = sb("EX", [128, 4, NCH, 2]); WV = sb("WV", [128, 2, NCH, 2])
        Sacc = [sb("Sacc%d" % i, [128, 257]) for i in range(2)]; Sbf = [sb("Sbf%d" % i, [128, 257], BF16) for i in range(2)]
        PT = [sb("PT%d" % i, [128, 128], BF16) for i in range(2)]; vs = [sb("vs%d" % i, [128, 257], BF16) for i in range(4)]
        dn = [sb("dn%d" % i, [128, 4]) for i in range(2)]
        ssq = sb("ssq", [128, NCH]); osig = tmp; obf = [sb("obf%d" % i, [128, 512], BF16) for i in range(2)]
        hlb = obf
        for (dst, src, key) in [(g0sb, g0, "g0sb"), (triU, triUd, "triU"), (triL, triLd, "triL"), (identb, identd, "identb")]:
            P.dma("sp", dst[:], src, w=[key])
        P.dve(lambda e: e.memset(onesb[:], 1.0), w=["onesb"])
        P.dve(lambda e: e.memset(onesf[:], 1.0), w=["onesf"])
        P.dve(lambda e: e.memset(epsc[:], EPS), w=["epsc"])
        mwb = [(G[1][:, 1024 * i:1024 * (i + 1)].rearrange("p (a b) -> p a b", b=128), ["mwst%d" % i]) for i in range(4)]
        modT = emit_modulation(P, nc, sb, pb[7], modw, modb, cT, 16, mwb)
        xin = G[0][:, 0:4096].rearrange("p (k n) -> p k n", k=8)
        sqv = XB[:, 0:4096].rearrange("p (k n) -> p k n", k=8)
        emit_adaln_in(P, nc, sb, pb, xT, modT, g0sb, hT, xin, "G0", sqv, "XB", tmp, rstd, onesb, epsc, A0)
        def half(s, dr):
            wfm = dr("wfm", [12, 128, 8, 128])
            wv = dr("wv", [2, 128, 8, 256])
            wo = dr("wo", [128, 8, 512])
            wg = dr("wg", [128, 8, 8])
            gbias = dr("gbias", [128, 8])
            cw = dr("cw", [128, 8, 4])
            cb = dr("cb", [128, 8])
            mnorm = dr("mnorm", [128, 512])
            lwa = dr("lwa", [128, 2, 4, 128])
            lwx = dr("lwx", [128, 2, 4, 128])
            lba = dr("lba", [128, 2, 4])
            lbx = dr("lbx", [128, 2, 4])
            llam = dr("llam", [128, 2, 4])
            for (dst, src, key) in [(gbs, gbias, "gbs"), (cws, cw, "cws"), (cbs, cb, "cbs"), (mns, mnorm, "mns"), (lbas, lba, "lbas"), (lbxs, lbx, "lbxs"), (lams, llam, "lams")]:
                P.dma("sp", dst[:], src, w=[key])
            for (dst, src, key) in [(wgb, wg, "wgb"), (lwab, lwa, "lwab"), (lwxb, lwx, "lwxb")]:
                P.dma("pool", dst[:], src, w=[key])
            P.act(lambda e: e.activation(out=LC[:], in_=lams[:], func=AF.Exp, scale=-1.0), r=["lams"], w=["LC"])
            P.act(lambda e: e.activation(out=LC[:], in_=LC[:], func=AF.Ln, bias=1.0), r=["LC"], w=["LC"])
            P.dve(lambda e: e.tensor_scalar(out=LC[:], in0=LC[:], scalar1=-8.0, scalar2=None, op0=ALU.mult), r=["LC"], w=["LC"])
            wfcnt = [0]

            def proj_fm(blk, dst, dkey, evac=None, pbo=0):
                s = wfcnt[0] % 4; wfcnt[0] += 1
                P.dma("pool", wfb[s][:], wfm[blk], w=["wfb%d" % s])
                for ti, (t0, n, col) in enumerate(TOK_TILES):
                    pbi = pbo + ti % 2
                    for k in range(8):
                        P.pe(lambda e, k=k, s=s, t0=t0, n=n, pbi=pbi: e.matmul(pb[pbi][:, :n], lhsT=wfb[s][:, k, :], rhs=hT[:, k, t0:t0 + n],
                                                                              start=(k == 0), stop=(k == 7)), r=["wfb%d" % s, "hT"], w=["pb%d" % pbi])
                    if evac is None:
                        P.act(lambda e, t0=t0, n=n, pbi=pbi: e.activation(out=dst[:, t0:t0 + n], in_=pb[pbi][:, :n], func=AF.Copy), r=["pb%d" % pbi], w=[sk(dkey, t0)])
                    else:
                        evac(ti, t0, n, pbi)

            for c in range(NCH):
                for k in range(8):
                    P.pe(lambda e, c=c, k=k: e.matmul(pb[7][:, c * 8:(c + 1) * 8], lhsT=hT[:, k, c * 128:(c + 1) * 128], rhs=wgb[:, k, :],
                                                      start=(k == 0), stop=(k == 7)), r=["hT", "wgb"], w=["pb7"])
            P.dve(lambda e: e.tensor_tensor(out=GA[:], in0=pb[7][:, 0:NCH * 8].rearrange("p (c g) -> p c g", g=8),
                                            in1=gbs[:].unsqueeze(1).to_broadcast([128, NCH, 8]), op=ALU.add), r=["pb7", "gbs"], w=["GA"])
            P.act(lambda e: e.activation(out=Lsp[:], in_=GA[:, :, 4:8], func=AF.Exp, scale=-1.0), r=["GA"], w=["Lsp"])
            P.act(lambda e: e.activation(out=Lsp[:], in_=Lsp[:], func=AF.Ln, bias=1.0), r=["Lsp"], w=["Lsp"])
            NG = NCH * 2
            for gi, (lh, sl) in enumerate([(triU, slice(0, 2)), (triL, slice(2, 4)), (onesf, slice(0, 2)), (onesf, slice(2, 4))]):
                P.pe(lambda e, gi=gi, lh=lh, sl=sl: e.matmul(pb[6][:, gi * NG:(gi + 1) * NG], lhsT=lh[:], rhs=Lsp[:, :, sl], start=True, stop=True),
                     r=["triU", "triL", "onesf", "Lsp"], w=["pb6"])
            P.act(lambda e: e.activation(out=EX[:].rearrange("p a c h -> p (a c h)"), in_=pb[6][:, 0:4 * NG], func=AF.Exp, scale=-1.0), r=["pb6"], w=["EX"])
            for d in range(2):
                P.dve(lambda e, d=d: e.tensor_tensor(out=WV[:, d], in0=GA[:, :, 2 * d:2 * d + 2],
                                                     in1=pb[6][:, d * NG:(d + 1) * NG].rearrange("p (c h) -> p c h", h=2), op=ALU.add), r=["GA", "pb6", "EX"], w=["WV"])
            lnc = sb("lnc%d" % s, [128, 1])
            P.dve(lambda e: e.memset(lnc[:], LN_DK), w=["lnc"])
            P.act(lambda e: e.activation(out=WV[:], in_=WV[:], func=AF.Exp, bias=lnc[:, 0:1], scale=1.0), r=["WV", "lnc"], w=["WV"])
            masks = [triU, triL]; orders = [ORDER_F, ORDER_B]
            Hm = [G[2], G[3]]

            def Hv(c):
                g = Hm[c // 17]; cc = c % 17
                return g[:, cc * 256:(cc + 1) * 256]

            for hh in range(2):
                caps = []
                for (blk, dstb, ga, gb, pbo) in [(2 * hh, qT, 0, 1, 0), (2 * hh + 1, kT, 2, 3, 2)]:
                    with P.capture() as cap:
                        proj_fm(blk, G[ga], "G%d" % ga, pbo=pbo)
                        for sg_ in range(2):
                            a_, b_ = SEGS[sg_]
                            emit_conv_seg(P, G[ga], "G%d" % ga, G[gb], "G%d" % gb, cws[:, blk, :], cbs[:, blk:blk + 1], ["cws", "cbs"], sg_)
                            P.act(lambda e, dstb=dstb, gb=gb, a_=a_, b_=b_: e.activation(out=dstb[:, a_:b_], in_=G[gb][:, a_:b_], func=AF.Silu),
                                  r=["G%d/%d" % (gb, sg_)], w=["QK"])
                    caps.append(cap)
                P.add_interleaved(caps)
                P.dma("pool", wvb[:], wv[hh], w=["wvb"])
                for c2 in range(NCH // 2):
                    pbi = c2 % 2
                    for cc in range(2):
                        c = 2 * c2 + cc
                        for k in range(8):
                            P.pe(lambda e, c=c, cc=cc, k=k, pbi=pbi: e.matmul(pb[pbi][:, cc * 256:(cc + 1) * 256], lhsT=hT[:, k, c * 128:(c + 1) * 128], rhs=wvb[:, k, :],
                                                                              start=(k == 0), stop=(k == 7)), r=["hT", "wvb"], w=["pb%d" % pbi])
                    P.act(lambda e, c2=c2, pbi=pbi: e.activation(out=VT[:, c2 * 512:(c2 + 1) * 512], in_=pb[pbi][:, :], func=AF.Copy), r=["pb%d" % pbi], w=["G1"])
                pT = pb[7][:].bitcast(BF16)
                for c4 in range(0, NCH, 4):
                    nn = min(4, NCH - c4)
                    for cc in range(nn):
                        c = c4 + cc
                        P.pe(lambda e, c=c, cc=cc: e.transpose(pT[:, cc * 128:(cc + 1) * 128], kT[:, c * 128:(c + 1) * 128], identb[:]), r=["QK", "identb"], w=["pb7"])
                    P.act(lambda e, c4=c4, nn=nn: e.activation(out=ktok[:, c4:c4 + nn, :].rearrange("p c d -> p (c d)"), in_=pT[:, 0:nn * 128], func=AF.Copy),
                          r=["pb7"], w=["G0"])
                P.dve(lambda e: e.memset(G[2][:, :], 0.0), w=["G2"])
                P.dve(lambda e: e.memset(G[3][:, :], 0.0), w=["G3"])
                vcnt = 0
                for i in range(NCH):
                    caps = []
                    for d in range(2):
                        with P.capture() as cap:
                            c = orders[d][i]; cprev = orders[d][i - 1] if i > 0 else None
                            qc = qT[:, c * 128:(c + 1) * 128]; kc = kT[:, c * 128:(c + 1) * 128]
                            pS = pb[0 + d]; pO = pb[2 + d]; pD = pb[4 + d]
                            kS, kO, kD = "pb%d" % d, "pb%d" % (2 + d), "pb%d" % (4 + d)
                            v = vs[vcnt % 4]; vk = "vs%d" % (vcnt % 4); vcnt += 1
                            wcol = WV[:, d, c, hh:hh + 1]
                            hk = "G%d" % (2 + c // 17)
                            P.pe(lambda e, pS=pS, kc=kc, qc=qc: e.matmul(pS[:, 0:128], lhsT=kc, rhs=qc, start=True, stop=True), r=["QK"], w=[kS])
                            P.dve(lambda e, d=d, pS=pS: e.tensor_tensor(out=PT[d][:], in0=pS[:, 0:128], in1=masks[d][:], op=ALU.mult),
                                  r=[kS, "triU", "triL"], w=["PT%d" % d])
                            P.act(lambda e, v=v, c=c, wcol=wcol: e.activation(out=v[:, 0:256], in_=vtok[:, c, :], func=AF.Identity, scale=wcol),
                                  r=["G1", "WV"], w=[vk])
                            P.act(lambda e, v=v, wcol=wcol: e.activation(out=v[:, 256:257], in_=wcol, func=AF.Identity), r=["WV"], w=[vk])
                            P.pe(lambda e, d=d, pO=pO, v=v, i=i: e.matmul(pO[:, 0:257], lhsT=PT[d][:], rhs=v[:], start=True, stop=(i == 0)), r=["PT%d" % d, vk], w=[kO])
                            if i > 0:
                                P.pe(lambda e, d=d, pO=pO, qc=qc: e.matmul(pO[:, 0:257], lhsT=qc, rhs=Sbf[d][:], start=False, stop=True), r=["QK", "Sbf%d" % d], w=[kO])
                            ebc = EX[:, d, c, hh:hh + 1]
                            dd = dn[d]; dk_ = "dn%d" % d
                            P.dve(lambda e, dd=dd, pO=pO: e.tensor_copy(out=dd[:, 1:2], in_=pO[:, 256:257]), r=[kO], w=[dk_])
                            P.dve(lambda e, dd=dd: e.scalar_tensor_tensor(out=dd[:, 0:1], in0=dd[:, 1:2], scalar=-1.0, in1=dd[:, 1:2], op0=ALU.mult, op1=ALU.max), r=[dk_], w=[dk_])
                            P.dve(lambda e, dd=dd: e.reciprocal(out=dd[:, 2:3], in_=dd[:, 0:1]), r=[dk_], w=[dk_])
                            P.dve(lambda e, dd=dd, ebc=ebc: e.tensor_tensor(out=dd[:, 3:4], in0=dd[:, 2:3], in1=ebc, op=ALU.min), r=[dk_, "EX"], w=[dk_])
                            P.dve(lambda e, dd=dd, pO=pO, c=c: e.scalar_tensor_tensor(out=Hv(c), in0=pO[:, 0:256], scalar=dd[:, 3:4], in1=Hv(c), op0=ALU.mult, op1=ALU.add),
                                  r=[kO, dk_, hk], w=[hk])
                            if i < NCH - 1:
                                P.pe(lambda e, pD=pD, c=c, v=v: e.matmul(pD[:, 0:257], lhsT=ktok[:, c, :], rhs=v[:], start=True, stop=True), r=["G0", vk], w=[kD])
                                if i == 0:
                                    P.dve(lambda e, d=d, pD=pD: e.tensor_copy(out=Sacc[d][:], in_=pD[:, 0:257]), r=[kD], w=["Sacc%d" % d])
                                else:
                                    eprev = EX[:, 2 + d, cprev, hh:hh + 1]
                                    P.dve(lambda e, d=d, pD=pD, eprev=eprev: e.scalar_tensor_tensor(out=Sacc[d][:], in0=Sacc[d][:], scalar=eprev, in1=pD[:, 0:257],
                                                                                                    op0=ALU.mult, op1=ALU.add), r=[kD, "EX", "Sacc%d" % d], w=["Sacc%d" % d])
                                ecur = EX[:, 2 + d, c, hh:hh + 1]
                                P.act(lambda e, d=d, ecur=ecur: e.activation(out=Sbf[d][:], in_=Sacc[d][:], func=AF.Identity, scale=ecur), r=["Sacc%d" % d, "EX"], w=["Sbf%d" % d])
                            emit_pe_warm(P, pb[7], onesb, hT, 3)
                        caps.append(cap)
                    P.add_interleaved(caps)
                Hall = [g[:, 0:17 * 256].rearrange("p (c v) -> p c v", v=256) for g in Hm]
                for gi in range(2):
                    P.dve(lambda e, gi=gi: e.tensor_tensor(out=G[gi][:, 0:17 * 256], in0=Hm[gi][:, 0:17 * 256], in1=Hm[gi][:, 0:17 * 256], op=ALU.mult),
                          r=["G%d" % (2 + gi)], w=["G%d" % gi])
                    P.dve(lambda e, gi=gi: e.tensor_reduce(out=ssq[:, gi * 17:(gi + 1) * 17], in_=G[gi][:, 0:17 * 256].rearrange("p (c v) -> p c v", v=256), axis=AX.X, op=ALU.add),
                          r=["G%d" % gi], w=["ssq"])
                P.act(lambda e: e.activation(out=ssq[:], in_=ssq[:], func=AF.Sqrt, bias=epsc[:, 0:1], scale=1.0 / 256), r=["ssq", "epsc"], w=["ssq"])
                P.dve(lambda e: e.reciprocal(out=ssq[:], in_=ssq[:]), r=["ssq"], w=["ssq"])
                for gi in range(2):
                    P.dve(lambda e, gi=gi: e.tensor_tensor(out=Hall[gi], in0=Hall[gi], in1=ssq[:, gi * 17:(gi + 1) * 17].unsqueeze(2).to_broadcast([128, 17, 256]), op=ALU.mult),
                          r=["G%d" % (2 + gi), "ssq"], w=["G%d" % (2 + gi)])
                    P.dve(lambda e, gi=gi, hh=hh: e.tensor_tensor(out=Hall[gi], in0=Hall[gi], in1=mns[:, hh * 256:(hh + 1) * 256].unsqueeze(1).to_broadcast([128, 17, 256]), op=ALU.mult),
                          r=["G%d" % (2 + gi), "mns"], w=["G%d" % (2 + gi)])
                P.dma("pool", wvb[:], wo[:, :, hh * 256:(hh + 1) * 256], w=["wvb"])
                for c2 in range(NCH // 2):
                    pbi = c2 % 2; ob = obf[c2 % 2]; og = osig[c2 % 2]
                    for cc in range(2):
                        c = 2 * c2 + cc
                        for k in range(8):
                            P.pe(lambda e, c=c, cc=cc, k=k, pbi=pbi, hh=hh: e.matmul(pb[pbi][:, cc * 256:(cc + 1) * 256], lhsT=hT[:, k, c * 128:(c + 1) * 128],
                                                                                     rhs=wvb[:, k, :], start=(k == 0), stop=(k == 7)), r=["hT", "wvb"], w=["pb%d" % pbi])
                    P.act(lambda e, pbi=pbi, og=og: e.activation(out=og[:], in_=pb[pbi][:, :], func=AF.Sigmoid), r=["pb%d" % pbi], w=["tmp%d" % (c2 % 2)])
                    c0 = 2 * c2
                    hsrc = Hm[c0 // 17][:, (c0 % 17) * 256:(c0 % 17) * 256 + 512] if (c0 % 17) != 16 else None
                    if hsrc is not None:
                        P.dve(lambda e, ob=ob, og=og, hsrc=hsrc: e.tensor_tensor(out=ob[:], in0=og[:], in1=hsrc, op=ALU.mult),
                              r=["tmp%d" % (c2 % 2), "G%d" % (2 + c0 // 17)], w=["obf%d" % (c2 % 2)])
                    else:
                        for cc in range(2):
                            P.dve(lambda e, ob=ob, og=og, cc=cc, c0=c0: e.tensor_tensor(out=ob[:, cc * 256:(cc + 1) * 256], in0=og[:, cc * 256:(cc + 1) * 256], in1=Hv(c0 + cc), op=ALU.mult),
                                  r=["tmp%d" % (c2 % 2), "G2", "G3"], w=["obf%d" % (c2 % 2)])
                    ch0 = (2 * s + hh) * 2
                    emit_tok2fm(P, pb[7], identb, ob, "obf%d" % (c2 % 2), obT, "obT", out_mix[:, ch0:ch0 + 2, c0 * 128:(c0 + 2) * 128])
            xlb = XB[:, :]
            G4 = QK
            seg_tiles = [[(ti, t) for ti, t in enumerate(TOK_TILES) if t[0] < SEG_SPLIT], [(ti, t) for ti, t in enumerate(TOK_TILES) if t[0] >= SEG_SPLIT]]
            for nb in range(4):
                proj_fm(4 + nb, G[0], "G0")
                for sg_ in range(2):
                    a_, b_ = SEGS[sg_]
                    emit_conv_seg(P, G[0], "G0", G[1], "G1", cws[:, 4 + nb, :], cbs[:, 4 + nb:5 + nb], ["cws", "cbs"], sg_)
                    P.act(lambda e, a_=a_, b_=b_: e.activation(out=xlb[:, a_:b_], in_=G[1][:, a_:b_], func=AF.Copy), r=["G1/%d" % sg_],
                          w=["XB/%d" % sg_] + ["XB%d" % k for k in range(8)])
                for d in range(2):
                    for sg_ in range(2):
                        a_, b_ = SEGS[sg_]
                        for (wsb, bsb, dst, dkey, wkey, bkey) in [(lwab, lbas, G[2], "G2", "lwab", "lbas"), (lwxb, lbxs, G[3], "G3", "lwxb", "lbxs")]:
                            for ti, (t0, n, col) in seg_tiles[sg_]:
                                pbi = ti % 2
                                P.pe(lambda e, wsb=wsb, d=d, nb=nb, t0=t0, n=n, pbi=pbi: e.matmul(pb[pbi][:, :n], lhsT=wsb[:, d, nb, :], rhs=xlb[:, t0:t0 + n], start=True, stop=True),
                                     r=[wkey, "XB/%d" % sg_], w=["pb%d" % pbi])
                                emit_pe_warm(P, pb[7], onesb, hT, 1)
                                P.act(lambda e, bsb=bsb, dst=dst, d=d, nb=nb, t0=t0, n=n, pbi=pbi: e.activation(out=dst[:, t0:t0 + n], in_=pb[pbi][:, :n], func=AF.Sigmoid,
                                                                                                          bias=bsb[:, d, nb:nb + 1], scale=1.0), r=["pb%d" % pbi, bkey], w=["%s/%d" % (dkey, sg_)])
                        P.act(lambda e, d=d, nb=nb, a_=a_, b_=b_: e.activation(out=G[2][:, a_:b_], in_=G[2][:, a_:b_], func=AF.Exp, scale=LC[:, d, nb:nb + 1]),
                              r=["G2/%d" % sg_, "LC"], w=["G2/%d" % sg_])
                    for sg_ in range(2):
                        a_, b_ = SEGS[sg_]
                        K2, K3, K0, K1 = "G2/%d" % sg_, "G3/%d" % sg_, "G0/%d" % sg_, "G1/%d" % sg_
                        P.dve(lambda e, a_=a_, b_=b_: e.tensor_tensor(out=G[3][:, a_:b_], in0=G[3][:, a_:b_], in1=G[1][:, a_:b_], op=ALU.mult), r=[K3, K1], w=[K3])
                        P.dve(lambda e, a_=a_, b_=b_: e.tensor_tensor(out=G[0][:, a_:b_], in0=G[2][:, a_:b_], in1=G[2][:, a_:b_], op=ALU.mult), r=[K2], w=[K0])
                        P.act(lambda e, a_=a_, b_=b_: e.activation(out=G[0][:, a_:b_], in_=G[0][:, a_:b_], func=AF.Sqrt, scale=-1.0, bias=1.0), r=[K0], w=[K0])
                        P.dve(lambda e, a_=a_, b_=b_: e.tensor_tensor(out=G[3][:, a_:b_], in0=G[3][:, a_:b_], in1=G[0][:, a_:b_], op=ALU.mult), r=[K3, K0], w=[K3])
                    S = SEG_SPLIT
                    if d == 0:
                        P.dve(lambda e: e.tensor_tensor_scan(out=G4[:, 0:S], data0=G[2][:, 0:S], data1=G[3][:, 0:S], initial=0.0, op0=ALU.mult, op1=ALU.add),
                              r=["G2/0", "G3/0"], w=["QK/0"])
                        P.dve(lambda e: e.tensor_tensor_scan(out=G4[:, S:T_ALL], data0=G[2][:, S:T_ALL], data1=G[3][:, S:T_ALL], initial=G4[:, S - 1:S],
                                                             op0=ALU.mult, op1=ALU.add), r=["G2/1", "G3/1", "QK/0"], w=["QK/1"])
                    else:
                        P.dve(lambda e: e.tensor_tensor_scan(out=G[0][:, 255::-1], data0=G[2][:, 255::-1], data1=G[3][:, 255::-1], initial=0.0, op0=ALU.mult, op1=ALU.add),
                              r=["G2/0", "G3/0"], w=["G0/0"])
                        P.dve(lambda e: e.tensor_tensor_scan(out=G[0][:, T_ALL - 1:S - 1:-1], data0=G[2][:, T_ALL - 1:S - 1:-1], data1=G[3][:, T_ALL - 1:S - 1:-1],
                                                             initial=G[0][:, 0:1], op0=ALU.mult, op1=ALU.add), r=["G2/1", "G3/1", "G0/0"], w=["G0/1"])
                        P.dve(lambda e: e.tensor_tensor_scan(out=G[0][:, S - 1:255:-1], data0=G[2][:, S - 1:255:-1], data1=G[3][:, S - 1:255:-1],
                                                             initial=G[0][:, S:S + 1], op0=ALU.mult, op1=ALU.add), r=["G2/0", "G3/0", "G0/1", "G0/0"], w=["G0/0"])
                        for sg_ in range(2):
                            a_, b_ = SEGS[sg_]
                            P.dve(lambda e, a_=a_, b_=b_: e.tensor_tensor(out=G4[:, a_:b_], in0=G4[:, a_:b_], in1=G[0][:, a_:b_], op=ALU.add),
                                  r=["QK/%d" % sg_, "G0/%d" % sg_], w=["QK/%d" % sg_])
                def gate_evac(ti, t0, n, pbi):
                    a = tmp[0]; b_ = tmp[1]; ob = hlb[ti % 2]; okey = "obf%d" % (ti % 2); pk = "pb%d" % pbi
                    P.dve(lambda e: e.tensor_copy(out=a[:, :n], in_=pb[pbi][:, :n]), r=[pk], w=["tmp0"])
                    P.dve(lambda e: e.tensor_tensor(out=b_[:, :n], in0=a[:, :n], in1=a[:, :n], op=ALU.mult), r=["tmp0"], w=["tmp1"])
                    P.dve(lambda e: e.tensor_scalar(out=b_[:, :n], in0=b_[:, :n], scalar1=0.044715, scalar2=1.0, op0=ALU.mult, op1=ALU.add), r=["tmp1"], w=["tmp1"])
                    P.dve(lambda e: e.tensor_tensor(out=b_[:, :n], in0=b_[:, :n], in1=a[:, :n], op=ALU.mult), r=["tmp1", "tmp0"], w=["tmp1"])
                    P.act(lambda e: e.activation(out=b_[:, :n], in_=b_[:, :n], func=AF.Sigmoid, scale=2.0 * 0.7978845608028654), r=["tmp1"], w=["tmp1"])
                    P.dve(lambda e: e.tensor_tensor(out=a[:, :n], in0=a[:, :n], in1=b_[:, :n], op=ALU.mult), r=["tmp0", "tmp1"], w=["tmp0"])
                    P.dve(lambda e: e.tensor_tensor(out=ob[:, :n], in0=a[:, :n], in1=G4[:, t0:t0 + n], op=ALU.mult), r=["tmp0", sk("QK", t0)], w=[okey])
                    P.dma("sp", out_mix[:, 8 + 4 * s + nb, t0:t0 + n], ob[:, :n], r=[okey], final=True)
                proj_fm(8 + nb, None, None, evac=gate_evac)

        for s_ in range(2):
            half(s_, drs[s_])
        P.emit()


def mix_common_consts(layer, I):
    d = {}
    d["modw"] = modblk(I["mod_w"][layer], 0, 2048)
    d["modb"] = colvec(I["mod_b"][layer][0:2048])
    d["g0"] = colvec(I["norm_g"][layer][0])
    r = np.arange(128)
    d["triU"] = (r[:, None] <= r[None, :]).astype(np.float32)
    d["triL"] = (r[:, None] >= r[None, :]).astype(np.float32)
    import ml_dtypes
    d["identb"] = np.eye(128, dtype=np.float32).astype(ml_dtypes.bfloat16)
    return d


def mix0_consts(I, s):
    W = I["ab_in_w"][0]
    h0, h1 = 2 * s, 2 * s + 1
    qcol = lambda h: np.arange(128 * h, 128 * h + 128)
    kcol = lambda h: 512 + np.arange(128 * h, 128 * h + 128)
    blocks = [qcol(h0), kcol(h0), qcol(h1), kcol(h1)]
    blocks += [3088 + 512 * s + 128 * n + np.arange(128) for n in range(4)]
    blocks += [4112 + 512 * s + 128 * n + np.arange(128) for n in range(4)]
    d = {}
    d["wfm"] = np.stack([kblk(W[:, c]) for c in blocks], 0)
    d["wv"] = np.stack([kblk(W[:, 1024 + 256 * h:1024 + 256 * h + 256]) for h in (h0, h1)], 0)
    d["wo"] = kblk(W[:, 2048 + 512 * s:2048 + 512 * s + 512])
    gcols = [3072 + t * 4 + h for t in range(4) for h in (h0, h1)]
    d["wg"] = kblk(W[:, gcols])
    gb = I["m_gate_b"][0]
    d["gbias"] = np.ascontiguousarray(np.broadcast_to(np.array([gb[t, h] for t in range(4) for h in (h0, h1)], np.float32)[None, :], (128, 8)))
    cw = np.zeros((128, 8, 4), np.float32); cb = np.zeros((128, 8), np.float32)
    for bi in range(4):
        cw[:, bi, :] = I["m_conv_w"][0][:, blocks[bi]].T
        cb[:, bi] = I["m_conv_b"][0][blocks[bi]]
    for n in range(4):
        ch = 512 * s + 128 * n + np.arange(128)
        cw[:, 4 + n, :] = I["l_conv_w"][0][:, ch].T
        cb[:, 4 + n] = I["l_conv_b"][0][ch]
    d["cw"] = cw; d["cb"] = cb
    d["mnorm"] = np.ascontiguousarray(np.broadcast_to(I["m_norm_w"][0][512 * s:512 * s + 512][None, :], (128, 512)))
    d["lwa"] = np.ascontiguousarray(I["l_wa"][0][:, 4 * s:4 * s + 4].transpose(2, 0, 1, 3))
    d["lwx"] = np.ascontiguousarray(I["l_wx"][0][:, 4 * s:4 * s + 4].transpose(2, 0, 1, 3))
    pc = lambda v: np.ascontiguousarray(v[:, 512 * s:512 * s + 512].reshape(2, 4, 128).transpose(2, 0, 1))
    d["lba"] = pc(I["l_ba"][0]); d["lbx"] = pc(I["l_bx"][0]); d["llam"] = pc(I["l_lam"][0])
    return d


def stream_fm(I_x, I_ctx, b):
    return fm(np.concatenate([I_ctx[b], I_x[b]], 0))


DK_SCALE = float(128.0 ** -0.5)


def phase_mix1(nc, P, pb, drs, xT, out_mix):
    dr = drs[0]
    modw = dr("modw", [16, 128, 8, 128])
    modb = dr("modb", [128, 16])
    cT = dr("cT", [128, 8, 2])
    g0 = dr("g0", [128, 8])
    triUd = dr("triU", [128, 128])
    triLd = dr("triL", [128, 128])
    identd = dr("identb", [128, 128], BF16)
    identfd = dr("identf", [128, 128])
    with ExitStack() as es:
        sb, ps = _mk(nc, es)
        hT = sb("hT", [128, 8, T_ALL], BF16)
        G = [sb("G%d" % i, [128, T_ALL]) for i in range(4)]
        QA = sb("QA", [128, T_ALL]); XB = sb("XB", [128, T_ALL], BF16)
        qab = QA[:].bitcast(BF16)
        qT = qab[:, 0:T_ALL]; kT = qab[:, T_ALL:2 * T_ALL]
        VT = G[1][:].bitcast(BF16); vtok = VT.rearrange("p (c v) -> p c v", v=256)
        ktok = G[0][:].bitcast(BF16)[:, 0:T_ALL].rearrange("p (c d) -> p c d", d=128)
        tmp = [sb("tmp%d" % i, [128, 512]) for i in range(4)]
        rstd = sb("rstd", [128, 512]); onesb = sb("onesb", [128, 128], BF16); onesf = sb("onesf", [128, 128]); epsc = sb("epsc", [128, 1])
        g0sb = sb("g0sb", [128, 8]); A0 = sb("A0", [128, 8, 2])
        triU = sb("triU_sb", [128, 128]); triL = sb("triL_sb", [128, 128]); identb = sb("identb_sb", [128, 128], BF16); identf = sb("identf_sb", [128, 128])
        wfb = [sb("wfb0", [128, 8, 128], BF16)] * 2
        wvb = sb("wvb", [128, 8, 256], BF16); wgab = sb("wgab", [128, 8, 32], BF16); wdtb = sb("wdtb", [128, 8, 16], BF16)
        alph = sb("alph_sb", [64, 256], BF16); gns = sb("gns", [128, 512]); cws = sb("cws", [128, 6, 4]); cbs = sb("cbs", [128, 6])
        dtbs = sb("dtbs", [128, 16]); Aneg = sb("Aneg", [128, 16]); sDs = sb("sDs", [128, 8])
        DEC = sb("DEC", [128, 6, NCH, 16])
        EBLg = sb("EBLg", [128, NCH])
        Sacc = [sb("Sacc%d" % i, [128, 256]) for i in range(2)]; Sbf = [sb("Sbf%d" % i, [128, 256], BF16) for i in range(2)]
        PT = [sb("PT%d" % i, [128, 128], BF16) for i in range(2)]
        CBm = [sb("CBm%d" % i, [128, 128]) for i in range(2)]; MT = [sb("MT%d" % i, [128, 512], BF16) for i in range(2)]
        obT = MT[0]
        xdt = [sb("xdt%d" % i, [128, 256], BF16) for i in range(2)]; xw = [sb("xw%d" % i, [128, 256], BF16) for i in range(2)]
        ssq = sb("ssq", [128, NCH]); obf = [sb("obf%d" % i, [128, 512], BF16) for i in range(2)]
        for (dst, src, key) in [(g0sb, g0, "g0sb"), (triU, triUd, "triU"), (triL, triLd, "triL"), (identb, identd, "identb"), (identf, identfd, "identf")]:
            P.dma("sp", dst[:], src, w=[key])
        P.dve(lambda e: e.memset(onesb[:], 1.0), w=["onesb"])
        P.dve(lambda e: e.memset(onesf[:], 1.0), w=["onesf"])
        P.dve(lambda e: e.memset(epsc[:], EPS), w=["epsc"])
        mwb = [(G[1][:, 1024 * i:1024 * (i + 1)].rearrange("p (a b) -> p a b", b=128), ["mwst%d" % i]) for i in range(4)]
        modT = emit_modulation(P, nc, sb, pb[7], modw, modb, cT, 16, mwb)
        xin = G[0][:, 0:4096].rearrange("p (k n) -> p k n", k=8)
        sqv = XB[:, 0:4096].rearrange("p (k n) -> p k n", k=8)
        XBK = ["XB"] + ["XB%d" % k for k in range(8)]
        emit_adaln_in(P, nc, sb, pb, xT, modT, g0sb, hT, xin, "G0", sqv, "XB", tmp, rstd, onesb, epsc, A0)
        def half(s, dr):
            wfm = dr("wfm", [10, 128, 8, 128])
            wga = dr("wga", [128, 8, 32])
            wv = dr("wv", [2, 128, 8, 256])
            wr = dr("wr", [128, 8, 512])
            wz = dr("wz", [128, 8, 512])
            wdt = dr("wdt", [128, 8, 16])
            alphd = dr("alph", [64, 256])
            gnorm = dr("gnorm", [128, 512])
            cw = dr("cw", [128, 6, 4])
            cb = dr("cb", [128, 6])
            dtbd = dr("dtb", [128, 16])
            alogd = dr("alog", [128, 16])
            sDd = dr("sD", [128, 8])
            for (dst, src, key) in [(cws, cw, "cws"), (cbs, cb, "cbs"), (gns, gnorm, "gns"), (dtbs, dtbd, "dtbs"), (Aneg, alogd, "Aneg"), (sDs, sDd, "sDs")]:
                P.dma("sp", dst[:], src, w=[key])
            for (dst, src, key) in [(wgab, wga, "wgab"), (wdtb, wdt, "wdtb"), (alph, alphd, "alph")]:
                P.dma("pool", dst[:], src, w=[key])
            P.act(lambda e: e.activation(out=Aneg[:], in_=Aneg[:], func=AF.Exp), r=["Aneg"], w=["Aneg"])
            P.dve(lambda e: e.tensor_scalar(out=Aneg[:], in0=Aneg[:], scalar1=-1.0, scalar2=None, op0=ALU.mult), r=["Aneg"], w=["Aneg"])
            wfcnt = [0]

            def proj_fm(blk, dst, dkey):
                sl = wfcnt[0] % 2; wfcnt[0] += 1
                if sl == 0:
                    wdst = wfb[0][:]; wk = "wfb0"; wl = lambda k: wfb[0][:, k, :]
                else:
                    wdst = wvb[:, :, 128:256]; wk = "wvb"; wl = lambda k: wvb[:, k, 128:256]
                P.dma("pool", wdst, wfm[blk], w=[wk])
                for ti, (t0, n, col) in enumerate(TOK_TILES):
                    pbi = ti % 2
                    for k in range(8):
                        P.pe(lambda e, k=k, t0=t0, n=n, pbi=pbi: e.matmul(pb[pbi][:, :n], lhsT=wl(k), rhs=hT[:, k, t0:t0 + n],
                                                                          start=(k == 0), stop=(k == 7)), r=[wk, "hT"], w=["pb%d" % pbi])
                    P.act(lambda e, t0=t0, n=n, pbi=pbi: e.activation(out=dst[:, t0:t0 + n], in_=pb[pbi][:, :n], func=AF.Copy), r=["pb%d" % pbi], w=[sk(dkey, t0)])

            def proj_tok2(wsb, wkey, c2, pbi):
                for cc in range(2):
                    c = 2 * c2 + cc
                    for k in range(8):
                        P.pe(lambda e, c=c, cc=cc, k=k: e.matmul(pb[pbi][:, cc * 256:(cc + 1) * 256], lhsT=hT[:, k, c * 128:(c + 1) * 128], rhs=wsb[:, k, :],
                                                                 start=(k == 0), stop=(k == 7)), r=["hT", wkey], w=["pb%d" % pbi])

            masks = [triU, triL]; orders = [ORDER_F, ORDER_B]
            Hm = [G[2], G[3]]

            def Hv(c):
                g = Hm[c // 17]; cc = c % 17
                return g[:, cc * 256:(cc + 1) * 256]

            def headnorm_gate(normsb, nkey, nslice, gate_w_dram, func, ch0):
                Hall = [g[:, 0:17 * 256].rearrange("p (c v) -> p c v", v=256) for g in Hm]
                for gi in range(2):
                    P.dve(lambda e, gi=gi: e.tensor_tensor(out=G[gi][:, 0:17 * 256], in0=Hm[gi][:, 0:17 * 256], in1=Hm[gi][:, 0:17 * 256], op=ALU.mult),
                          r=["G%d" % (2 + gi)], w=["G%d" % gi])
                    P.dve(lambda e, gi=gi: e.tensor_reduce(out=ssq[:, gi * 17:(gi + 1) * 17], in_=G[gi][:, 0:17 * 256].rearrange("p (c v) -> p c v", v=256), axis=AX.X, op=ALU.add),
                          r=["G%d" % gi], w=["ssq"])
                P.act(lambda e: e.activation(out=ssq[:], in_=ssq[:], func=AF.Sqrt, bias=epsc[:, 0:1], scale=1.0 / 256), r=["ssq", "epsc"], w=["ssq"])
                P.dve(lambda e: e.reciprocal(out=ssq[:], in_=ssq[:]), r=["ssq"], w=["ssq"])
                for gi in range(2):
                    P.dve(lambda e, gi=gi: e.tensor_tensor(out=Hall[gi], in0=Hall[gi], in1=ssq[:, gi * 17:(gi + 1) * 17].unsqueeze(2).to_broadcast([128, 17, 256]), op=ALU.mult),
                          r=["G%d" % (2 + gi), "ssq"], w=["G%d" % (2 + gi)])
                    P.dve(lambda e, gi=gi: e.tensor_tensor(out=Hall[gi], in0=Hall[gi], in1=normsb[:, nslice].unsqueeze(1).to_broadcast([128, 17, 256]), op=ALU.mult),
                          r=["G%d" % (2 + gi), nkey], w=["G%d" % (2 + gi)])
                gate_out(gate_w_dram, func, ch0)

            def gate_out(gate_w_dram, func, ch0):
                P.dma("pool", wvb[:], gate_w_dram, w=["wvb"])
                for c2 in range(NCH // 2):
                    pbi = c2 % 2; ob = obf[c2 % 2]; og = tmp[c2 % 2]
                    proj_tok2(wvb, "wvb", c2, pbi)
                    P.act(lambda e, pbi=pbi, og=og: e.activation(out=og[:], in_=pb[pbi][:, :], func=func), r=["pb%d" % pbi], w=["tmp%d" % (c2 % 2)])
                    c0 = 2 * c2
                    for cc in range(2):
                        P.dve(lambda e, ob=ob, og=og, cc=cc, c0=c0: e.tensor_tensor(out=ob[:, cc * 256:(cc + 1) * 256], in0=og[:, cc * 256:(cc + 1) * 256], in1=Hv(c0 + cc), op=ALU.mult),
                              r=["tmp%d" % (c2 % 2), "G2", "G3"], w=["obf%d" % (c2 % 2)])
                    emit_tok2fm(P, pb[7], identb, ob, "obf%d" % (c2 % 2), obT, "MT0", out_mix[:, ch0:ch0 + 2, c0 * 128:(c0 + 2) * 128])

            P.dve(lambda e: e.memset(XB[0:64, :], 1.0), r=XBK, w=XBK)
            for d in range(2):
                for ti, (t0, n, col) in enumerate(TOK_TILES):
                    pbi = ti % 2
                    for k in range(8):
                        P.pe(lambda e, k=k, d=d, t0=t0, n=n, pbi=pbi: e.matmul(pb[pbi][0:16, :n], lhsT=wgab[:, k, d * 16:(d + 1) * 16], rhs=hT[:, k, t0:t0 + n],
                                                                              start=(k == 0), stop=(k == 7)), r=["wgab", "hT"], w=["pb%d" % pbi])
                    P.act(lambda e, d=d, t0=t0, n=n, pbi=pbi: e.activation(out=XB[32 * d:32 * d + 16, t0:t0 + n], in_=pb[pbi][0:16, :n], func=AF.Copy),
                          r=["pb%d" % pbi], w=XBK)
            for hh in range(2):
                P.dve(lambda e: e.memset(G[2][:, :], 0.0), w=["G2"])
                P.dve(lambda e: e.memset(G[3][:, :], 0.0), w=["G3"])
                for d in range(2):
                    proj_fm(2 * hh, G[0], "G0")
                    proj_fm(2 * hh + 1, G[1], "G1")
                    ecol = 127 if d == 0 else 0
                    for c4 in range(0, NCH, 4):
                        nn = min(4, NCH - c4); W = nn * 128; cs = slice(c4 * 128, c4 * 128 + W)
                        for cc in range(nn):
                            c = c4 + cc
                            P.pe(lambda e, c=c, cc=cc, d=d, hh=hh: e.matmul(pb[6][:, cc * 128:(cc + 1) * 128], lhsT=XB[32 * d:32 * d + 17, c * 128:(c + 1) * 128],
                                                                            rhs=alph[32 * d:32 * d + 17, hh * 128:(hh + 1) * 128], start=True, stop=True), r=XBK + ["alph"], w=["pb6"])
                        P.act(lambda e, W=W: e.activation(out=tmp[0][:, :W], in_=pb[6][:, :W], func=AF.Exp, scale=-1.0), r=["pb6"], w=["tmp0"])
                        P.act(lambda e, W=W: e.activation(out=tmp[0][:, :W], in_=tmp[0][:, :W], func=AF.Ln, bias=1.0), r=["tmp0"], w=["tmp0"])
                        for cc in range(nn):
                            P.pe(lambda e, cc=cc, d=d: e.matmul(pb[7][:, cc * 128:(cc + 1) * 128], lhsT=tmp[0][:, cc * 128:(cc + 1) * 128], rhs=masks[d][:], start=True, stop=True),
                                 r=["tmp0", "triU", "triL"], w=["pb7"])
                        P.act(lambda e, W=W: e.activation(out=tmp[1][:, :W], in_=pb[7][:, :W], func=AF.Exp, scale=-1.0 / 16), r=["pb7"], w=["tmp1"])
                        P.act(lambda e, W=W: e.activation(out=tmp[2][:, :W], in_=pb[7][:, :W], func=AF.Exp, scale=1.0 / 16), r=["pb7"], w=["tmp2"])
                        P.dve(lambda e, W=W, cs=cs: e.tensor_tensor(out=qT[:, cs], in0=G[0][:, cs], in1=tmp[1][:, :W], op=ALU.mult), r=["G0", "tmp1"], w=["QA"])
                        P.dve(lambda e, W=W, cs=cs: e.tensor_tensor(out=kT[:, cs], in0=G[1][:, cs], in1=tmp[2][:, :W], op=ALU.mult), r=["G1", "tmp2"], w=["QA"])
                        P.dve(lambda e, W=W, c4=c4, nn=nn, ecol=ecol: e.tensor_copy(out=EBLg[:, c4:c4 + nn], in_=tmp[1][:, ecol:W:128]), r=["tmp1"], w=["EBLg"])
                    if d == 0:
                        pass
                    P.dma("pool", wvb[:], wv[hh], w=["wvb"])
                    for c2 in range(NCH // 2):
                        pbi = c2 % 2
                        proj_tok2(wvb, "wvb", c2, pbi)
                        P.act(lambda e, c2=c2, pbi=pbi: e.activation(out=VT[:, c2 * 512:(c2 + 1) * 512], in_=pb[pbi][:, :], func=AF.Copy), r=["pb%d" % pbi], w=["G1"])
                    pT = pb[7][:].bitcast(BF16)
                    for c4 in range(0, NCH, 4):
                        nn = min(4, NCH - c4)
                        for cc in range(nn):
                            c = c4 + cc
                            P.pe(lambda e, c=c, cc=cc: e.transpose(pT[:, cc * 128:(cc + 1) * 128], kT[:, c * 128:(c + 1) * 128], identb[:]), r=["QA", "identb"], w=["pb7"])
                        P.act(lambda e, c4=c4, nn=nn: e.activation(out=ktok[:, c4:c4 + nn, :].rearrange("p c d -> p (c d)"), in_=pT[:, 0:nn * 128], func=AF.Copy),
                              r=["pb7"], w=["G0"])
                    for i in range(NCH):
                        c = orders[d][i]; cprev = orders[d][i - 1] if i > 0 else None
                        par = i % 2
                        qc = qT[:, c * 128:(c + 1) * 128]; kc = kT[:, c * 128:(c + 1) * 128]
                        pS = pb[0 + par]; pO = pb[2 + par]; pD = pb[4 + par]
                        kS, kO, kD = "pb%d" % par, "pb%d" % (2 + par), "pb%d" % (4 + par)
                        hk = "G%d" % (2 + c // 17)
                        P.pe(lambda e, pS=pS, kc=kc, qc=qc: e.matmul(pS[:, 0:128], lhsT=kc, rhs=qc, start=True, stop=True), r=["QA"], w=[kS])
                        P.dve(lambda e, par=par, pS=pS, d=d: e.tensor_tensor(out=PT[par][:], in0=pS[:, 0:128], in1=masks[d][:], op=ALU.mult),
                              r=[kS, "triU", "triL"], w=["PT%d" % par])
                        P.pe(lambda e, par=par, pO=pO, c=c, i=i: e.matmul(pO[:, 0:256], lhsT=PT[par][:], rhs=vtok[:, c, :], start=True, stop=(i == 0)), r=["PT%d" % par, "G1"], w=[kO])
                        if i > 0:
                            P.pe(lambda e, pO=pO, qc=qc: e.matmul(pO[:, 0:256], lhsT=qc, rhs=Sbf[0][:], start=False, stop=True), r=["QA", "Sbf0"], w=[kO])
                        P.dve(lambda e, pO=pO, c=c: e.scalar_tensor_tensor(out=Hv(c), in0=pO[:, 0:256], scalar=DK_SCALE, in1=Hv(c), op0=ALU.mult, op1=ALU.add),
                              r=[kO, hk], w=[hk])
                        if i < NCH - 1:
                            P.pe(lambda e, pD=pD, c=c: e.matmul(pD[:, 0:256], lhsT=ktok[:, c, :], rhs=vtok[:, c, :], start=True, stop=True), r=["G0", "G1"], w=[kD])
                            if i == 0:
                                P.dve(lambda e, pD=pD: e.tensor_copy(out=Sacc[0][:], in_=pD[:, 0:256]), r=[kD], w=["Sacc0"])
                            else:
                                eprev = EBLg[:, cprev:cprev + 1]
                                P.dve(lambda e, pD=pD, eprev=eprev: e.scalar_tensor_tensor(out=Sacc[0][:], in0=Sacc[0][:], scalar=eprev, in1=pD[:, 0:256],
                                                                                            op0=ALU.mult, op1=ALU.add), r=[kD, "EBLg", "Sacc0"], w=["Sacc0"])
                            ecur = EBLg[:, c:c + 1]
                            P.act(lambda e, ecur=ecur: e.activation(out=Sbf[0][:], in_=Sacc[0][:], func=AF.Identity, scale=ecur), r=["Sacc0", "EBLg"], w=["Sbf0"])
                        emit_pe_warm(P, pb[7], onesb, hT, 3)
                headnorm_gate(gns, "gns", slice(hh * 256, (hh + 1) * 256), wr[:, :, hh * 256:(hh + 1) * 256], AF.Silu, (2 * s + hh) * 2)
            DT, LA, B_S, EBS, EBL, W_S = [DEC[:, j] for j in range(6)]
            BL = EBL
            for half, pbx in [(0, pb[7]), (1, pb[6])]:
                for cc in range(17):
                    c = half * 17 + cc
                    for k in range(8):
                        P.pe(lambda e, c=c, cc=cc, k=k, pbx=pbx: e.matmul(pbx[:, cc * 16:(cc + 1) * 16], lhsT=hT[:, k, c * 128:(c + 1) * 128], rhs=wdtb[:, k, :],
                                                                          start=(k == 0), stop=(k == 7)), r=["hT", "wdtb"], w=["pb7" if half == 0 else "pb6"])
                P.dve(lambda e, half=half, pbx=pbx: e.tensor_tensor(out=DT[:, half * 17:(half + 1) * 17, :], in0=pbx[:, 0:272].rearrange("p (c g) -> p c g", g=16),
                                                                    in1=dtbs[:].unsqueeze(1).to_broadcast([128, 17, 16]), op=ALU.add), r=["pb7" if half == 0 else "pb6", "dtbs"], w=["DEC0"])
            P.act(lambda e: e.activation(out=DT, in_=DT, func=AF.Exp), r=["DEC0"], w=["DEC0"])
            P.act(lambda e: e.activation(out=DT, in_=DT, func=AF.Ln, bias=1.0), r=["DEC0"], w=["DEC0"])
            P.dve(lambda e: e.tensor_tensor(out=LA, in0=DT, in1=Aneg[:].unsqueeze(1).to_broadcast([128, NCH, 16]), op=ALU.mult), r=["DEC0", "Aneg"], w=["DEC1"])
            for d in range(2):
                pcs, pbl = pb[6], pb[7]
                P.pe(lambda e, d=d: e.matmul(pb[6][:, 0:272], lhsT=masks[d][:], rhs=LA[:, :, d * 8:(d + 1) * 8], start=True, stop=True), r=["triU", "triL", "DEC1"], w=["pb6"])
                P.pe(lambda e, d=d: e.matmul(pb[7][:, 0:272], lhsT=onesf[:], rhs=LA[:, :, d * 8:(d + 1) * 8], start=True, stop=True), r=["onesf", "DEC1"], w=["pb7"])
                P.dve(lambda e, d=d: e.tensor_copy(out=B_S[:, :, d * 8:(d + 1) * 8], in_=pb[6][:, 0:272].rearrange("p (c g) -> p c g", g=8)), r=["pb6"], w=["DEC2"])
                P.dve(lambda e, d=d: e.tensor_copy(out=BL[:, :, d * 8:(d + 1) * 8], in_=pb[7][:, 0:272].rearrange("p (c g) -> p c g", g=8)), r=["pb7"], w=["DEC5"])
            P.act(lambda e: e.activation(out=EBS, in_=B_S, func=AF.Exp), r=["DEC2"], w=["DEC4"])
            P.dve(lambda e: e.tensor_tensor(out=W_S, in0=BL, in1=B_S, op=ALU.subtract), r=["DEC2", "DEC5"], w=["DEC6"])
            P.act(lambda e: e.activation(out=EBL, in_=BL, func=AF.Exp), r=["DEC5", "DEC6"], w=["DEC5"])
            P.act(lambda e: e.activation(out=W_S, in_=W_S, func=AF.Exp), r=["DEC6"], w=["DEC6"])
            P.dve(lambda e: e.tensor_tensor(out=W_S, in0=W_S, in1=DT, op=ALU.mult), r=["DEC6", "DEC0"], w=["DEC6"])
            BT = qT; CT = kT
            btok = XB[:, :].rearrange("p (c n) -> p c n", n=128)
            for (blk, dstb) in [(8, BT), (9, CT)]:
                proj_fm(blk, G[3], "G3")
                for sg_ in range(2):
                    a_, b_ = SEGS[sg_]
                    emit_conv_seg(P, G[3], "G3", G[2], "G2", cws[:, blk - 4, :], cbs[:, blk - 4:blk - 3], ["cws", "cbs"], sg_)
                    P.act(lambda e, dstb=dstb, a_=a_, b_=b_: e.activation(out=dstb[:, a_:b_], in_=G[2][:, a_:b_], func=AF.Silu), r=["G2/%d" % sg_], w=["QA"])
            pT = pb[7][:].bitcast(BF16)
            for c4 in range(0, NCH, 4):
                nn = min(4, NCH - c4)
                for cc in range(nn):
                    c = c4 + cc
                    P.pe(lambda e, c=c, cc=cc: e.transpose(pT[:, cc * 128:(cc + 1) * 128], BT[:, c * 128:(c + 1) * 128], identb[:]), r=["QA", "identb"], w=["pb7"])
                P.act(lambda e, c4=c4, nn=nn: e.activation(out=btok[:, c4:c4 + nn, :].rearrange("p c d -> p (c d)"), in_=pT[:, 0:nn * 128], func=AF.Copy),
                      r=["pb7"], w=XBK)
            xtok = [G[0][:, 0:17 * 256].rearrange("p (c v) -> p c v", v=256), G[1][:, 0:17 * 256].rearrange("p (c v) -> p c v", v=256)]

            def Xv(c):
                return [G[0], G[1]][c // 17][:, (c % 17) * 256:(c % 17 + 1) * 256]

            for hf in range(2):
                for xb in range(2):
                    blk = 4 + 2 * hf + xb
                    proj_fm(blk, G[3], "G3")
                    for sg_ in range(2):
                        a_, b_ = SEGS[sg_]
                        emit_conv_seg(P, G[3], "G3", G[2], "G2", cws[:, blk - 4, :], cbs[:, blk - 4:blk - 3], ["cws", "cbs"], sg_)
                        P.act(lambda e, a_=a_, b_=b_: e.activation(out=G[3][:, a_:b_], in_=G[2][:, a_:b_], func=AF.Silu), r=["G2/%d" % sg_], w=["G3/%d" % sg_])
                    for c4 in range(0, NCH, 4):
                        nn = min(4, NCH - c4)
                        for cc in range(nn):
                            c = c4 + cc
                            P.pe(lambda e, c=c, cc=cc: e.transpose(pb[6][:, cc * 128:(cc + 1) * 128], G[3][:, c * 128:(c + 1) * 128], identf[:]), r=["G3", "identf"], w=["pb6"])
                        for cc in range(nn):
                            c = c4 + cc
                            P.act(lambda e, c=c, cc=cc, xb=xb: e.activation(out=Xv(c)[:, xb * 128:(xb + 1) * 128], in_=pb[6][:, cc * 128:(cc + 1) * 128], func=AF.Copy),
                                  r=["pb6"], w=["G%d" % (c // 17)])
                P.dve(lambda e: e.memset(G[2][:, :], 0.0), w=["G2"])
                P.dve(lambda e: e.memset(G[3][:, :], 0.0), w=["G3"])
                for i in range(NCH):
                    caps = []
                    for d in range(2):
                        with P.capture() as cap:
                            c = orders[d][i]
                            hs = slice(d * 8 + 4 * hf, d * 8 + 4 * hf + 4)
                            pA = pb[0 + d]; pB = pb[2 + d]; pC = pb[4 + d]
                            kA, kB, kC = "pb%d" % d, "pb%d" % (2 + d), "pb%d" % (4 + d)
                            hk = "G%d" % (2 + c // 17); xk = "G%d" % (c // 17)
                            Bc = BT[:, c * 128:(c + 1) * 128]; Cc = CT[:, c * 128:(c + 1) * 128]
                            P.pe(lambda e, pA=pA, Bc=Bc, Cc=Cc: e.matmul(pA[:, 0:128], lhsT=Bc, rhs=Cc, start=True, stop=True), r=["QA"], w=[kA])
                            P.dve(lambda e, d=d, pA=pA: e.tensor_tensor(out=CBm[d][:], in0=pA[:, 0:128], in1=masks[d][:], op=ALU.mult), r=[kA, "triU", "triL"], w=["CBm%d" % d])
                            R = tmp[d]; Rk = "tmp%d" % d
                            P.pool(lambda e, R=R, d=d, c=c, hs=hs: e.tensor_tensor(out=R[:].rearrange("p (e t) -> p e t", e=4), in0=masks[d][:].unsqueeze(1).to_broadcast([128, 4, 128]),
                                                                                  in1=LA[:, c, hs].unsqueeze(2).to_broadcast([128, 4, 128]), op=ALU.mult), r=["triU", "triL", "DEC1"], w=[Rk])
                            P.pe(lambda e, pB=pB, R=R: e.matmul(pB[:, :], lhsT=onesf[:], rhs=R[:], start=True, stop=True), r=["onesf", Rk], w=[kB])
                            Dl = tmp[2 + d]; Dk = "tmp%d" % (2 + d)
                            for e4 in range(4):
                                bcol = B_S[:, c, d * 8 + 4 * hf + e4:d * 8 + 4 * hf + e4 + 1]
                                P.dve(lambda e, pB=pB, Dl=Dl, e4=e4, bcol=bcol: e.tensor_scalar(out=Dl[:, e4 * 128:(e4 + 1) * 128], in0=pB[:, e4 * 128:(e4 + 1) * 128], scalar1=bcol, scalar2=0.0,
                                                                                                op0=ALU.subtract, op1=ALU.min), r=[kB, "DEC2"], w=[Dk])
                            P.act(lambda e, Dl=Dl: e.activation(out=Dl[:], in_=Dl[:], func=AF.Exp), r=[Dk], w=[Dk])
                            P.dve(lambda e, d=d, Dl=Dl: e.tensor_tensor(out=MT[d][:].rearrange("p (e t) -> p e t", e=4), in0=Dl[:].rearrange("p (e t) -> p e t", e=4),
                                                                        in1=CBm[d][:].unsqueeze(1).to_broadcast([128, 4, 128]), op=ALU.mult), r=[Dk, "CBm%d" % d], w=["MT%d" % d])
                            for e4 in range(4):
                                hi = d * 8 + 4 * hf + e4
                                P.act(lambda e, d=d, c=c, e4=e4, hi=hi: e.activation(out=xdt[d][:, e4 * 64:(e4 + 1) * 64], in_=Xv(c)[:, e4 * 64:(e4 + 1) * 64], func=AF.Identity,
                                                                                     scale=DT[:, c, hi:hi + 1]), r=[xk, "DEC0"], w=["xdt%d" % d])
                            for e4 in range(4):
                                hi = d * 8 + 4 * hf + e4
                                P.act(lambda e, d=d, c=c, e4=e4, hi=hi: e.activation(out=xw[d][:, e4 * 64:(e4 + 1) * 64], in_=Xv(c)[:, e4 * 64:(e4 + 1) * 64], func=AF.Identity,
                                                                                     scale=W_S[:, c, hi:hi + 1]), r=[xk, "DEC6"], w=["xw%d" % d])
                            for e4 in range(4):
                                P.pe(lambda e, pC=pC, d=d, e4=e4: e.matmul(pC[:, e4 * 64:(e4 + 1) * 64], lhsT=MT[d][:, e4 * 128:(e4 + 1) * 128], rhs=xdt[d][:, e4 * 64:(e4 + 1) * 64], start=True, stop=True),
                                     r=["MT%d" % d, "xdt%d" % d], w=[kC])
                            if i > 0:
                                P.pe(lambda e, pC=pC, d=d, Cc=Cc: e.matmul(pC[:, 256:512], lhsT=Cc, rhs=Sbf[d][:], start=True, stop=True), r=["QA", "Sbf%d" % d], w=[kC])
                                P.dve(lambda e, pC=pC, d=d, c=c, hs=hs, Dl=Dl: e.tensor_tensor(out=Dl[:, 0:256].rearrange("p (e q) -> p e q", e=4), in0=pC[:, 256:512].rearrange("p (e q) -> p e q", e=4),
                                                                                        in1=EBS[:, c, hs].unsqueeze(2).to_broadcast([128, 4, 64]), op=ALU.mult), r=[kC, "DEC4", "MT%d" % d], w=[Dk])
                                P.dve(lambda e, Dl=Dl, c=c: e.tensor_tensor(out=Hv(c), in0=Hv(c), in1=Dl[:, 0:256], op=ALU.add), r=[Dk, hk], w=[hk])
                            P.dve(lambda e, pC=pC, c=c: e.tensor_tensor(out=Hv(c), in0=Hv(c), in1=pC[:, 0:256], op=ALU.add), r=[kC, hk], w=[hk])
                            if i < NCH - 1:
                                P.pe(lambda e, pA=pA, c=c, d=d: e.matmul(pA[:, 256:512], lhsT=btok[:, c, :], rhs=xw[d][:], start=True, stop=True), r=XBK + ["xw%d" % d], w=[kA])
                                if i == 0:
                                    P.dve(lambda e, pA=pA, d=d: e.tensor_copy(out=Sacc[d][:], in_=pA[:, 256:512]), r=[kA], w=["Sacc%d" % d])
                                else:
                                    P.dve(lambda e, d=d, c=c, hs=hs: e.tensor_tensor(out=Sacc[d][:].rearrange("p (e q) -> p e q", e=4), in0=Sacc[d][:].rearrange("p (e q) -> p e q", e=4),
                                                                                     in1=EBL[:, c, hs].unsqueeze(2).to_broadcast([128, 4, 64]), op=ALU.mult), r=["Sacc%d" % d, "DEC5"], w=["Sacc%d" % d])
                                    P.dve(lambda e, pA=pA, d=d: e.tensor_tensor(out=Sacc[d][:], in0=Sacc[d][:], in1=pA[:, 256:512], op=ALU.add), r=[kA, "Sacc%d" % d], w=["Sacc%d" % d])
                                P.act(lambda e, d=d: e.activation(out=Sbf[d][:], in_=Sacc[d][:], func=AF.Copy), r=["Sacc%d" % d], w=["Sbf%d" % d])
                            emit_pe_warm(P, pb[7], onesb, hT, 3)
                        caps.append(cap)
                    P.add_interleaved(caps)
                for gi in range(2):
                    for e4 in range(4):
                        hv = Hm[gi][:, 0:17 * 256].rearrange("p (c v) -> p c v", v=256)[:, :, e4 * 64:(e4 + 1) * 64]
                        xv = [G[0], G[1]][gi][:, 0:17 * 256].rearrange("p (c v) -> p c v", v=256)[:, :, e4 * 64:(e4 + 1) * 64]
                        P.dve(lambda e, hv=hv, xv=xv, e4=e4, hf=hf: e.scalar_tensor_tensor(out=hv, in0=xv, scalar=sDs[:, 4 * hf + e4:4 * hf + e4 + 1], in1=hv, op0=ALU.mult, op1=ALU.add),
                              r=["G%d" % gi, "G%d" % (2 + gi), "sDs"], w=["G%d" % (2 + gi)])
                gate_out(wz[:, :, hf * 256:(hf + 1) * 256], AF.Silu, 8 + 4 * s + 2 * hf)

        for s_ in range(2):
            half(s_, drs[s_])
        P.emit()


def mix1_consts(I, s):
    import ml_dtypes
    W = I["cd_in_w"][0]
    h0, h1 = 2 * s, 2 * s + 1
    blocks = [np.arange(128 * h0, 128 * h0 + 128), 512 + np.arange(128 * h0, 128 * h0 + 128),
              np.arange(128 * h1, 128 * h1 + 128), 512 + np.arange(128 * h1, 128 * h1 + 128)]
    xoff = 4128
    blocks += [xoff + 512 * s + 128 * n + np.arange(128) for n in range(4)]
    blocks += [xoff + 1024 + 128 * s + np.arange(128), xoff + 1280 + 128 * s + np.arange(128)]
    d = {}
    d["wfm"] = np.stack([kblk(W[:, c]) for c in blocks], 0)
    d["wga"] = kblk(W[:, 3072:3104])
    d["wv"] = np.stack([kblk(W[:, 1024 + 256 * h:1024 + 256 * h + 256]) for h in (h0, h1)], 0)
    d["wr"] = kblk(W[:, 2048 + 512 * s:2048 + 512 * s + 512])
    d["wz"] = kblk(W[:, 3104 + 512 * s:3104 + 512 * s + 512])
    dtc = [5664 + dd * 16 + 8 * s + e for dd in range(2) for e in range(8)]
    d["wdt"] = kblk(W[:, dtc])
    al = np.zeros((64, 256), np.float32)
    for dd in range(2):
        al[32 * dd:32 * dd + 16] = I["g_alpha_w"][0][dd][:, 256 * s:256 * s + 256]
        al[32 * dd + 16] = I["g_alpha_b"][0][dd][256 * s:256 * s + 256]
    d["alph"] = al
    d["gnorm"] = np.ascontiguousarray(np.broadcast_to(I["g_norm_w"][0][512 * s:512 * s + 512][None, :], (128, 512)))
    cw = np.zeros((128, 6, 4), np.float32); cb = np.zeros((128, 6), np.float32)
    for bi in range(6):
        cols = blocks[4 + bi] - xoff
        cw[:, bi, :] = I["s_conv_w"][0][:, cols].T
        cb[:, bi] = I["s_conv_b"][0][cols]
    d["cw"] = cw; d["cb"] = cb
    bc = lambda v: np.ascontiguousarray(np.broadcast_to(np.asarray(v, np.float32)[None, :], (128, len(v))))
    d["dtb"] = bc([I["s_dt_bias"][0][dd, 8 * s + e] for dd in range(2) for e in range(8)])
    d["alog"] = bc([I["s_A_log"][0][dd, 8 * s + e] for dd in range(2) for e in range(8)])
    d["sD"] = bc(I["s_D"][0][8 * s:8 * s + 8])
    d["identf"] = np.eye(128, dtype=np.float32)
    return d


def build_fused():
    _PHASE[0] = 0
    nc = bass.Bass("TRN2", target_bir_lowering=False)
    mkdr = lambda sfx: (lambda name, shape, dt=F32: nc.dram_tensor(name + sfx, shape, dt, kind="ExternalInput").ap())
    xT = nc.dram_tensor("xT", [128, 8, T_ALL], F32, kind="ExternalInput").ap()
    msel = nc.dram_tensor("msel", [128, 2], F32, kind="ExternalInput").ap()
    xo = nc.dram_tensor("xo", [128, 8, 2048], F32, kind="ExternalOutput").ap()
    mix0s = nc.dram_tensor("mix0s", [128, 16, T_ALL], BF16, kind="Internal").ap()
    mix1s = nc.dram_tensor("mix1s", [128, 16, T_ALL], BF16, kind="Internal").ap()
    x1s = nc.dram_tensor("x1s", [128, 8, T_ALL], F32, kind="Internal").ap()
    with ExitStack() as es:
        P = Prog(nc, es)
        pb = [es.enter_context(nc.psum_tensor("pb%d" % i, [128, 512], F32)) for i in range(8)]
        phase_mix0(nc, P, pb, [mkdr("_m0s0"), mkdr("_m0s1")], xT, mix0s)
        phase_post(nc, P, pb, 0, mkdr("_p0"), TOK_TILES,
                   lambda t0, n: [xT[:, :, t0:t0 + n]], lambda t0, n: [mix0s[:, :, t0:t0 + n]], lambda t0, n: x1s[:, :, t0:t0 + n])
        phase_mix1(nc, P, pb, [mkdr("_m1s0"), mkdr("_m1s1")], x1s, mix1s)
        lat = lambda a, t0, n: [a[:, :, 256 + 2048 * r + t0:256 + 2048 * r + t0 + n] for r in range(2)]
        phase_post(nc, P, pb, 1, mkdr("_p1"), [(i * 512, 512, 0) for i in range(4)],
                   lambda t0, n: lat(x1s, t0, n), lambda t0, n: lat(mix1s, t0, n), lambda t0, n: xo[:, :, t0:t0 + n], msel_d=msel)
    return nc


def kernel(**I):
    I = {k: np.asarray(v) for k, v in I.items()}
    cores = list(range(8))
    shared = {}
    for h in range(2):
        d = dict(mix_common_consts(0, I)); d.update(mix0_consts(I, h))
        shared.update({k + "_m0s%d" % h: v for k, v in d.items()})
        d = dict(mix_common_consts(1, I)); d.update(mix1_consts(I, h))
        shared.update({k + "_m1s%d" % h: v for k, v in d.items()})
    shared.update({k + "_p0": v for k, v in post_consts(0, I).items()})
    shared.update({k + "_p1": v for k, v in post_consts(1, I).items()})
    in_maps = []
    for core in cores:
        b, s = core // 2, core % 2
        d = dict(shared)
        d["xT"] = stream_fm(I["x"], I["ctx"], b)
        cT = c_cols(I, b)
        for sfx in ("_m0s0", "_m0s1", "_m1s0", "_m1s1", "_p0", "_p1"):
            d["cT" + sfx] = cT
        m = np.zeros((128, 2), np.float32); m[:, s] = 1.0
        d["msel"] = m
        in_maps.append(d)
    res = run_bass_kernel_spmd(build_fused(), in_maps, core_ids=cores).results
    out = np.zeros((4, 4096, 1024), np.float32)
    for core in cores:
        b, s = core // 2, core % 2
        out[b, 2048 * s:2048 * s + 2048] = fm_inv(res[core]["xo"])
    return out
```
